# Optimizing a Trainium2 kernel written in Bass

```python
import math
import jax
import jax.numpy as jnp
from jax import lax
import numpy as np

D_MODEL = 1024
BATCH = 16
SEQ = 4096
DEPTH = 2

DIFF_HEADS = 4
DIFF_HEAD_DIM = 64
DSA_HEADS = 4
DSA_HEAD_DIM = 128
IDX_HEADS = 8
IDX_HEAD_DIM = 64
DSA_TOPK_MAX = 256
SSM_HEADS = 16
SSM_HEAD_DIM = 64
SSM_GROUPS = 2
SSM_STATE = 128
SSM_CONV = 4
SSM_CHUNK = 128
SSM_INNER = SSM_HEADS * SSM_HEAD_DIM
SSM_CONV_DIM = SSM_INNER + 2 * SSM_GROUPS * SSM_STATE
MOBA_HEADS = 8
MOBA_HEAD_DIM = 64
MOBA_BLOCK = 256
MOBA_TOPK = 3
MOBA_Q_CHUNK = 16
D_FF = ((8 * D_MODEL + 3 * 256 - 1) // (3 * 256)) * 256

ROPE_THETA = 10000.0
Q_BLOCK = 128
RMS_EPS = 1e-6

A_QK = 2 * DIFF_HEADS * DIFF_HEAD_DIM
A_V = DIFF_HEADS * 2 * DIFF_HEAD_DIM
B_Q = DSA_HEADS * DSA_HEAD_DIM
I_Q = IDX_HEADS * IDX_HEAD_DIM
EVEN_WIDTHS = (A_QK, A_QK, A_V, B_Q, DSA_HEAD_DIM, DSA_HEAD_DIM, I_Q, IDX_HEAD_DIM, IDX_HEADS)
EVEN_IN = sum(EVEN_WIDTHS)
EVEN_MIX = A_V + B_Q
M_QKV = MOBA_HEADS * MOBA_HEAD_DIM
ODD_WIDTHS = (SSM_INNER, SSM_CONV_DIM, SSM_HEADS, M_QKV, M_QKV, M_QKV)
ODD_IN = sum(ODD_WIDTHS)
ODD_MIX = SSM_INNER + M_QKV

kernel_name = 'hybrid_diff_dsa_ssd_moba_block'


def _split(t, widths):
    outs, start = [], 0
    for w in widths:
        outs.append(t[..., start:start + w])
        start += w
    return outs


def rms_norm(x, g):
    xf = x.astype(jnp.float32)
    y = xf * lax.rsqrt(jnp.mean(xf * xf, axis=-1, keepdims=True) + RMS_EPS)
    return (y * g.astype(jnp.float32)).astype(x.dtype)


def rope_tables(seq, dim):
    inv = ROPE_THETA ** (-jnp.arange(0, dim, 2, dtype=jnp.float32) / dim)
    ang = jnp.arange(seq, dtype=jnp.float32)[:, None] * inv[None, :]
    return jnp.cos(ang), jnp.sin(ang)


def apply_rope(t, cos, sin):
    half = t.shape[-1] // 2
    tf = t.astype(jnp.float32)
    t1, t2 = tf[..., :half], tf[..., half:]
    c = cos[None, :, None, :]
    s = sin[None, :, None, :]
    return jnp.concatenate([t1 * c - t2 * s, t2 * c + t1 * s], axis=-1).astype(t.dtype)


def diff_attention(q, k, v, lam, lam_init, subln_g):
    bsz, seq, h2, d = q.shape
    heads = h2 // 2
    scale = d ** -0.5
    kpos = jnp.arange(seq)

    def block(i):
        start = i * Q_BLOCK
        qb = lax.dynamic_slice_in_dim(q, start, Q_BLOCK, axis=1)
        logits = jnp.einsum('bqhd,bkhd->bhqk', qb, k).astype(jnp.float32) * scale
        qpos = start + jnp.arange(Q_BLOCK)
        logits = jnp.where(kpos[None, :] <= qpos[:, None], logits, -jnp.inf)
        p = jax.nn.softmax(logits, axis=-1).reshape(bsz, heads, 2, Q_BLOCK, seq)
        attn = p[:, :, 0] - lam * p[:, :, 1]
        return jnp.einsum('bhqk,bkhe->bqhe', attn.astype(v.dtype), v)

    out = lax.map(block, jnp.arange(seq // Q_BLOCK))
    out = out.transpose(1, 0, 2, 3, 4).reshape(bsz, seq, heads, 2 * d)
    out = rms_norm(out, subln_g) * (1.0 - lam_init)
    return out.reshape(bsz, seq, heads * 2 * d)


def dsa_attention(q, k, v, qi, ki, wi, topk):
    bsz, seq, hq, dh = q.shape
    scale = dh ** -0.5
    kpos = jnp.arange(seq)
    wi = wi.astype(jnp.float32) * (IDX_HEADS ** -0.5 * IDX_HEAD_DIM ** -0.5)
    take = jax.vmap(lambda t, idx: t[idx])

    def block(i):
        start = i * Q_BLOCK
        qpos = start + jnp.arange(Q_BLOCK)
        qib = lax.dynamic_slice_in_dim(qi, start, Q_BLOCK, axis=1)
        wib = lax.dynamic_slice_in_dim(wi, start, Q_BLOCK, axis=1)
        rel = jax.nn.relu(jnp.einsum('bqhd,bkd->bqhk', qib, ki).astype(jnp.float32))
        score = jnp.einsum('bqhk,bqh->bqk', rel, wib)
        score = jnp.where(kpos[None, None, :] <= qpos[None, :, None], score, -jnp.inf)
        _, idx = lax.top_k(score, topk)
        valid = idx <= qpos[None, :, None]
        ks = take(k, idx)
        vs = take(v, idx)
        qb = lax.dynamic_slice_in_dim(q, start, Q_BLOCK, axis=1)
        logits = jnp.einsum('bqhd,bqkd->bhqk', qb, ks).astype(jnp.float32) * scale
        logits = jnp.where(valid[:, None], logits, -jnp.inf)
        p = jax.nn.softmax(logits, axis=-1)
        return jnp.einsum('bhqk,bqkd->bqhd', p.astype(v.dtype), vs)

    out = lax.map(block, jnp.arange(seq // Q_BLOCK))
    return out.transpose(1, 0, 2, 3, 4).reshape(bsz, seq, hq * dh)


def ssd_scan(x, dt, a, bm, cm):
    bsz, seq, heads, hdim = x.shape
    groups, nstate = bm.shape[2], bm.shape[3]
    rep = heads // groups
    nch = seq // SSM_CHUNK
    f32 = jnp.float32
    xdt = x.astype(f32) * dt[..., None]

    def chunks(t):
        return t.reshape(bsz, nch, SSM_CHUNK, *t.shape[2:]).swapaxes(0, 1)

    tri = jnp.tril(jnp.ones((SSM_CHUNK, SSM_CHUNK), dtype=bool))

    def step(state, inp):
        xc, dac, bg, cg = inp
        bc = jnp.repeat(bg, rep, axis=2)
        cc = jnp.repeat(cg, rep, axis=2)
        acum = jnp.cumsum(dac, axis=1)
        seg = acum[:, :, None, :] - acum[:, None, :, :]
        decay = jnp.exp(jnp.where(tri[None, :, :, None], seg, -jnp.inf))
        scores = jnp.einsum('bthn,bshn->btsh', cc, bc) * decay
        y = jnp.einsum('btsh,bshp->bthp', scores, xc)
        y = y + jnp.einsum('bthn,bhpn->bthp', cc, state) * jnp.exp(acum)[..., None]
        tail = jnp.exp(acum[:, -1:, :] - acum)
        state = (state * jnp.exp(acum[:, -1, :])[:, :, None, None]
                 + jnp.einsum('bshn,bshp->bhpn', bc * tail[..., None], xc))
        return state, y

    state0 = jnp.zeros((bsz, heads, hdim, nstate), f32)
    _, ys = lax.scan(step, state0, (chunks(xdt), chunks(dt * a), chunks(bm.astype(f32)), chunks(cm.astype(f32))))
    return ys.swapaxes(0, 1).reshape(bsz, seq, heads, hdim)


def moba_attention(q, k, v):
    bsz, seq, heads, d = q.shape
    nblk = -(-seq // MOBA_BLOCK)
    pad = nblk * MOBA_BLOCK - seq

    def blocks(t):
        t = jnp.pad(t, ((0, 0), (0, pad), (0, 0), (0, 0)))
        return t.reshape(bsz, nblk, MOBA_BLOCK, heads, d).transpose(0, 3, 1, 2, 4)

    kb, vb = blocks(k), blocks(v)
    kmean = jnp.mean(kb.astype(jnp.float32), axis=3)
    nsel = min(MOBA_TOPK, nblk)
    scale = d ** -0.5
    blk_ids = jnp.arange(nblk)
    in_blk = jnp.arange(MOBA_BLOCK)
    take = jax.vmap(jax.vmap(lambda t, idx: t[idx]))

    def chunk(i):
        start = i * MOBA_Q_CHUNK
        own = start // MOBA_BLOCK
        qpos = start + jnp.arange(MOBA_Q_CHUNK)
        qc = lax.dynamic_slice_in_dim(q, start, MOBA_Q_CHUNK, axis=1).transpose(0, 2, 1, 3)
        gate = jnp.einsum('bhqd,bhnd->bhqn', qc.astype(jnp.float32), kmean)
        gate = jnp.where(blk_ids < own, gate, -jnp.inf)
        _, sel = lax.top_k(gate, nsel)
        valid = sel < own
        ks = take(kb, sel)
        vs = take(vb, sel)
        l_sel = jnp.einsum('bhqd,bhqnkd->bhqnk', qc, ks).astype(jnp.float32) * scale
        l_sel = jnp.where(valid[..., None], l_sel, -jnp.inf).reshape(bsz, heads, MOBA_Q_CHUNK, nsel * MOBA_BLOCK)
        k_own = lax.dynamic_index_in_dim(kb, own, axis=2, keepdims=False)
        v_own = lax.dynamic_index_in_dim(vb, own, axis=2, keepdims=False)
        l_own = jnp.einsum('bhqd,bhkd->bhqk', qc, k_own).astype(jnp.float32) * scale
        l_own = jnp.where(own * MOBA_BLOCK + in_blk[None, :] <= qpos[:, None], l_own, -jnp.inf)
        p = jax.nn.softmax(jnp.concatenate([l_sel, l_own], axis=-1), axis=-1).astype(v.dtype)
        p_sel = p[..., :nsel * MOBA_BLOCK].reshape(bsz, heads, MOBA_Q_CHUNK, nsel, MOBA_BLOCK)
        p_own = p[..., nsel * MOBA_BLOCK:]
        return (jnp.einsum('bhqnk,bhqnkd->bqhd', p_sel, vs)
                + jnp.einsum('bhqk,bhkd->bqhd', p_own, v_own))

    out = lax.map(chunk, jnp.arange(seq // MOBA_Q_CHUNK))
    return out.transpose(1, 0, 2, 3, 4).reshape(bsz, seq, heads * d)


def even_mixer(h, w_in, w_out, diff_lambda, diff_subln, lam_init, rope_a, rope_b, rope_i, dsa_topk):
    bsz, seq, _ = h.shape
    aq, ak, av, bq, bk, bv, iq, ik, iw = _split(h @ w_in, EVEN_WIDTHS)
    aq = apply_rope(aq.reshape(bsz, seq, 2 * DIFF_HEADS, DIFF_HEAD_DIM), *rope_a)
    ak = apply_rope(ak.reshape(bsz, seq, 2 * DIFF_HEADS, DIFF_HEAD_DIM), *rope_a)
    av = av.reshape(bsz, seq, DIFF_HEADS, 2 * DIFF_HEAD_DIM)
    lf = diff_lambda.astype(jnp.float32)
    lam = jnp.exp(jnp.sum(lf[0] * lf[1])) - jnp.exp(jnp.sum(lf[2] * lf[3])) + lam_init
    a_out = diff_attention(aq, ak, av, lam, lam_init, diff_subln)
    bq = apply_rope(bq.reshape(bsz, seq, DSA_HEADS, DSA_HEAD_DIM), *rope_b)
    bk = apply_rope(bk[:, :, None, :], *rope_b)[:, :, 0]
    iq = apply_rope(iq.reshape(bsz, seq, IDX_HEADS, IDX_HEAD_DIM), *rope_i)
    ik = apply_rope(ik[:, :, None, :], *rope_i)[:, :, 0]
    b_out = dsa_attention(bq, bk, bv, iq, ik, iw, dsa_topk)
    return jnp.concatenate([a_out, b_out], axis=-1) @ w_out


def odd_mixer(h, w_in, w_out, conv_w, conv_b, dt_bias, a_log, d_skip, norm_g, rope_m):
    bsz, seq, _ = h.shape
    f32 = jnp.float32
    z, xbc, dt_raw, mq, mk, mv = _split(h @ w_in, ODD_WIDTHS)
    xbc = lax.conv_general_dilated(xbc, conv_w[:, None, :], window_strides=(1,),
                                   padding=[(SSM_CONV - 1, 0)],
                                   dimension_numbers=('NWC', 'WIO', 'NWC'),
                                   feature_group_count=SSM_CONV_DIM)
    xbc = jax.nn.silu(xbc + conv_b)
    xs, bm, cm = _split(xbc, (SSM_INNER, SSM_GROUPS * SSM_STATE, SSM_GROUPS * SSM_STATE))
    xs = xs.reshape(bsz, seq, SSM_HEADS, SSM_HEAD_DIM)
    bm = bm.reshape(bsz, seq, SSM_GROUPS, SSM_STATE)
    cm = cm.reshape(bsz, seq, SSM_GROUPS, SSM_STATE)
    dt = jax.nn.softplus(dt_raw.astype(f32) + dt_bias.astype(f32))
    a = -jnp.exp(a_log.astype(f32))
    y = ssd_scan(xs, dt, a, bm, cm) + d_skip.astype(f32)[:, None] * xs.astype(f32)
    y = y.reshape(bsz, seq, SSM_INNER).astype(h.dtype) * jax.nn.silu(z)
    y = rms_norm(y.reshape(bsz, seq, SSM_GROUPS, SSM_INNER // SSM_GROUPS),
                 norm_g.reshape(SSM_GROUPS, SSM_INNER // SSM_GROUPS)).reshape(bsz, seq, SSM_INNER)
    mq = apply_rope(mq.reshape(bsz, seq, MOBA_HEADS, MOBA_HEAD_DIM), *rope_m)
    mk = apply_rope(mk.reshape(bsz, seq, MOBA_HEADS, MOBA_HEAD_DIM), *rope_m)
    mv = mv.reshape(bsz, seq, MOBA_HEADS, MOBA_HEAD_DIM)
    m_out = moba_attention(mq, mk, mv)
    return jnp.concatenate([y, m_out], axis=-1) @ w_out


def swiglu(h, w_gate, w_up, w_down):
    return (jax.nn.silu(h @ w_gate) * (h @ w_up)) @ w_down


def setup_inputs(seed: int = 0) -> dict:
    key = jax.random.key(seed)
    k = jax.random.split(key, 20)
    f32 = jnp.float32
    ne = (DEPTH + 1) // 2
    no = DEPTH // 2

    def dense(kk, shape, fan_in):
        return jax.random.normal(kk, shape, f32) * fan_in ** -0.5

    def gain(kk, shape):
        return 1.0 + 0.02 * jax.random.normal(kk, shape, f32)

    dt0 = jnp.exp(jax.random.uniform(k[18], (no, SSM_HEADS), f32, math.log(1e-3), math.log(1e-1)))
    return {
        'x': jax.random.normal(k[0], (BATCH, SEQ, D_MODEL), f32),
        'norm_mix_pre': gain(k[1], (DEPTH, D_MODEL)),
        'norm_mix_post': gain(k[2], (DEPTH, D_MODEL)),
        'norm_ffn_pre': gain(k[3], (DEPTH, D_MODEL)),
        'norm_ffn_post': gain(k[4], (DEPTH, D_MODEL)),
        'ffn_gate': dense(k[5], (DEPTH, D_MODEL, D_FF), D_MODEL),
        'ffn_up': dense(k[6], (DEPTH, D_MODEL, D_FF), D_MODEL),
        'ffn_down': dense(k[7], (DEPTH, D_FF, D_MODEL), D_FF),
        'even_w_in': dense(k[8], (ne, D_MODEL, EVEN_IN), D_MODEL),
        'even_w_out': dense(k[9], (ne, EVEN_MIX, D_MODEL), EVEN_MIX),
        'diff_lambda': 0.1 * jax.random.normal(k[10], (ne, 4, DIFF_HEAD_DIM), f32),
        'diff_subln': gain(k[11], (ne, 2 * DIFF_HEAD_DIM)),
        'odd_w_in': dense(k[12], (no, D_MODEL, ODD_IN), D_MODEL),
        'odd_w_out': dense(k[13], (no, ODD_MIX, D_MODEL), ODD_MIX),
        'ssm_conv_w': dense(k[14], (no, SSM_CONV, SSM_CONV_DIM), SSM_CONV),
        'ssm_conv_b': 0.02 * jax.random.normal(k[15], (no, SSM_CONV_DIM), f32),
        'ssm_dt_bias': dt0 + jnp.log(-jnp.expm1(-dt0)),
        'ssm_a_log': jnp.log(jax.random.uniform(k[16], (no, SSM_HEADS), f32, 1.0, 16.0)),
        'ssm_d': 1.0 + 0.1 * jax.random.normal(k[17], (no, SSM_HEADS), f32),
        'ssm_norm': gain(k[19], (no, SSM_INNER)),
    }


def reference(x, norm_mix_pre, norm_mix_post, norm_ffn_pre, norm_ffn_post, ffn_gate, ffn_up, ffn_down,
              even_w_in, even_w_out, diff_lambda, diff_subln, odd_w_in, odd_w_out, ssm_conv_w,
              ssm_conv_b, ssm_dt_bias, ssm_a_log, ssm_d, ssm_norm):
    seq = x.shape[1]
    rope_a = rope_tables(seq, DIFF_HEAD_DIM)
    rope_b = rope_tables(seq, DSA_HEAD_DIM)
    rope_i = rope_tables(seq, IDX_HEAD_DIM)
    rope_m = rope_tables(seq, MOBA_HEAD_DIM)
    dsa_topk = min(DSA_TOPK_MAX, seq // 4)
    h = x
    for i in range(DEPTH):
        j = i // 2
        hn = rms_norm(h, norm_mix_pre[i])
        if i % 2 == 0:
            lam_init = 0.8 - 0.6 * math.exp(-0.3 * i)
            mix = even_mixer(hn, even_w_in[j], even_w_out[j], diff_lambda[j], diff_subln[j], lam_init,
                             rope_a, rope_b, rope_i, dsa_topk)
        else:
            mix = odd_mixer(hn, odd_w_in[j], odd_w_out[j], ssm_conv_w[j], ssm_conv_b[j], ssm_dt_bias[j],
                            ssm_a_log[j], ssm_d[j], ssm_norm[j], rope_m)
        h = h + rms_norm(mix, norm_mix_post[i])
        hn = rms_norm(h, norm_ffn_pre[i])
        h = h + rms_norm(swiglu(hn, ffn_gate[i], ffn_up[i], ffn_down[i]), norm_ffn_post[i])
    return h
```

```python
from contextlib import ExitStack
import math
import numpy as np
import ml_dtypes
import concourse.bass as bass
import concourse.mybir as mybir
from concourse.bass_utils import run_bass_kernel_spmd

F32 = mybir.dt.float32
BF16 = mybir.dt.bfloat16
AF = mybir.ActivationFunctionType
ALU = mybir.AluOpType
AX = mybir.AxisListType

NCORES = 8
SPC = 2
S = 4096
D = 1024
DFF = 2816
NFT = DFF // 128
EPS = 1e-6
TB = 512
NTB = S // TB
NEG = -60000.0


class Buf:
    __slots__ = ("name", "w", "r")

    def __init__(self, name=""):
        self.name = name
        self.w = None
        self.r = {}


class Tile:
    __slots__ = ("t", "b")

    def __init__(self, t, name):
        self.t = t
        self.b = Buf(name)

    def __getitem__(self, idx):
        return self.t[idx]


class Prog:
    ENG = ("pe", "act", "dve", "pool", "sp")

    def __init__(self, nc, stack):
        self.nc = nc
        self.stack = stack
        self.eobj = {"pe": nc.tensor, "act": nc.scalar, "dve": nc.vector,
                     "pool": nc.gpsimd, "sp": nc.sync}
        self.esem = {}
        self.ecnt = {}
        for e in ("pe", "act", "dve", "pool"):
            self.esem[e] = stack.enter_context(nc.semaphore("s_" + e))
            self.ecnt[e] = 0
        self.waited = {e: {} for e in self.ENG}
        self.rings = {}
        for q, n in (("sp", 24), ("pool", 12), ("act", 8)):
            sems = [stack.enter_context(nc.semaphore(f"r_{q}{i}")) for i in range(n)]
            self.rings[q] = {"sems": sems, "n": 0}
        self.ninstr = 0

    def _need(self, eng, toks):
        best = {}
        for tk in toks:
            if tk is None:
                continue
            key, sem, val = tk
            if key == "pe" and eng == "pe":
                continue
            if self.waited[eng].get(key, 0) >= val:
                continue
            if key not in best or best[key][2] < val:
                best[key] = tk
        for key, (k, sem, val) in best.items():
            self.eobj[eng].wait_ge(sem, val)
            self.waited[eng][key] = val
            self.ninstr += 1

    def _deps(self, reads, writes):
        toks = []
        for b in reads:
            toks.append(b.w)
        for b in writes:
            toks.append(b.w)
            toks.extend(b.r.values())
        return toks

    def _record(self, tok, reads, writes):
        for b in reads:
            old = b.r.get(tok[0])
            if old is None or old[2] < tok[2]:
                b.r[tok[0]] = tok
        for b in writes:
            b.w = tok
            b.r = {}

    @staticmethod
    def _bufs(lst):
        return [x.b if isinstance(x, Tile) else x for x in lst]

    def op(self, eng, fn, reads=(), writes=()):
        reads = self._bufs(reads)
        writes = self._bufs(writes)
        self._need(eng, self._deps(reads, writes))
        ins = fn(self.eobj[eng])
        self.ecnt[eng] += 1
        ins.then_inc(self.esem[eng], 1)
        tok = (eng, self.esem[eng], self.ecnt[eng])
        self._record(tok, reads, writes)
        self.ninstr += 1
        return ins

    def dma(self, q, out, in_, reads=(), writes=(), **kw):
        reads = self._bufs(reads)
        writes = self._bufs(writes)
        ring = self.rings[q]
        n = ring["n"]
        R = len(ring["sems"])
        sem = ring["sems"][n % R]
        key = f"ring_{q}{n % R}"
        prev = 16 * (n // R)
        toks = self._deps(reads, writes)
        if prev > 0:
            toks.append((key, sem, prev))
        self._need(q, toks)
        ins = self.eobj[q].dma_start(out=out, in_=in_, **kw)
        ins.then_inc(sem, 16)
        ring["n"] = n + 1
        tok = (key, sem, prev + 16)
        self._record(tok, reads, writes)
        self.ninstr += 1
        return ins

    def barrier(self):
        toks = []
        for e in ("pe", "act", "dve", "pool"):
            if self.ecnt[e] > 0:
                toks.append((e, self.esem[e], self.ecnt[e]))
        for q, ring in self.rings.items():
            R = len(ring["sems"])
            for i in range(min(R, ring["n"])):
                cnt = (ring["n"] - 1 - i) // R + 1
                toks.append((f"ring_{q}{i}", ring["sems"][i], 16 * cnt))
        for e in self.ENG:
            self._need(e, toks)

    def sb(self, ctx, name, shape, dt):
        self.uid = getattr(self, "uid", 0) + 1
        name = f"sb{self.uid}_{name}"
        return Tile(ctx.enter_context(self.nc.sbuf_tensor(name, list(shape), dt)), name)

    def ps(self, ctx, name, shape, dt=F32):
        self.uid = getattr(self, "uid", 0) + 1
        name = f"ps{self.uid}_{name}"
        return Tile(ctx.enter_context(self.nc.psum_tensor(name, list(shape), dt)), name)


def bcast_rows(ap_1d, nparts):
    return ap_1d.partition_broadcast(nparts)


def _rope_np(dim):
    inv = (np.float32(10000.0) ** (-np.arange(0, dim, 2, dtype=np.float32) / np.float32(dim))).astype(np.float32)
    ang = (np.arange(S, dtype=np.float32)[:, None] * inv[None, :]).astype(np.float32)
    return np.cos(ang).astype(np.float32).T.copy(), np.sin(ang).astype(np.float32).T.copy()


def rope_tables_host():
    c64, s64 = _rope_np(64)
    c128, s128 = _rope_np(128)
    tab = np.zeros((3, 2, 128, S), np.float32)
    tab[0, 0] = np.tile(c64, (4, 1)); tab[0, 1] = np.tile(s64, (4, 1))
    tab[1, 0] = np.tile(c128, (2, 1)); tab[1, 1] = np.tile(s128, (2, 1))
    tab[2, 0, 0:64] = c128; tab[2, 1, 0:64] = s128
    tab[2, 0, 64:96] = c64; tab[2, 1, 64:96] = s64
    return tab


def pair_cols_h64(base, p):
    A = [base + (4 * p + m) * 64 + d for m in range(4) for d in range(32)]
    return A, [c + 32 for c in A]


def pair_cols_h128(base, p):
    A = [base + (2 * p + m) * 128 + d for m in range(2) for d in range(64)]
    return A, [c + 64 for c in A]


E_AQ, E_AK, E_AV, E_BQ, E_BK, E_BV, E_IQ, E_IK, E_IW = 0, 512, 1024, 1536, 2048, 2176, 2304, 2816, 2880


def l0_layout():
    fm_cols, types = [], []
    for base in (E_AQ, E_AK, E_IQ):
        for p in range(2):
            A, B = pair_cols_h64(base, p)
            fm_cols += A + B
            types.append(0)
    for p in range(2):
        A, B = pair_cols_h128(E_BQ, p)
        fm_cols += A + B
        types.append(1)
    A = [E_BK + d for d in range(64)] + [E_IK + d for d in range(32)] + [0] * 32
    B = [E_BK + 64 + d for d in range(64)] + [E_IK + 32 + d for d in range(32)] + [0] * 32
    fm_cols += A + B
    types.append(2)
    tm_cols = list(range(E_AV, E_AV + 512)) + list(range(E_BV, E_BV + 128)) + list(range(E_IW, E_IW + 8))
    return np.array(fm_cols), types, np.array(tm_cols)


def load_weight_bf16(P, ctx, name, w_ap, K, N, wt=None):
    kc = K // 128
    if wt is None:
        wt = P.sb(ctx, name, [128, kc, N], BF16)
    src = w_ap.rearrange("(c p) n -> p c n", p=128)
    step = max(1, 2048 // N) if N <= 2048 else 1
    for c0 in range(0, kc, step):
        c1 = min(kc, c0 + step)
        if N <= 2048:
            P.dma("pool", wt[:, c0:c1, :], src[:, c0:c1, :], writes=[wt])
        else:
            for n0 in range(0, N, 2048):
                n1 = min(N, n0 + 2048)
                P.dma("pool", wt[:, c0:c1, n0:n1], src[:, c0:c1, n0:n1], writes=[wt])
    return wt


def rms_block(P, xt, ss, junk, lnv, rstd, ntile):
    for j in range(ntile):
        P.op("act", lambda e, j=j: e.activation(out=junk[:, :], in_=xt[j][:, :], func=AF.Square,
                                                  accum_out=ss[:, j:j + 1]),
             reads=[xt[j]], writes=[junk, ss])
    P.op("act", lambda e: e.activation(out=lnv[:, 0:ntile], in_=ss[:, 0:ntile], func=AF.Ln,
                                        scale=1.0 / D, bias=P.eps_t[:, 0:1]),
         reads=[ss], writes=[lnv])
    P.op("act", lambda e: e.activation(out=rstd[:, 0:ntile], in_=lnv[:, 0:ntile], func=AF.Exp, scale=-0.5),
         reads=[lnv], writes=[rstd])


def phase_inproj(P, nc, cfg):
    x_ap, g_ap = cfg["x"], cfg["g"]
    nfm, ntm = cfg["nfm"], cfg["ntm"]
    with ExitStack() as ctx:
        wfm = load_weight_bf16(P, ctx, "wfm", cfg["wfm"], D, nfm * 128)
        wtm = load_weight_bf16(P, ctx, "wtm", cfg["wtm"], D, ntm)
        gbc = P.sb(ctx, "gbc", [128, D], F32)
        P.dma("sp", gbc[:, :], g_ap.partition_broadcast(128), writes=[gbc])
        xt = [[P.sb(ctx, f"xt{r}_{j}", [128, D], F32) for j in range(4)] for r in range(2)]
        hn = [P.sb(ctx, f"hn{j}", [128, D], BF16) for j in range(4)]
        hnT = [P.sb(ctx, f"hnT{r}", [128, 8, TB], BF16) for r in range(2)]
        junk = P.sb(ctx, "junk", [128, D], BF16)
        ss = [P.sb(ctx, f"ss{r}", [128, 4], F32) for r in range(2)]
        lnv = P.sb(ctx, "lnv", [128, 4], F32)
        rstd = [P.sb(ctx, f"rstd{r}", [128, 4], F32) for r in range(2)]
        ntab = len(set(t for t in cfg["types"] if t is not None))
        tabs = {}
        for ty in sorted(set(t for t in cfg["types"] if t is not None)):
            tabs[ty] = [[P.sb(ctx, f"tab{ty}_{cs}_{r}", [128, TB], F32) for cs in range(2)] for r in range(2)]
        tmp = [[P.sb(ctx, f"rt{r}_{i}", [128, TB], F32) for i in range(4)] for r in range(2)]
        oA = [P.sb(ctx, f"oA{r}", [128, TB], BF16) for r in range(3)]
        oB = [P.sb(ctx, f"oB{r}", [128, TB], BF16) for r in range(3)]
        tp = [P.ps(ctx, f"tp{r}", [128, TB], BF16) for r in range(2)]
        psA = [P.ps(ctx, f"psA{r}", [128, TB]) for r in range(2)]
        psB = [P.ps(ctx, f"psB{r}", [128, TB]) for r in range(2)]
        pst = [P.ps(ctx, f"pst{r}", [128, TB]) for r in range(2)]
        extra = cfg["alloc"](P, ctx) if "alloc" in cfg else None

        blocks = [(s, tb) for s in range(SPC) for tb in range(NTB)]

        def issue_loads(bi):
            s, tb = blocks[bi]
            r = bi % 2
            for j in range(4):
                t0 = tb * TB + j * 128
                P.dma("sp", xt[r][j][:, :], x_ap[s, t0:t0 + 128, :], writes=[xt[r][j]])
            for ty, tt in tabs.items():
                for cs in range(2):
                    P.dma("sp", tt[r][cs][:, :], cfg["rope"][ty, cs, :, tb * TB:(tb + 1) * TB], writes=[tt[r][cs]])

        issue_loads(0)
        ocnt = 0
        for bi, (s, tb) in enumerate(blocks):
            r = bi % 2
            if bi + 1 < len(blocks):
                issue_loads(bi + 1)
            rms_block(P, xt[r], ss[r], junk, lnv, rstd[r], 4)
            for j in range(4):
                P.op("dve", lambda e, j=j: e.scalar_tensor_tensor(
                    out=hn[j][:, :], in0=xt[r][j][:, :], scalar=rstd[r][:, j:j + 1], in1=gbc[:, :],
                    op0=ALU.mult, op1=ALU.mult), reads=[xt[r][j], rstd[r], gbc], writes=[hn[j]])
            for c in range(8):
                tpc = tp[c % 2]
                for j in range(4):
                    P.op("pe", lambda e, j=j, c=c, tpc=tpc: e.transpose(
                        out=tpc[:, j * 128:(j + 1) * 128], in_=hn[j][:, c * 128:(c + 1) * 128],
                        identity=P.ident[:, :]), reads=[hn[j], P.ident], writes=[tpc])
                P.op("act", lambda e, c=c, tpc=tpc: e.copy(out=hnT[r][:, c, :], in_=tpc[:, :]),
                     reads=[tpc], writes=[hnT[r]])
            ft = 0
            pi = 0
            while ft < nfm:
                ty = cfg["types"][pi]
                if ty is None:
                    pr = pi % 2
                    for c in range(8):
                        P.op("pe", lambda e, c=c, ft=ft, pr=pr: e.matmul(
                            psA[pr][:, :], lhsT=wfm[:, c, ft * 128:(ft + 1) * 128], rhs=hnT[r][:, c, :],
                            start=(c == 0), stop=(c == 7)), reads=[wfm, hnT[r]], writes=[psA[pr]])
                    cfg["plain_handler"](P, extra, s, tb, ft, psA[pr])
                    ft += 1
                    pi += 1
                    continue
                pr = pi % 2
                for half, ps in ((0, psA[pr]), (1, psB[pr])):
                    for c in range(8):
                        P.op("pe", lambda e, c=c, ps=ps, f=ft + half: e.matmul(
                            ps[:, :], lhsT=wfm[:, c, f * 128:(f + 1) * 128], rhs=hnT[r][:, c, :],
                            start=(c == 0), stop=(c == 7)), reads=[wfm, hnT[r]], writes=[ps])
                cosT, sinT = tabs[ty][r]
                t1, t2, t3, t4 = tmp[pr]
                A, B = psA[pr], psB[pr]
                for (o, a, b) in ((t1, A, cosT), (t2, B, sinT), (t3, B, cosT), (t4, A, sinT)):
                    P.op("dve", lambda e, o=o, a=a, b=b: e.tensor_tensor(out=o[:, :], in0=a[:, :], in1=b[:, :], op=ALU.mult),
                         reads=[a, b], writes=[o])
                oa, ob = oA[ocnt % 3], oB[ocnt % 3]
                ocnt += 1
                if cfg.get("rope_f32") and cfg["rope_f32"](pi):
                    cfg["rope_f32_handler"](P, extra, s, tb, pi, ft, t1, t2, t3, t4, oa, ob)
                else:
                    P.op("pool", lambda e, oa=oa: e.tensor_tensor(out=oa[:, :], in0=t1[:, :], in1=t2[:, :], op=ALU.subtract),
                         reads=[t1, t2], writes=[oa])
                    P.op("pool", lambda e, ob=ob: e.tensor_tensor(out=ob[:, :], in0=t3[:, :], in1=t4[:, :], op=ALU.add),
                         reads=[t3, t4], writes=[ob])
                P.dma("sp", cfg["fmT"][s, ft, :, tb * TB:(tb + 1) * TB], oa[:, :], reads=[oa])
                P.dma("sp", cfg["fmT"][s, ft + 1, :, tb * TB:(tb + 1) * TB], ob[:, :], reads=[ob])
                ft += 2
                pi += 1
            cfg["tm_handler"](P, extra, s, tb, r, hnT[r], wtm, pst)
    P.barrier()


def l0_alloc(P, ctx):
    ex = {}
    ex["tmo"] = [P.sb(ctx, f"tmo{r}", [128, 640], BF16) for r in range(2)]
    ex["tmw"] = [P.sb(ctx, f"tmw{r}", [128, 8], F32) for r in range(2)]
    ex["cnt"] = 0
    return ex


def make_l0_tm_handler(tmv, tmw):
    def handler(P, ex, s, tb, r, hnT, wtm, pst):
        for j in range(4):
            t0 = tb * TB + j * 128
            for c in range(8):
                P.op("pe", lambda e: e.matmul(pst[0][:, :], lhsT=hnT[:, c, j * 128:(j + 1) * 128], rhs=wtm[:, c, 0:512],
                                              start=(c == 0), stop=(c == 7)), reads=[hnT, wtm], writes=[pst[0]])
            for c in range(8):
                P.op("pe", lambda e: e.matmul(pst[1][:, 0:136], lhsT=hnT[:, c, j * 128:(j + 1) * 128], rhs=wtm[:, c, 512:648],
                                              start=(c == 0), stop=(c == 7)), reads=[hnT, wtm], writes=[pst[1]])
            k = ex["cnt"] % 2
            ex["cnt"] += 1
            o, w = ex["tmo"][k], ex["tmw"][k]
            P.op("act", lambda e: e.copy(out=o[:, 0:512], in_=pst[0][:, :]), reads=[pst[0]], writes=[o])
            P.op("act", lambda e: e.copy(out=o[:, 512:640], in_=pst[1][:, 0:128]), reads=[pst[1]], writes=[o])
            P.op("act", lambda e: e.copy(out=w[:, :], in_=pst[1][:, 128:136]), reads=[pst[1]], writes=[w])
            P.dma("sp", tmv[s, t0:t0 + 128, :], o[:, :], reads=[o])
            P.dma("sp", tmw[s, :, t0 // 128, :], w[:, :], reads=[w])
    return handler


def build_program(phases, debug=False, h2_input=False):
    nc = bass.Bass("TRN2", target_bir_lowering=False)
    dbg_names = set(debug) if isinstance(debug, (list, tuple, set)) else None

    def din(name, shape, dt=F32):
        return nc.dram_tensor(name, list(shape), dt, kind="ExternalInput").ap()

    def dsc(name, shape, dt):
        ext = debug and (dbg_names is None or name in dbg_names)
        return nc.dram_tensor(name, list(shape), dt, kind="ExternalOutput" if ext else "Internal").ap()

    spec = {}

    spec["x"] = (din, ("x", [SPC, S, D],))
    spec["norms"] = (din, ("norms", [8, D]))
    spec["rope"] = (din, ("rope", [3, 2, 128, S],))
    spec["ident"] = (din, ("ident", [128, 128], BF16,))
    spec["wfm0"] = (din, ("wfm0", [D, 18 * 128],))
    spec["wtm0"] = (din, ("wtm0", [D, 648],))
    spec["fm0"] = (dsc, ("fm0", [SPC, 18, 128, S], BF16,))
    spec["tmv0"] = (dsc, ("tmv0", [SPC, S, 640], BF16,))
    spec["tmw0"] = (dsc, ("tmw0", [SPC, 128, 32, 8], F32,))
    spec["mixT0"] = (dsc, ("mixT0", [SPC, 1024, S], BF16,))
    spec["diff_lambda"] = (din, ("diff_lambda", [4, 64],))
    spec["diff_subln"] = (din, ("diff_subln", [128],))
    spec["tri"] = (din, ("tri", [128, 128], BF16,))
    spec["dmask"] = (din, ("dmask", [4, 128, TB],))
    spec["pow2"] = (din, ("pow2", [NIT],))
    spec["wout0"] = (din, ("wout0", [1024, D],))
    spec["wg"] = (din, ("wg", [2, D, DFF],))
    spec["wu"] = (din, ("wu", [2, D, DFF],))
    spec["wd"] = (din, ("wd", [2, DFF, D],))
    spec["h1"] = (dsc, ("h1", [SPC, S, D], F32,))
    spec["h2"] = (dsc, ("h2", [SPC, S, D], F32,))
    spec["wfm1"] = (din, ("wfm1", [D, 20 * 128],))
    spec["wtm1"] = (din, ("wtm1", [D, 1552],))
    spec["wout1"] = (din, ("wout1", [1536, D],))
    spec["convw"] = (din, ("convw", [1536, 4],))
    spec["convb"] = (din, ("convb", [128, 12],))
    spec["pm"] = (din, ("pm", [256],))
    spec["oh"] = (din, ("oh", [256],))
    spec["blk1h"] = (din, ("blk1h", [16, S], BF16,))
    spec["triU"] = (din, ("triU", [128, 128],))
    spec["sel16"] = (din, ("sel16", [16, 16 * 128],))
    spec["ssm_norm"] = (din, ("ssm_norm", [1024],))
    spec["dsk"] = (din, ("dsk", [1024],))
    spec["dt_bias"] = (din, ("dt_bias", [16],))
    spec["a_log"] = (din, ("a_log", [16],))
    spec["fm1"] = (dsc, ("fm1", [SPC, 20, 128, S], BF16,))
    spec["xbcT"] = (dsc, ("xbcT", [SPC, 12, 128, S], BF16,))
    spec["mqf"] = (dsc, ("mqf", [SPC, 4, 128, S], F32,))
    spec["kmean"] = (dsc, ("kmean", [SPC, 4, 128, 16], F32,))
    spec["tm1"] = (dsc, ("tm1", [SPC, S, 1536], BF16,))
    spec["dtraw"] = (dsc, ("dtraw", [SPC, 128, 32, 16], F32,))
    spec["negT"] = (dsc, ("negT", [SPC, 128, S], BF16,))
    spec["mixT1"] = (dsc, ("mixT1", [SPC, 1536, S], BF16,))
    spec["h3"] = (dsc, ("h3", [SPC, S, D], F32,))
    if h2_input:
        spec["h2"] = (din, ("h2", [SPC, S, D]))
    if not debug:
        spec["out"] = (lambda name, shape, dt: nc.dram_tensor(name, list(shape), dt, kind="ExternalOutput").ap(), ("out", [SPC, S, D], F32))

    class Lazy(dict):
        def __missing__(self, key):
            fn, args = spec[key]
            v = fn(*args)
            self[key] = v
            return v

    T = Lazy()
    T["dbg"] = DBG
    with ExitStack() as stack:
        P = Prog(nc, stack)
        P.ident = P.sb(stack, "ident_sb", [128, 128], BF16)
        P.eps_t = P.sb(stack, "eps_t", [128, 1], F32)
        P.one_t = P.sb(stack, "one_t", [128, 1], F32)
        P.dma("sp", P.ident[:, :], T["ident"], writes=[P.ident])
        P.op("dve", lambda e: e.memset(P.eps_t[:, :], EPS), writes=[P.eps_t])
        P.op("dve", lambda e: e.memset(P.one_t[:, :], 1.0), writes=[P.one_t])
        _, types0, _ = l0_layout()
        if "A0" in phases:
            phase_inproj(P, nc, dict(x=T["x"], g=T["norms"][0], wfm=T["wfm0"], wtm=T["wtm0"], nfm=18, ntm=648,
                                     types=types0, rope=T["rope"], fmT=T["fm0"], alloc=l0_alloc,
                                     tm_handler=make_l0_tm_handler(T["tmv0"], T["tmw0"])))
        if "B0" in phases:
            phase_diff(P, nc, T, 0.8 - 0.6 * math.exp(-0.3 * 0))
        if "B1" in phases:
            phase_dsa(P, nc, T)
        if "C0" in phases:
            phase_outproj(P, nc, T["mixT0"], T["wout0"], 1024, T["norms"][1], T["x"], T["h1"],
                          (T["wg"][0], T["wu"][0], T["wd"][0], T["norms"][2], T["norms"][3], T["h1"], T["h2"]))
        if "A1" in phases:
            phase_inproj(P, nc, make_l1_cfg(T))
        if "B2" in phases:
            phase_ssd(P, nc, T)
        if "B3" in phases:
            phase_moba_gate(P, nc, T)
            phase_moba_attn(P, nc, T)
        if "C1" in phases:
            phase_outproj(P, nc, T["mixT1"], T["wout1"], 1536, T["norms"][5], T["h2"], T["h3"],
                          (T["wg"][1], T["wu"][1], T["wd"][1], T["norms"][6], T["norms"][7], T["h3"], T["h3"] if debug else T["out"]))
        P.barrier()
        nc.used_inputs = set(k for k in T.keys() if k in spec and spec[k][0] is din)
        print("instructions:", P.ninstr, {e: P.ecnt[e] for e in P.ecnt})
    return nc


def host_inputs(inputs):
    x = np.ascontiguousarray(inputs["x"], dtype=np.float32)
    fm_cols, _, tm_cols = l0_layout()
    w_in0 = np.asarray(inputs["even_w_in"][0], np.float32)
    fm_cols1, _, tm_cols1 = l1_layout()
    w_in1 = np.asarray(inputs["odd_w_in"][0], np.float32)
    norms = np.stack([inputs["norm_mix_pre"][0], inputs["norm_mix_post"][0], inputs["norm_ffn_pre"][0], inputs["norm_ffn_post"][0],
                      inputs["norm_mix_pre"][1], inputs["norm_mix_post"][1], inputs["norm_ffn_pre"][1], inputs["norm_ffn_post"][1]]).astype(np.float32)
    common = {
        "norms": norms,
        "rope": rope_tables_host(),
        "ident": np.eye(128, dtype=np.float32).astype(ml_dtypes.bfloat16),
        "wfm0": np.ascontiguousarray(w_in0[:, fm_cols]),
        "wtm0": np.ascontiguousarray(w_in0[:, tm_cols]),
        "diff_lambda": np.asarray(inputs["diff_lambda"][0], np.float32),
        "diff_subln": np.asarray(inputs["diff_subln"][0], np.float32),
        "dmask": np.stack([np.where(np.arange(TB)[None, :] <= 128 * qt + np.arange(128)[:, None], 0.0, NEG) for qt in range(4)]).astype(np.float32),
        "pow2": (0.5 ** np.arange(1, NIT + 1)).astype(np.float32),
        "wout0": np.asarray(inputs["even_w_out"][0], np.float32),
        "wg": np.asarray(inputs["ffn_gate"], np.float32),
        "wu": np.asarray(inputs["ffn_up"], np.float32),
        "wd": np.asarray(inputs["ffn_down"], np.float32),
        "wfm1": np.ascontiguousarray(w_in1[:, fm_cols1]),
        "wtm1": np.ascontiguousarray(w_in1[:, tm_cols1]),
        "wout1": np.asarray(inputs["odd_w_out"][0], np.float32),
        "convw": np.ascontiguousarray(np.asarray(inputs["ssm_conv_w"][0], np.float32).T),
        "convb": np.ascontiguousarray(np.asarray(inputs["ssm_conv_b"][0], np.float32).reshape(12, 128).T),
        "pm": np.where(np.arange(16)[None, :] < np.arange(16)[:, None], 0.0, NEG).astype(np.float32).reshape(256),
        "oh": np.eye(16, dtype=np.float32).reshape(256),
        "blk1h": (np.arange(S)[None, :] // 256 == np.arange(16)[:, None]).astype(np.float32).astype(ml_dtypes.bfloat16),
        "triU": np.triu(np.ones((128, 128), np.float32)),
        "sel16": np.repeat(np.eye(16, dtype=np.float32)[:, :, None], 128, axis=2).reshape(16, 16 * 128),
        "ssm_norm": np.asarray(inputs["ssm_norm"][0], np.float32),
        "dsk": np.repeat(np.asarray(inputs["ssm_d"][0], np.float32), 64),
        "dt_bias": np.asarray(inputs["ssm_dt_bias"][0], np.float32),
        "a_log": np.asarray(inputs["ssm_a_log"][0], np.float32),
        "tri": np.triu(np.ones((128, 128), np.float32)).astype(ml_dtypes.bfloat16),
    }
    maps = []
    for c in range(NCORES):
        m = dict(common)
        m["x"] = x[c * SPC:(c + 1) * SPC]
        maps.append(m)
    return maps


def filter_maps(nc, maps):
    used = nc.used_inputs
    return [{k: v for k, v in m.items() if k in used} for m in maps]


DBG = {}
ALL_PHASES = ["A0", "B0", "B1", "C0", "A1", "B2", "B3", "C1"]


def kernel(**inputs):
    nc = build_program(ALL_PHASES)
    maps = filter_maps(nc, host_inputs(inputs))
    res = run_bass_kernel_spmd(nc, maps, core_ids=list(range(NCORES)))
    return np.concatenate([r["out"] for r in res.results], axis=0)


def attn_core(P, A, qT, kT_of, v1_of, qb, E, scale, rd, mask_of=None, tri=None, diag_only_tri=True):
    nkt = 4 * (qb + 1)
    started = [False, False]

    def stage1(kt):
        r = kt - 4 * qb
        st = A["st"][A["i"] % 2]
        pt = A["pt"][A["i"] % len(A["pt"])]
        A["i"] += 1
        P.op("pe", lambda e: e.matmul(st[:, :], lhsT=kT_of(kt), rhs=qT, start=True, stop=True),
             reads=rd, writes=[st])
        P.op("act", lambda e: e.activation(out=pt[:, :], in_=st[:, :], func=AF.Exp, scale=scale),
             reads=[st], writes=[pt])
        if mask_of is not None:
            m_ap, m_t = mask_of(kt)
            P.op("pool", lambda e: e.tensor_tensor(out=pt[:, :], in0=pt[:, :], in1=m_ap, op=ALU.mult),
                 reads=[pt, m_t], writes=[pt])
        elif r >= 0:
            P.op("pool", lambda e: e.tensor_tensor(out=pt[:, r * 128:(r + 1) * 128], in0=pt[:, r * 128:(r + 1) * 128],
                                                   in1=tri[:, :], op=ALU.mult), reads=[pt, tri], writes=[pt])
        return pt

    def stage2(kt, pt):
        r = kt - 4 * qb
        for qt in range(4):
            if r > qt:
                continue
            acc = A["acc"][qt // 2]
            first = not started[qt // 2]
            started[qt // 2] = True
            last = (kt == 4 * qb + qt)
            P.op("pe", lambda e: e.matmul(acc[:, qt % 2, 0:E + 1], lhsT=pt[:, qt * 128:(qt + 1) * 128], rhs=v1_of(kt),
                                          start=first, stop=last), reads=[pt] + rd, writes=[acc])

    prev = None
    for kt in range(nkt):
        cur = stage1(kt)
        if prev is not None:
            stage2(*prev)
        prev = (kt, cur)
    stage2(*prev)


def q_rows_h64(fm, s, base_tile, m):
    p, ml = m // 4, m % 4
    return fm[s, base_tile + 2 * p, 32 * ml:32 * ml + 32, :], fm[s, base_tile + 2 * p + 1, 32 * ml:32 * ml + 32, :]


def compute_lambda(P, ctx, dl_ap, lam_init):
    lf = P.sb(ctx, "lf", [128, 4, 64], F32)
    P.dma("sp", lf[:, :, :], dl_ap.rearrange("a d -> (a d)").partition_broadcast(128).rearrange("p (a d) -> p a d", a=4), writes=[lf])
    pr = P.sb(ctx, "lpr", [128, 2, 64], F32)
    sm = P.sb(ctx, "lsm", [128, 2], F32)
    ex = P.sb(ctx, "lex", [128, 2], F32)
    nl = P.sb(ctx, "nlam", [128, 1], F32)
    P.op("dve", lambda e: e.tensor_tensor(out=pr[:, 0, :], in0=lf[:, 0, :], in1=lf[:, 1, :], op=ALU.mult), reads=[lf], writes=[pr])
    P.op("dve", lambda e: e.tensor_tensor(out=pr[:, 1, :], in0=lf[:, 2, :], in1=lf[:, 3, :], op=ALU.mult), reads=[lf, pr], writes=[pr])
    P.op("dve", lambda e: e.tensor_reduce(out=sm[:, :], in_=pr[:, :, :], axis=AX.X, op=ALU.add), reads=[pr], writes=[sm])
    P.op("act", lambda e: e.activation(out=ex[:, :], in_=sm[:, :], func=AF.Exp), reads=[sm], writes=[ex])
    P.op("dve", lambda e: e.tensor_tensor(out=nl[:, :], in0=ex[:, 1:2], in1=ex[:, 0:1], op=ALU.subtract), reads=[ex], writes=[nl])
    P.op("dve", lambda e: e.tensor_scalar(out=nl[:, :], in0=nl[:, :], scalar1=-lam_init, scalar2=None, op0=ALU.add), reads=[nl], writes=[nl])
    return nl


def phase_diff(P, nc, T, lam_init):
    fm, tmv, mixT = T["fm0"], T["tmv0"], T["mixT0"]
    with ExitStack() as ctx:
        nlam = compute_lambda(P, ctx, T["diff_lambda"], lam_init)
        g2 = P.sb(ctx, "g2", [128, 128], F32)
        P.dma("sp", g2[:, :], T["diff_subln"].partition_broadcast(128), writes=[g2])
        P.op("dve", lambda e: e.tensor_scalar(out=g2[:, :], in0=g2[:, :], scalar1=1.0 - lam_init, scalar2=None, op0=ALU.mult),
             reads=[g2], writes=[g2])
        tri = P.sb(ctx, "tri", [128, 128], BF16)
        P.dma("sp", tri[:, :], T["tri"], writes=[tri])
        kT = [P.sb(ctx, f"kT{r}", [128, S], BF16) for r in range(2)]
        v1 = [P.sb(ctx, f"v1{r}", [128, 32, 129], BF16) for r in range(2)]
        for r in range(2):
            P.op("pool", lambda e: e.memset(v1[r][:, :, 128:129], 1.0), writes=[v1[r]])
        qT = [P.sb(ctx, f"qT{r}", [128, TB], BF16) for r in range(2)]
        A = {"st": [P.ps(ctx, f"st{r}", [128, TB]) for r in range(2)],
             "pt": [P.sb(ctx, f"pt{r}", [128, TB], BF16) for r in range(3)], "i": 0}
        accs = [[P.ps(ctx, f"acc{m}_{r}", [128, 2, 256]) for r in range(2)] for m in range(2)]
        tp = P.ps(ctx, "tpo", [128, TB], BF16)
        o1 = [P.sb(ctx, f"o1_{r}", [128, 4, 128], F32) for r in range(2)]
        rc = [P.sb(ctx, f"rc{r}", [128, 8], F32) for r in range(2)]
        ss = P.sb(ctx, "dss", [128, 4], F32)
        lnv = P.sb(ctx, "dlnv", [128, 4], F32)
        rstd = P.sb(ctx, "drstd", [128, 4], F32)
        junk = P.sb(ctx, "djunk", [128, 128], BF16)
        ob = [P.sb(ctx, f"ob{r}", [128, 4, 128], BF16) for r in range(2)]
        oT = [P.sb(ctx, f"oT{r}", [128, TB], BF16) for r in range(2)]
        it = 0
        hi = 0
        for s in range(SPC):
            for h in range(4):
                kr = hi % 2
                hi += 1
                hh = h % 2
                p = h // 2
                srcs = [(4 + 2 * p, 64 * hh), (5 + 2 * p, 64 * hh), (4 + 2 * p, 64 * hh + 32), (5 + 2 * p, 64 * hh + 32)]
                for i, (tile, row) in enumerate(srcs):
                    P.dma("sp", kT[kr][32 * i:32 * i + 32, :], fm[s, tile, row:row + 32, :], writes=[kT[kr]])
                for half in range(2):
                    P.dma("sp", v1[kr][:, 16 * half:16 * half + 16, 0:128],
                          tmv[s, 2048 * half:2048 * (half + 1), h * 128:(h + 1) * 128].rearrange("(kt p) e -> p kt e", p=128),
                          writes=[v1[kr]])
                for qb in range(NTB):
                    qr = it % 2
                    it += 1
                    qsrcs = [(2 * p, 64 * hh), (2 * p + 1, 64 * hh), (2 * p, 64 * hh + 32), (2 * p + 1, 64 * hh + 32)]
                    for i, (tile, row) in enumerate(qsrcs):
                        P.dma("sp", qT[qr][32 * i:32 * i + 32, :], fm[s, tile, row:row + 32, qb * TB:(qb + 1) * TB], writes=[qT[qr]])
                    for m in range(2):
                        A["acc"] = accs[m]
                        attn_core(P, A, qT[qr][64 * m:64 * m + 64, :],
                                  lambda kt: kT[kr][64 * m:64 * m + 64, kt * 128:(kt + 1) * 128],
                                  lambda kt: v1[kr][:, kt, :], qb, 128, 0.125, [qT[qr], kT[kr], v1[kr]], tri=tri)
                        for qt in range(4):
                            acc = accs[m][qt // 2]
                            P.op("dve", lambda e: e.reciprocal(out=rc[qr][:, 4 * m + qt:4 * m + qt + 1], in_=acc[:, qt % 2, 128:129]),
                                 reads=[acc], writes=[rc[qr]])
                        if m == 1:
                            P.op("dve", lambda e: e.tensor_scalar(out=rc[qr][:, 4:8], in0=rc[qr][:, 4:8], scalar1=nlam[:, 0:1], scalar2=None,
                                                                  op0=ALU.mult), reads=[rc[qr], nlam], writes=[rc[qr]])
                        for qt in range(4):
                            acc = accs[m][qt // 2]
                            if m == 0:
                                P.op("dve", lambda e: e.tensor_scalar(out=o1[qr][:, qt, :], in0=acc[:, qt % 2, 0:128],
                                                                      scalar1=rc[qr][:, qt:qt + 1], scalar2=None, op0=ALU.mult),
                                     reads=[acc, rc[qr]], writes=[o1[qr]])
                            else:
                                P.op("dve", lambda e: e.scalar_tensor_tensor(out=o1[qr][:, qt, :], in0=acc[:, qt % 2, 0:128],
                                                                             scalar=rc[qr][:, 4 + qt:5 + qt], in1=o1[qr][:, qt, :],
                                                                             op0=ALU.mult, op1=ALU.add),
                                     reads=[acc, rc[qr], o1[qr]], writes=[o1[qr]])
                    for qt in range(4):
                        P.op("act", lambda e: e.activation(out=junk[:, :], in_=o1[qr][:, qt, :], func=AF.Square, accum_out=ss[:, qt:qt + 1]),
                             reads=[o1[qr]], writes=[junk, ss])
                    P.op("act", lambda e: e.activation(out=lnv[:, :], in_=ss[:, :], func=AF.Ln, scale=1.0 / 128, bias=P.eps_t[:, 0:1]),
                         reads=[ss], writes=[lnv])
                    P.op("act", lambda e: e.activation(out=rstd[:, :], in_=lnv[:, :], func=AF.Exp, scale=-0.5), reads=[lnv], writes=[rstd])
                    for qt in range(4):
                        P.op("dve", lambda e: e.scalar_tensor_tensor(out=ob[qr][:, qt, :], in0=o1[qr][:, qt, :], scalar=rstd[:, qt:qt + 1],
                                                                     in1=g2[:, :], op0=ALU.mult, op1=ALU.mult),
                             reads=[o1[qr], rstd, g2], writes=[ob[qr]])
                        P.op("pe", lambda e: e.transpose(out=tp[:, qt * 128:(qt + 1) * 128], in_=ob[qr][:, qt, :], identity=P.ident[:, :]),
                             reads=[ob[qr], P.ident], writes=[tp])
                    P.op("act", lambda e: e.copy(out=oT[qr][:, :], in_=tp[:, :]), reads=[tp], writes=[oT[qr]])
                    P.dma("sp", mixT[s, h * 128:(h + 1) * 128, qb * TB:(qb + 1) * TB], oT[qr][:, :], reads=[oT[qr]])
    P.barrier()


def rstd_from_ss(P, ss, lnv, rstd, n, dim):
    P.op("act", lambda e: e.activation(out=lnv[:, 0:n], in_=ss[:, 0:n], func=AF.Ln, scale=1.0 / dim, bias=P.eps_t[:, 0:1]),
         reads=[ss], writes=[lnv])
    P.op("act", lambda e: e.activation(out=rstd[:, 0:n], in_=lnv[:, 0:n], func=AF.Exp, scale=-0.5), reads=[lnv], writes=[rstd])


def phase_outproj(P, nc, mixT, wout_ap, kmix, g_ap, h_in, h_out, ffn_args):
    kc = kmix // 128
    with ExitStack() as octx:
      wg_t = P.sb(octx, "wg", [128, 8, DFF], BF16)
      wu_t = P.sb(octx, "wu", [128, 8, DFF], BF16)
      with ExitStack() as ctx:
        wout = load_weight_bf16(P, ctx, "wout", wout_ap, kmix, D)
        pre = {"wg": load_weight_bf16(P, ctx, "wg", ffn_args[0], D, DFF, wt=wg_t),
               "wu": load_weight_bf16(P, ctx, "wu", ffn_args[1], D, DFF, wt=wu_t)}
        gbc = P.sb(ctx, "gbc", [128, D], F32)
        P.dma("sp", gbc[:, :], g_ap.partition_broadcast(128), writes=[gbc])
        mt = [P.sb(ctx, f"mt{r}", [128, kc, TB], BF16) for r in range(2)]
        ht = [[P.sb(ctx, f"ht{r}_{j}", [128, D], F32) for j in range(4)] for r in range(2)]
        mo = [P.sb(ctx, f"mo{r}", [128, D], F32) for r in range(2)]
        junk = P.sb(ctx, "junk", [128, D], BF16)
        ss = [P.sb(ctx, f"ss{r}", [128, 1], F32) for r in range(2)]
        lnv = P.sb(ctx, "lnv", [128, 1], F32)
        rstd = [P.sb(ctx, f"rstd{r}", [128, 1], F32) for r in range(2)]
        ps = [P.ps(ctx, f"pso{r}", [128, TB]) for r in range(4)]
        blocks = [(s, tb) for s in range(SPC) for tb in range(NTB)]

        def loads(bi):
            s, tb = blocks[bi]
            r = bi % 2
            P.dma("sp", mt[r][:, :, :], mixT[s, :, tb * TB:(tb + 1) * TB].rearrange("(c p) t -> p c t", p=128), writes=[mt[r]])
            for j in range(4):
                t0 = tb * TB + j * 128
                P.dma("sp", ht[r][j][:, :], h_in[s, t0:t0 + 128, :], writes=[ht[r][j]])

        loads(0)
        k = 0
        for bi, (s, tb) in enumerate(blocks):
            r = bi % 2
            if bi + 1 < len(blocks):
                loads(bi + 1)
            for j in range(4):
                t0 = tb * TB + j * 128
                kk = k % 2
                k += 1
                for half in range(2):
                    pp = ps[2 * kk + half]
                    for c in range(kc):
                        P.op("pe", lambda e: e.matmul(pp[:, :], lhsT=mt[r][:, c, j * 128:(j + 1) * 128],
                                                      rhs=wout[:, c, half * 512:(half + 1) * 512], start=(c == 0), stop=(c == kc - 1)),
                             reads=[mt[r], wout], writes=[pp])
                    P.op("act", lambda e: e.copy(out=mo[kk][:, half * 512:(half + 1) * 512], in_=pp[:, :]), reads=[pp], writes=[mo[kk]])
                P.op("act", lambda e: e.activation(out=junk[:, :], in_=mo[kk][:, :], func=AF.Square, accum_out=ss[kk][:, 0:1]),
                     reads=[mo[kk]], writes=[junk, ss[kk]])
                rstd_from_ss(P, ss[kk], lnv, rstd[kk], 1, D)
                P.op("dve", lambda e: e.scalar_tensor_tensor(out=mo[kk][:, :], in0=mo[kk][:, :], scalar=rstd[kk][:, 0:1], in1=gbc[:, :],
                                                             op0=ALU.mult, op1=ALU.mult), reads=[mo[kk], rstd[kk], gbc], writes=[mo[kk]])
                P.op("pool", lambda e: e.tensor_tensor(out=ht[r][j][:, :], in0=ht[r][j][:, :], in1=mo[kk][:, :], op=ALU.add),
                     reads=[ht[r][j], mo[kk]], writes=[ht[r][j]])
                P.dma("sp", h_out[s, t0:t0 + 128, :], ht[r][j][:, :], reads=[ht[r][j]])
      P.barrier()
      phase_ffn(P, nc, *ffn_args, pre=pre)


FB = 256


def phase_ffn(P, nc, wg_ap, wu_ap, wd_ap, gpre_ap, gpost_ap, h_in, h_out, pre=None):
    with ExitStack() as ctx:
        wg, wu = pre["wg"], pre["wu"]
        wd = load_weight_bf16(P, ctx, "wd", wd_ap, DFF, D)
        gpre = P.sb(ctx, "gpre", [128, D], F32)
        gpost = P.sb(ctx, "gpost", [128, D], F32)
        P.dma("sp", gpre[:, :], gpre_ap.partition_broadcast(128), writes=[gpre])
        P.dma("sp", gpost[:, :], gpost_ap.partition_broadcast(128), writes=[gpost])
        ht = [[P.sb(ctx, f"ht{r}_{j}", [128, D], F32) for j in range(2)] for r in range(2)]
        hn = [P.sb(ctx, f"hn{j}", [128, D], BF16) for j in range(2)]
        hnT = P.sb(ctx, "hnT", [128, 8, FB], BF16)
        actT = P.sb(ctx, "actT", [128, NFT, FB], BF16)
        sg = [P.sb(ctx, f"sg{r}", [128, FB], F32) for r in range(2)]
        mo = [P.sb(ctx, f"mo{r}", [128, D], F32) for r in range(2)]
        junk = P.sb(ctx, "junk", [128, D], BF16)
        ss = [P.sb(ctx, f"ss{r}", [128, 2], F32) for r in range(2)]
        lnv = P.sb(ctx, "lnv", [128, 2], F32)
        rstd = [P.sb(ctx, f"rstd{r}", [128, 2], F32) for r in range(2)]
        ss2 = [P.sb(ctx, f"ss2{r}", [128, 1], F32) for r in range(2)]
        rstd2 = [P.sb(ctx, f"rstd2{r}", [128, 1], F32) for r in range(2)]
        tp = [P.ps(ctx, f"tp{r}", [128, FB], BF16) for r in range(2)]
        psg = [P.ps(ctx, f"psg{r}", [128, FB]) for r in range(2)]
        psu = [P.ps(ctx, f"psu{r}", [128, FB]) for r in range(2)]
        psd = [P.ps(ctx, f"psd{r}", [128, TB]) for r in range(2)]
        nb = S // FB
        blocks = [(s, tb) for s in range(SPC) for tb in range(nb)]

        def loads(bi):
            s, tb = blocks[bi]
            r = bi % 2
            for j in range(2):
                t0 = tb * FB + j * 128
                P.dma("sp", ht[r][j][:, :], h_in[s, t0:t0 + 128, :], writes=[ht[r][j]])

        loads(0)
        k = 0
        for bi, (s, tb) in enumerate(blocks):
            r = bi % 2
            if bi + 1 < len(blocks):
                loads(bi + 1)
            for j in range(2):
                P.op("act", lambda e: e.activation(out=junk[:, :], in_=ht[r][j][:, :], func=AF.Square, accum_out=ss[r][:, j:j + 1]),
                     reads=[ht[r][j]], writes=[junk, ss[r]])
            rstd_from_ss(P, ss[r], lnv, rstd[r], 2, D)
            for j in range(2):
                P.op("dve", lambda e: e.scalar_tensor_tensor(out=hn[j][:, :], in0=ht[r][j][:, :], scalar=rstd[r][:, j:j + 1], in1=gpre[:, :],
                                                             op0=ALU.mult, op1=ALU.mult), reads=[ht[r][j], rstd[r], gpre], writes=[hn[j]])
            for c in range(8):
                tpc = tp[c % 2]
                for j in range(2):
                    P.op("pe", lambda e: e.transpose(out=tpc[:, j * 128:(j + 1) * 128], in_=hn[j][:, c * 128:(c + 1) * 128],
                                                     identity=P.ident[:, :]), reads=[hn[j], P.ident], writes=[tpc])
                P.op("dve", lambda e: e.tensor_copy(out=hnT[:, c, :], in_=tpc[:, :]), reads=[tpc], writes=[hnT])
            for f in range(NFT):
                fr = f % 2
                for c in range(8):
                    P.op("pe", lambda e: e.matmul(psg[fr][:, :], lhsT=wg[:, c, f * 128:(f + 1) * 128], rhs=hnT[:, c, :],
                                                  start=(c == 0), stop=(c == 7)), reads=[wg, hnT], writes=[psg[fr]])
                for c in range(8):
                    P.op("pe", lambda e: e.matmul(psu[fr][:, :], lhsT=wu[:, c, f * 128:(f + 1) * 128], rhs=hnT[:, c, :],
                                                  start=(c == 0), stop=(c == 7)), reads=[wu, hnT], writes=[psu[fr]])
                P.op("act", lambda e: e.activation(out=sg[fr][:, :], in_=psg[fr][:, :], func=AF.Silu), reads=[psg[fr]], writes=[sg[fr]])
                P.op("dve", lambda e: e.tensor_tensor(out=actT[:, f, :], in0=psu[fr][:, :], in1=sg[fr][:, :], op=ALU.mult),
                     reads=[psu[fr], sg[fr]], writes=[actT])
            for j in range(2):
                t0 = tb * FB + j * 128
                kk = k % 2
                k += 1
                for half in range(2):
                    pp = psd[half]
                    for f in range(NFT):
                        P.op("pe", lambda e: e.matmul(pp[:, :], lhsT=actT[:, f, j * 128:(j + 1) * 128],
                                                      rhs=wd[:, f, half * 512:(half + 1) * 512], start=(f == 0), stop=(f == NFT - 1)),
                             reads=[actT, wd], writes=[pp])
                    P.op("act", lambda e: e.copy(out=mo[kk][:, half * 512:(half + 1) * 512], in_=pp[:, :]), reads=[pp], writes=[mo[kk]])
                P.op("act", lambda e: e.activation(out=junk[:, :], in_=mo[kk][:, :], func=AF.Square, accum_out=ss2[kk][:, 0:1]),
                     reads=[mo[kk]], writes=[junk, ss2[kk]])
                rstd_from_ss(P, ss2[kk], lnv, rstd2[kk], 1, D)
                P.op("dve", lambda e: e.scalar_tensor_tensor(out=mo[kk][:, :], in0=mo[kk][:, :], scalar=rstd2[kk][:, 0:1], in1=gpost[:, :],
                                                             op0=ALU.mult, op1=ALU.mult), reads=[mo[kk], rstd2[kk], gpost], writes=[mo[kk]])
                P.op("pool", lambda e: e.tensor_tensor(out=ht[r][j][:, :], in0=ht[r][j][:, :], in1=mo[kk][:, :], op=ALU.add),
                     reads=[ht[r][j], mo[kk]], writes=[ht[r][j]])
                P.dma("sp", h_out[s, t0:t0 + 128, :], ht[r][j][:, :], reads=[ht[r][j]])
    P.barrier()


NIT = 12
TOPK = 256


def phase_dsa(P, nc, T):
    fm, tmv, tmw, mixT = T["fm0"], T["tmv0"], T["tmw0"], T["mixT0"]
    dbg = T.get("dbg", {})
    n_s, n_qb, stages = dbg.get("n_s", SPC), dbg.get("n_qb", NTB), dbg.get("stages", "idx,bis,tr,att")
    with ExitStack() as ctx:
        dmask = P.sb(ctx, "dmask", [128, 4, TB], F32)
        P.dma("sp", dmask[:, :, :], T["dmask"].rearrange("a p k -> p a k"), writes=[dmask])
        pow2 = P.sb(ctx, "pow2", [128, NIT], F32)
        P.dma("sp", pow2[:, :], T["pow2"].partition_broadcast(128), writes=[pow2])
        bkT = P.sb(ctx, "bkT", [128, S], BF16)
        ikT = P.sb(ctx, "ikT", [128, S], BF16)
        bv1 = P.sb(ctx, "bv1", [128, 32, 129], BF16)
        P.op("pool", lambda e: e.memset(bv1[:, :, 128:129], 1.0), writes=[bv1])
        iw = P.sb(ctx, "iw", [128, 32, 8], F32)
        iqT = [[P.sb(ctx, f"iqT{r}_{i}", [128, TB], BF16) for i in range(4)] for r in range(2)]
        bqT = [[P.sb(ctx, f"bqT{r}_{i}", [128, TB], BF16) for i in range(4)] for r in range(2)]
        score = [P.sb(ctx, f"score{r}", [128, S], F32) for r in range(2)]
        mask = [P.sb(ctx, f"mask{i}", [128, S], BF16) for i in range(4)]
        maskT = P.sb(ctx, "maskT", [128, 32, TB], BF16)
        junk = P.sb(ctx, "junk", [128, S], BF16)
        rl = [P.sb(ctx, f"rl{r}", [128, TB], F32) for r in range(4)]
        sm = {n: P.sb(ctx, "sm_" + n, [128, 1], F32) for n in ("hi", "lo", "rng", "th", "cand", "cnt", "m")}
        steps = P.sb(ctx, "steps", [128, NIT], F32)
        A = {"st": [P.ps(ctx, f"st{r}", [128, TB]) for r in range(2)],
             "pt": [P.sb(ctx, f"pt{r}", [128, TB], BF16) for r in range(3)], "i": 0}
        accs = [[P.ps(ctx, f"acc{m}_{r}", [128, 2, 256]) for r in range(2)] for m in range(2)]
        tpx = P.ps(ctx, "tpx", [128, TB], BF16)
        ist = A["st"] + [P.ps(ctx, "st_x", [128, TB])]
        ii = 0
        rc = P.sb(ctx, "rc", [128, 4], F32)
        ob = [P.sb(ctx, f"ob{r}", [128, 4, 128], BF16) for r in range(2)]
        oT = [P.sb(ctx, f"oT{r}", [128, TB], BF16) for r in range(2)]
        bi = 0
        hcnt = 0
        sc_i = 0
        for s in range(n_s):
            P.dma("sp", bkT[0:64, :], fm[s, 16, 0:64, :], writes=[bkT])
            P.dma("sp", bkT[64:128, :], fm[s, 17, 0:64, :], writes=[bkT])
            for rep in range(2):
                P.dma("sp", ikT[64 * rep:64 * rep + 32, :], fm[s, 16, 64:96, :], writes=[ikT])
                P.dma("sp", ikT[64 * rep + 32:64 * rep + 64, :], fm[s, 17, 64:96, :], writes=[ikT])
            for half in range(2):
                P.dma("sp", bv1[:, 16 * half:16 * half + 16, 0:128],
                      tmv[s, 2048 * half:2048 * (half + 1), 512:640].rearrange("(kt p) e -> p kt e", p=128), writes=[bv1])
            P.dma("sp", iw[:, :, :], tmw[s, :, :, :], writes=[iw])
            for qb in range(n_qb):
                r = bi % 2
                bi += 1
                c0, c1 = qb * TB, (qb + 1) * TB
                for i in range(4):
                    p, hl0 = (2 * i) // 4, (2 * i) % 4
                    for k2 in range(2):
                        hl = hl0 + k2
                        P.dma("sp", iqT[r][i][64 * k2:64 * k2 + 32, :], fm[s, 8 + 2 * p, 32 * hl:32 * hl + 32, c0:c1], writes=[iqT[r][i]])
                        P.dma("sp", iqT[r][i][64 * k2 + 32:64 * k2 + 64, :], fm[s, 9 + 2 * p, 32 * hl:32 * hl + 32, c0:c1], writes=[iqT[r][i]])
                for h in range(4):
                    p, hl = h // 2, h % 2
                    P.dma("sp", bqT[r][h][0:64, :], fm[s, 12 + 2 * p, 64 * hl:64 * hl + 64, c0:c1], writes=[bqT[r][h]])
                    P.dma("sp", bqT[r][h][64:128, :], fm[s, 13 + 2 * p, 64 * hl:64 * hl + 64, c0:c1], writes=[bqT[r][h]])
                for qt in range(4):
                    gq = 4 * qb + qt
                    n = 128 * (gq + 1)
                    sc = score[sc_i % 2]
                    sc_i += 1
                    for kc in range(qb + 1 if "idx" in stages else 0):
                        for h in range(8):
                            st = ist[ii % 3]
                            rlb = rl[ii % 4]
                            ii += 1
                            ro = 64 * (h % 2)
                            P.op("pe", lambda e: e.matmul(st[:, :], lhsT=iqT[r][h // 2][ro:ro + 64, qt * 128:(qt + 1) * 128],
                                                          rhs=ikT[ro:ro + 64, kc * TB:(kc + 1) * TB], start=True, stop=True),
                                 reads=[iqT[r][h // 2], ikT], writes=[st])
                            P.op("act", lambda e: e.activation(out=rlb[:, :], in_=st[:, :], func=AF.Relu), reads=[st], writes=[rlb])
                            if h == 0:
                                P.op("dve", lambda e: e.tensor_scalar(out=sc[:, kc * TB:(kc + 1) * TB], in0=rlb[:, :], scalar1=iw[:, gq, 0:1],
                                                                      scalar2=None, op0=ALU.mult), reads=[rlb, iw], writes=[sc])
                            else:
                                P.op("dve", lambda e: e.scalar_tensor_tensor(out=sc[:, kc * TB:(kc + 1) * TB], in0=rlb[:, :],
                                                                             scalar=iw[:, gq, h:h + 1], in1=sc[:, kc * TB:(kc + 1) * TB],
                                                                             op0=ALU.mult, op1=ALU.add), reads=[rlb, iw, sc], writes=[sc])
                    P.op("pool", lambda e: e.tensor_tensor(out=sc[:, c0:c1], in0=sc[:, c0:c1], in1=dmask[:, qt, :], op=ALU.add),
                         reads=[sc, dmask], writes=[sc])
                    th = sm["th"]
                    if gq < 2 or "bis" not in stages:
                        P.op("dve", lambda e: e.memset(th[:, :], NEG / 2), writes=[th])
                    else:
                        P.op("dve", lambda e: e.tensor_reduce(out=sm["hi"][:, :], in_=sc[:, 0:n], axis=AX.X, op=ALU.max), reads=[sc], writes=[sm["hi"]])
                        P.op("dve", lambda e: e.tensor_reduce(out=th[:, :], in_=sc[:, 0:128 * gq], axis=AX.X, op=ALU.min), reads=[sc], writes=[th])
                        P.op("dve", lambda e: e.tensor_tensor(out=sm["rng"][:, :], in0=sm["hi"][:, :], in1=th[:, :], op=ALU.subtract),
                             reads=[sm["hi"], th], writes=[sm["rng"]])
                        P.op("dve", lambda e: e.tensor_scalar(out=steps[:, :], in0=pow2[:, :], scalar1=sm["rng"][:, 0:1], scalar2=None, op0=ALU.mult),
                             reads=[pow2, sm["rng"]], writes=[steps])
                        P.op("dve", lambda e: e.tensor_tensor(out=sm["cand"][:, :], in0=th[:, :], in1=steps[:, 0:1], op=ALU.add),
                             reads=[th, steps], writes=[sm["cand"]])
                        for j in range(NIT):
                            P.op("dve", lambda e: e.tensor_scalar(out=junk[:, 0:n], in0=sc[:, 0:n], scalar1=sm["cand"][:, 0:1], scalar2=None,
                                                                  op0=ALU.is_ge, op1=ALU.add, accum_out=sm["cnt"][:, 0:1]),
                                 reads=[sc, sm["cand"]], writes=[junk, sm["cnt"]])
                            P.op("dve", lambda e: e.tensor_scalar(out=sm["m"][:, :], in0=sm["cnt"][:, :], scalar1=float(TOPK), scalar2=-0.5,
                                                                  op0=ALU.is_ge, op1=ALU.add), reads=[sm["cnt"]], writes=[sm["m"]])
                            P.op("dve", lambda e: e.scalar_tensor_tensor(out=sm["cand"][:, :], in0=sm["m"][:, :], scalar=steps[:, j:j + 1],
                                                                         in1=sm["cand"][:, :], op0=ALU.mult, op1=ALU.add),
                                 reads=[sm["m"], steps, sm["cand"]], writes=[sm["cand"]])
                        P.op("dve", lambda e: e.scalar_tensor_tensor(out=th[:, :], in0=sm["rng"][:, :], scalar=-(0.5 ** (NIT + 1)),
                                                                     in1=sm["cand"][:, :], op0=ALU.mult, op1=ALU.add),
                             reads=[sm["rng"], sm["cand"]], writes=[th])
                    P.op("dve", lambda e: e.tensor_scalar(out=mask[qt][:, 0:n], in0=sc[:, 0:n], scalar1=th[:, 0:1], scalar2=None, op0=ALU.is_ge),
                         reads=[sc, th], writes=[mask[qt]])
                for kt in range(4 * qb + 4 if "tr" in stages else 0):
                    for qt in range(4):
                        if kt <= 4 * qb + qt:
                            P.op("pe", lambda e: e.transpose(out=tpx[:, qt * 128:(qt + 1) * 128], in_=mask[qt][:, kt * 128:(kt + 1) * 128],
                                                             identity=P.ident[:, :]), reads=[mask[qt], P.ident], writes=[tpx])
                    P.op("act", lambda e: e.copy(out=maskT[:, kt, :], in_=tpx[:, :]), reads=[tpx], writes=[maskT])
                for h in range(4 if "att" in stages else 0):
                    A["acc"] = accs[hcnt % 2]
                    orr = hcnt % 2
                    hcnt += 1
                    attn_core(P, A, bqT[r][h][:, :], lambda kt: bkT[:, kt * 128:(kt + 1) * 128], lambda kt: bv1[:, kt, :],
                              qb, 128, 128 ** -0.5, [bqT[r][h], bkT, bv1], mask_of=lambda kt: (maskT[:, kt, :], maskT))
                    for qt in range(4):
                        acc = A["acc"][qt // 2]
                        P.op("dve", lambda e: e.reciprocal(out=rc[:, qt:qt + 1], in_=acc[:, qt % 2, 128:129]), reads=[acc], writes=[rc])
                        P.op("dve", lambda e: e.tensor_scalar(out=ob[orr][:, qt, :], in0=acc[:, qt % 2, 0:128], scalar1=rc[:, qt:qt + 1],
                                                              scalar2=None, op0=ALU.mult), reads=[acc, rc], writes=[ob[orr]])
                        P.op("pe", lambda e: e.transpose(out=tpx[:, qt * 128:(qt + 1) * 128], in_=ob[orr][:, qt, :], identity=P.ident[:, :]),
                             reads=[ob[orr], P.ident], writes=[tpx])
                    P.op("act", lambda e: e.copy(out=oT[orr][:, :], in_=tpx[:, :]), reads=[tpx], writes=[oT[orr]])
                    P.dma("sp", mixT[s, 512 + h * 128:512 + (h + 1) * 128, c0:c1], oT[orr][:, :], reads=[oT[orr]])
    P.barrier()


O_Z, O_XBC, O_DT, O_MQ, O_MK, O_MV = 0, 1024, 2560, 2576, 3088, 3600


def l1_layout():
    fm_cols = list(range(O_XBC, O_XBC + 1536))
    types = [None] * 12
    for base in (O_MQ, O_MK):
        for p in range(2):
            A, B = pair_cols_h64(base, p)
            fm_cols += A + B
            types.append(0)
    tm_cols = list(range(O_Z, O_Z + 1024)) + list(range(O_MV, O_MV + 512)) + list(range(O_DT, O_DT + 16))
    return np.array(fm_cols), types, np.array(tm_cols)


def make_l1_cfg(T):
    def alloc(P, ctx):
        ex = {}
        ex["xb"] = [P.sb(ctx, f"xb{f}", [128, 3 + TB], F32) for f in range(12)]
        ex["cacc"] = [P.sb(ctx, f"cacc{r}", [128, TB], F32) for r in range(2)]
        ex["co"] = [P.sb(ctx, f"co{r}", [128, TB], BF16) for r in range(2)]
        ex["convw"] = P.sb(ctx, "convw", [128, 12, 4], F32)
        ex["convb"] = P.sb(ctx, "convb", [128, 12], F32)
        P.dma("sp", ex["convw"][:, :, :], T["convw"].rearrange("(f p) j -> p f j", p=128), writes=[ex["convw"]])
        P.dma("sp", ex["convb"][:, :], T["convb"], writes=[ex["convb"]])
        ex["fa"] = [P.sb(ctx, f"fa{r}", [128, TB], F32) for r in range(2)]
        ex["fb"] = [P.sb(ctx, f"fb{r}", [128, TB], F32) for r in range(2)]
        ex["km"] = [P.sb(ctx, f"km{r}", [128, 2, 2], F32) for r in range(2)]
        ex["tmo"] = [P.sb(ctx, f"tmo{r}", [128, 1536], BF16) for r in range(2)]
        ex["tmw"] = [P.sb(ctx, f"tmw{r}", [128, 16], F32) for r in range(2)]
        ex["cnt"] = 0
        ex["c2"] = 0
        ex["c3"] = 0
        return ex

    def plain(P, ex, s, tb, ft, ps):
        xb = ex["xb"][ft]
        k = ex["c2"] % 2
        ex["c2"] += 1
        acc, co = ex["cacc"][k], ex["co"][k]
        cw = ex["convw"]
        if tb == 0:
            P.op("pool", lambda e: e.memset(xb[:, 0:3], 0.0), writes=[xb])
        P.op("act", lambda e: e.copy(out=xb[:, 3:3 + TB], in_=ps[:, :]), reads=[ps], writes=[xb])
        P.op("dve", lambda e: e.tensor_scalar(out=acc[:, :], in0=xb[:, 0:TB], scalar1=cw[:, ft, 0:1], scalar2=None, op0=ALU.mult),
             reads=[xb, cw], writes=[acc])
        for j in range(1, 4):
            P.op("dve", lambda e: e.scalar_tensor_tensor(out=acc[:, :], in0=xb[:, j:j + TB], scalar=cw[:, ft, j:j + 1], in1=acc[:, :],
                                                         op0=ALU.mult, op1=ALU.add), reads=[xb, cw, acc], writes=[acc])
        P.op("act", lambda e: e.activation(out=co[:, :], in_=acc[:, :], func=AF.Silu, bias=ex["convb"][:, ft:ft + 1]),
             reads=[acc, ex["convb"]], writes=[co])
        P.op("pool", lambda e: e.tensor_copy(out=xb[:, 0:3], in_=xb[:, TB:TB + 3]), reads=[xb], writes=[xb])
        P.dma("sp", T["xbcT"][s, ft, :, tb * TB:(tb + 1) * TB], co[:, :], reads=[co])

    def rope_f32(pi):
        return True

    def rope_handler(P, ex, s, tb, pi, ft, t1, t2, t3, t4, oa, ob):
        k = ex["c3"] % 2
        ex["c3"] += 1
        fa, fb = ex["fa"][k], ex["fb"][k]
        P.op("pool", lambda e: e.tensor_tensor(out=fa[:, :], in0=t1[:, :], in1=t2[:, :], op=ALU.subtract), reads=[t1, t2], writes=[fa])
        P.op("pool", lambda e: e.tensor_tensor(out=fb[:, :], in0=t3[:, :], in1=t4[:, :], op=ALU.add), reads=[t3, t4], writes=[fb])
        P.op("act", lambda e: e.copy(out=oa[:, :], in_=fa[:, :]), reads=[fa], writes=[oa])
        P.op("act", lambda e: e.copy(out=ob[:, :], in_=fb[:, :]), reads=[fb], writes=[ob])
        pr = pi - 12
        if pr < 2:
            P.dma("sp", T["mqf"][s, 2 * pr, :, tb * TB:(tb + 1) * TB], fa[:, :], reads=[fa])
            P.dma("sp", T["mqf"][s, 2 * pr + 1, :, tb * TB:(tb + 1) * TB], fb[:, :], reads=[fb])
        else:
            km = ex["km"][k]
            P.op("dve", lambda e: e.tensor_reduce(out=km[:, 0, :], in_=fa[:, :].rearrange("p (b t) -> p b t", b=2), axis=AX.X, op=ALU.add),
                 reads=[fa], writes=[km])
            P.op("dve", lambda e: e.tensor_reduce(out=km[:, 1, :], in_=fb[:, :].rearrange("p (b t) -> p b t", b=2), axis=AX.X, op=ALU.add),
                 reads=[fb, km], writes=[km])
            P.op("dve", lambda e: e.tensor_scalar(out=km[:, :, :], in0=km[:, :, :], scalar1=1.0 / 256, scalar2=None, op0=ALU.mult),
                 reads=[km], writes=[km])
            q = pr - 2
            P.dma("sp", T["kmean"][s, 2 * q, :, 2 * tb:2 * tb + 2], km[:, 0, :], reads=[km])
            P.dma("sp", T["kmean"][s, 2 * q + 1, :, 2 * tb:2 * tb + 2], km[:, 1, :], reads=[km])

    def tm_handler(P, ex, s, tb, r, hnT, wtm, pst):
        for j in range(4):
            t0 = tb * TB + j * 128
            k = ex["cnt"] % 2
            ex["cnt"] += 1
            o, w = ex["tmo"][k], ex["tmw"][k]
            for grp in range(4):
                n0 = grp * 512
                nw = 512 if grp < 3 else 16
                pp = pst[grp % 2]
                for c in range(8):
                    P.op("pe", lambda e: e.matmul(pp[:, 0:nw], lhsT=hnT[:, c, j * 128:(j + 1) * 128], rhs=wtm[:, c, n0:n0 + nw],
                                                  start=(c == 0), stop=(c == 7)), reads=[hnT, wtm], writes=[pp])
                if grp < 3:
                    P.op("act", lambda e: e.copy(out=o[:, n0:n0 + 512], in_=pp[:, :]), reads=[pp], writes=[o])
                else:
                    P.op("act", lambda e: e.copy(out=w[:, :], in_=pp[:, 0:16]), reads=[pp], writes=[w])
            P.dma("sp", T["tm1"][s, t0:t0 + 128, :], o[:, :], reads=[o])
            P.dma("sp", T["dtraw"][s, :, t0 // 128, :], w[:, :], reads=[w])

    _, types1, _ = l1_layout()
    return dict(x=T["h2"], g=T["norms"][4], wfm=T["wfm1"], wtm=T["wtm1"], nfm=20, ntm=1552, types=types1, rope=T["rope"],
                fmT=T["fm1"], alloc=alloc, tm_handler=tm_handler, plain_handler=plain, rope_f32=rope_f32, rope_f32_handler=rope_handler)


BIGQ = 240000.0


def phase_moba_gate(P, nc, T):
    with ExitStack() as ctx:
        pm = P.sb(ctx, "pm", [128, 16, 16], F32)
        oh = P.sb(ctx, "oh", [128, 16, 16], F32)
        P.dma("sp", pm[:, :, :], T["pm"].partition_broadcast(128).rearrange("p (a b) -> p a b", a=16), writes=[pm])
        P.dma("sp", oh[:, :, :], T["oh"].partition_broadcast(128).rearrange("p (a b) -> p a b", a=16), writes=[oh])
        kmbd = P.sb(ctx, "kmbd", [128, 4, 64], F32)
        qf = [P.sb(ctx, f"qf{r}", [128, 4, TB], F32) for r in range(2)]
        g2 = P.sb(ctx, "g2", [128, 8, 16], F32)
        mx = P.sb(ctx, "mx", [128, 8, 8], F32)
        sel = P.sb(ctx, "sel", [128, 8, 16], F32)
        negm = [P.sb(ctx, f"negm{r}", [128, 128], BF16) for r in range(2)]
        nT = [P.sb(ctx, f"nT{r}", [128, TB], BF16) for r in range(2)]
        gps = [P.ps(ctx, f"gps{r}", [128, 128]) for r in range(2)]
        tpx = P.ps(ctx, "tpx", [128, TB], BF16)
        bi = 0
        k = 0
        for s in range(SPC):
            P.op("dve", lambda e: e.memset(kmbd[:, :, :], 0.0), writes=[kmbd])
            for f in range(4):
                for hl in range(4):
                    P.dma("sp", kmbd[32 * hl:32 * hl + 32, f, 16 * hl:16 * hl + 16], T["kmean"][s, f, 32 * hl:32 * hl + 32, :], writes=[kmbd])
            for tb in range(NTB):
                r = bi % 2
                bi += 1
                P.dma("sp", qf[r][:, :, :], T["mqf"][s, :, :, tb * TB:(tb + 1) * TB].rearrange("f p t -> p f t"), writes=[qf[r]])
                for j in range(4):
                    own = (tb * 4 + j) // 2
                    kk = k % 2
                    k += 1
                    gp = gps[kk]
                    for p in range(2):
                        for ab in range(2):
                            P.op("pe", lambda e: e.matmul(gp[:, p * 64:(p + 1) * 64], lhsT=qf[r][:, 2 * p + ab, j * 128:(j + 1) * 128],
                                                          rhs=kmbd[:, 2 * p + ab, :], start=(ab == 0), stop=(ab == 1)),
                                 reads=[qf[r], kmbd], writes=[gp])
                    P.op("dve", lambda e: e.tensor_tensor(out=g2[:, :, :], in0=gp[:, :].rearrange("p (h n) -> p h n", h=8),
                                                          in1=pm[:, own, :].unsqueeze(1).to_broadcast([128, 8, 16]), op=ALU.add),
                         reads=[gp, pm], writes=[g2])
                    for h in range(8):
                        P.op("dve", lambda e: e.max(out=mx[:, h, :], in_=g2[:, h, :]), reads=[g2], writes=[mx])
                    P.op("dve", lambda e: e.tensor_tensor(out=sel[:, :, :], in0=g2[:, :, :], in1=mx[:, :, 2:3].to_broadcast([128, 8, 16]),
                                                          op=ALU.is_ge), reads=[g2, mx], writes=[sel])
                    P.op("dve", lambda e: e.tensor_tensor(out=sel[:, :, :], in0=sel[:, :, :],
                                                          in1=oh[:, own, :].unsqueeze(1).to_broadcast([128, 8, 16]), op=ALU.max),
                         reads=[sel, oh], writes=[sel])
                    P.op("dve", lambda e: e.tensor_scalar(out=negm[kk][:, :].rearrange("p (h n) -> p h n", h=8), in0=sel[:, :, :],
                                                          scalar1=-1.0, scalar2=BIGQ, op0=ALU.add, op1=ALU.mult), reads=[sel], writes=[negm[kk]])
                    P.op("pe", lambda e: e.transpose(out=tpx[:, j * 128:(j + 1) * 128], in_=negm[kk][:, :], identity=P.ident[:, :]),
                         reads=[negm[kk], P.ident], writes=[tpx])
                P.op("act", lambda e: e.copy(out=nT[r][:, :], in_=tpx[:, :]), reads=[tpx], writes=[nT[r]])
                P.dma("sp", T["negT"][s, :, tb * TB:(tb + 1) * TB], nT[r][:, :], reads=[nT[r]])
    P.barrier()


def phase_moba_attn(P, nc, T):
    fm, tm1, mixT = T["fm1"], T["tm1"], T["mixT1"]
    with ExitStack() as ctx:
        tri = P.sb(ctx, "tri", [128, 128], BF16)
        P.dma("sp", tri[:, :], T["tri"], writes=[tri])
        ka = [P.sb(ctx, f"ka{r}", [80, S], BF16) for r in range(4)]
        v1 = [P.sb(ctx, f"v1{r}", [128, 32, 65], BF16) for r in range(4)]
        for r in range(4):
            P.op("pool", lambda e: e.memset(v1[r][:, :, 64:65], 1.0), writes=[v1[r]])
            P.dma("sp", ka[r][64:80, :], T["blk1h"], writes=[ka[r]])
        qa = [P.sb(ctx, f"qa{r}", [80, TB], BF16) for r in range(4)]
        A = {"st": [P.ps(ctx, f"st{r}", [128, TB]) for r in range(2)],
             "pt": [P.sb(ctx, f"pt{r}", [128, TB], BF16) for r in range(3)], "i": 0}
        accs = [[P.ps(ctx, f"acc{m}_{r}", [128, 2, 256]) for r in range(2)] for m in range(2)]
        tpx = P.ps(ctx, "tpx", [128, TB], BF16)
        rc = P.sb(ctx, "rc", [128, 4], F32)
        ob = [P.sb(ctx, f"ob{r}", [128, 4, 128], BF16) for r in range(2)]
        oT = [P.sb(ctx, f"oT{r}", [128, TB], BF16) for r in range(2)]
        hpi = 0
        qi = 0
        for s in range(SPC):
            for hp in range(4):
                kb = 2 * (hpi % 2)
                hpi += 1
                for hh in range(2):
                    h = 2 * hp + hh
                    p, hl = h // 4, h % 4
                    P.dma("sp", ka[kb + hh][0:32, :], fm[s, 16 + 2 * p, 32 * hl:32 * hl + 32, :], writes=[ka[kb + hh]])
                    P.dma("sp", ka[kb + hh][32:64, :], fm[s, 17 + 2 * p, 32 * hl:32 * hl + 32, :], writes=[ka[kb + hh]])
                    for half in range(2):
                        P.dma("sp", v1[kb + hh][:, 16 * half:16 * half + 16, 0:64],
                              tm1[s, 2048 * half:2048 * (half + 1), 1024 + h * 64:1024 + (h + 1) * 64].rearrange("(kt p) e -> p kt e", p=128),
                              writes=[v1[kb + hh]])
                for qb in range(NTB):
                    qr = 2 * (qi % 2)
                    orr = qi % 2
                    qi += 1
                    c0, c1 = qb * TB, (qb + 1) * TB
                    for hh in range(2):
                        h = 2 * hp + hh
                        p, hl = h // 4, h % 4
                        q = qa[qr + hh]
                        P.dma("sp", q[0:32, :], fm[s, 12 + 2 * p, 32 * hl:32 * hl + 32, c0:c1], writes=[q])
                        P.dma("sp", q[32:64, :], fm[s, 13 + 2 * p, 32 * hl:32 * hl + 32, c0:c1], writes=[q])
                        P.dma("sp", q[64:80, :], T["negT"][s, h * 16:(h + 1) * 16, c0:c1], writes=[q])
                    for hh in range(2):
                        q = qa[qr + hh]
                        kk, vv = ka[kb + hh], v1[kb + hh]
                        A["acc"] = accs[hh]
                        attn_core(P, A, q[0:80, :], lambda kt: kk[0:80, kt * 128:(kt + 1) * 128], lambda kt: vv[:, kt, :],
                                  qb, 64, 0.125, [q, kk, vv], tri=tri)
                        for qt in range(4):
                            acc = A["acc"][qt // 2]
                            P.op("dve", lambda e: e.reciprocal(out=rc[:, qt:qt + 1], in_=acc[:, qt % 2, 64:65]), reads=[acc], writes=[rc])
                            P.op("dve", lambda e: e.tensor_scalar(out=ob[orr][:, qt, hh * 64:(hh + 1) * 64], in0=acc[:, qt % 2, 0:64],
                                                                  scalar1=rc[:, qt:qt + 1], scalar2=None, op0=ALU.mult),
                                 reads=[acc, rc], writes=[ob[orr]])
                    for qt in range(4):
                        P.op("pe", lambda e: e.transpose(out=tpx[:, qt * 128:(qt + 1) * 128], in_=ob[orr][:, qt, :], identity=P.ident[:, :]),
                             reads=[ob[orr], P.ident], writes=[tpx])
                    P.op("act", lambda e: e.copy(out=oT[orr][:, :], in_=tpx[:, :]), reads=[tpx], writes=[oT[orr]])
                    P.dma("sp", mixT[s, 1024 + hp * 128:1024 + (hp + 1) * 128, c0:c1], oT[orr][:, :], reads=[oT[orr]])
    P.barrier()


def v3(ap2d, a):
    return ap2d.rearrange("p (a b) -> p a b", a=a)


def phase_ssd(P, nc, T):
    xbcT, tm1, mixT = T["xbcT"], T["tm1"], T["mixT1"]
    dbg = T.get("dbg", {})
    n_s2, n_ch = dbg.get("n_s", SPC), dbg.get("n_ch", 32)
    upto = dbg.get("upto", 99)
    pre = dbg.get("pre", 99)
    pre3 = dbg.get("pre3", 99)
    with ExitStack() as ctx:
        tri = P.sb(ctx, "tri", [128, 128], BF16)
        P.dma("sp", tri[:, :], T["tri"], writes=[tri])
        ones = P.sb(ctx, "ones", [128, 128], BF16)
        dsp = [P.sb(ctx, f"dsp{i}", [128, 512], BF16) for i in range(3)]
        dres = P.sb(ctx, "dres", [128, 512], F32)
        P.op("dve", lambda e: e.memset(ones[:, :], 1.0), writes=[ones])
        gn = P.sb(ctx, "gn", [128, 1024], F32)
        P.dma("sp", gn[:, :], T["ssm_norm"].partition_broadcast(128), writes=[gn])
        dsk = P.sb(ctx, "dsk", [128, 1024], F32)
        P.dma("sp", dsk[:, :], T["dsk"].partition_broadcast(128), writes=[dsk])
        dtb = P.sb(ctx, "dtb", [128, 16], F32)
        P.dma("sp", dtb[:, :], T["dt_bias"].partition_broadcast(128), writes=[dtb])
        abc_ = P.sb(ctx, "a_bc", [128, 16], F32)
        P.dma("sp", abc_[:, :], T["a_log"].partition_broadcast(128), writes=[abc_])
        P.op("act", lambda e: e.activation(out=abc_[:, :], in_=abc_[:, :], func=AF.Exp), reads=[abc_], writes=[abc_])
        P.op("dve", lambda e: e.tensor_scalar(out=abc_[:, :], in0=abc_[:, :], scalar1=-1.0, scalar2=None, op0=ALU.mult), reads=[abc_], writes=[abc_])
        dt_all = P.sb(ctx, "dt_all", [128, 512], F32)
        da_all = P.sb(ctx, "da_all", [128, 512], F32)
        nac = P.sb(ctx, "nac", [128, 512], F32)
        eac = P.sb(ctx, "eac", [128, 512], F32)
        eal = P.sb(ctx, "eal", [128, 512], F32)
        state = P.sb(ctx, "state", [128, 1024], F32)
        stt = P.sb(ctx, "stt", [128, 1024], F32)
        state_bf = P.sb(ctx, "state_bf", [128, 1024], BF16)
        Rm = [P.sb(ctx, f"Rm{i}", [128, 2048], BF16) for i in range(2)]
        exa = P.sb(ctx, "exa", [128, 2048], F32)
        dec = P.sb(ctx, "dec", [128, 2048], F32)
        cbm = P.sb(ctx, "cbm", [128, 256], BF16)
        scT = P.sb(ctx, "scT", [128, 2048], BF16)
        xT = [P.sb(ctx, f"xT{r}", [128, 8, 128], BF16) for r in range(2)]
        bcT = [P.sb(ctx, f"bcT{r}", [128, 4, 128], BF16) for r in range(2)]
        zt = [P.sb(ctx, f"zt{r}", [128, 1024], BF16) for r in range(2)]
        xtm = P.sb(ctx, "xtm", [128, 1024], BF16)
        xdt = P.sb(ctx, "xdt", [128, 1024], BF16)
        xdtt = P.sb(ctx, "xdtt", [128, 1024], BF16)
        btm = P.sb(ctx, "btm", [128, 256], BF16)
        ytmp = P.sb(ctx, "ytmp", [128, 1024], F32)
        y = P.sb(ctx, "y", [128, 1024], F32)
        t2 = P.sb(ctx, "t2", [128, 1024], F32)
        sz = P.sb(ctx, "sz", [128, 1024], F32)
        yn = P.sb(ctx, "yn", [128, 1024], BF16)
        junk = P.sb(ctx, "junk", [128, 512], BF16)
        ss = P.sb(ctx, "ss", [128, 2], F32)
        lnv = P.sb(ctx, "lnv", [128, 2], F32)
        rstd = P.sb(ctx, "rstd", [128, 2], F32)
        yT = [P.sb(ctx, f"yT{r}", [128, 8, TB], BF16) for r in range(2)]
        abc = P.ps(ctx, "abc", [128, 1024])
        cbp = P.ps(ctx, "cbp", [128, 512])
        tpb = P.ps(ctx, "tpb", [128, 1024], BF16)
        yps = [P.ps(ctx, f"yps{g}", [128, 512]) for g in range(2)]
        yip = [P.ps(ctx, f"yip{g}", [128, 512]) for g in range(2)]
        ci = 0
        for s in range(n_s2):
            if pre >= 1:
                P.dma("sp", dt_all[:, :], T["dtraw"][s].rearrange("p c h -> p (c h)"), writes=[dt_all])
            if pre >= 1:
                P.op("dve", lambda e: e.tensor_tensor(out=v3(dt_all[:, :], 32), in0=v3(dt_all[:, :], 32),
                                                      in1=dtb[:, :].unsqueeze(1).to_broadcast([128, 32, 16]), op=ALU.add), reads=[dt_all, dtb], writes=[dt_all])
            if pre >= 2:
                P.op("act", lambda e: e.activation(out=dt_all[:, :], in_=dt_all[:, :], func=AF.Exp), reads=[dt_all], writes=[dt_all])
            if pre >= 2:
                P.op("act", lambda e: e.activation(out=dt_all[:, :], in_=dt_all[:, :], func=AF.Ln, bias=P.one_t[:, 0:1]), reads=[dt_all], writes=[dt_all])
            if pre >= 2:
                P.op("dve", lambda e: e.tensor_tensor(out=v3(da_all[:, :], 32), in0=v3(dt_all[:, :], 32),
                                                      in1=abc_[:, :].unsqueeze(1).to_broadcast([128, 32, 16]), op=ALU.mult), reads=[dt_all, abc_], writes=[da_all])
            if pre >= 3:
                if pre3 >= 1:
                    P.op("dve", lambda e: e.tensor_copy(out=dsp[0][:, :], in_=da_all[:, :]), reads=[da_all], writes=[dsp[0]])
                if pre3 >= 2:
                    P.op("dve", lambda e: e.tensor_tensor(out=dres[:, :], in0=da_all[:, :], in1=dsp[0][:, :], op=ALU.subtract), reads=[da_all, dsp[0]], writes=[dres])
                if pre3 >= 3:
                    P.op("dve", lambda e: e.tensor_copy(out=dsp[1][:, :], in_=dres[:, :]), reads=[dres], writes=[dsp[1]])
                if pre3 >= 4:
                    P.op("dve", lambda e: e.tensor_tensor(out=dres[:, :], in0=dres[:, :], in1=dsp[1][:, :], op=ALU.subtract), reads=[dres, dsp[1]], writes=[dres])
                if pre3 >= 5:
                    P.op("dve", lambda e: e.tensor_copy(out=dsp[2][:, :], in_=dres[:, :]), reads=[dres], writes=[dsp[2]])
                if pre3 >= 6:
                    for i in range(3):
                        P.op("pe", lambda e: e.matmul(yps[0][:, :], lhsT=tri[:, :], rhs=dsp[i][:, :], start=(i == 0), stop=(i == 2)), reads=[tri, dsp[i]], writes=[yps[0]])
                if pre3 >= 7:
                    P.op("dve", lambda e: e.tensor_scalar(out=nac[:, :], in0=yps[0][:, :], scalar1=-1.0, scalar2=None, op0=ALU.mult), reads=[yps[0]], writes=[nac])
                if pre3 >= 8:
                    P.op("act", lambda e: e.activation(out=eac[:, :], in_=nac[:, :], func=AF.Exp, scale=-1.0), reads=[nac], writes=[eac])
            if pre >= 4:
                for i in range(3):
                    P.op("pe", lambda e: e.matmul(yps[1][:, :], lhsT=ones[:, :], rhs=dsp[i][:, :], start=(i == 0), stop=(i == 2)), reads=[ones, dsp[i]], writes=[yps[1]])
                P.op("dve", lambda e: e.tensor_copy(out=eal[:, :], in_=yps[1][:, :]), reads=[yps[1]], writes=[eal])
                P.op("act", lambda e: e.activation(out=eal[:, :], in_=eal[:, :], func=AF.Exp), reads=[eal], writes=[eal])
            if pre >= 5:
                P.op("dve", lambda e: e.memset(state[:, :], 0.0), writes=[state])
            if pre >= 5:
                P.op("pool", lambda e: e.memset(state_bf[:, :], 0.0), writes=[state_bf])

            def loads(ch, r):
                t0 = ch * 128
                P.dma("sp", xT[r][:, :, :], xbcT[s, 0:8, :, t0:t0 + 128].rearrange("f p t -> p f t"), writes=[xT[r]])
                P.dma("sp", bcT[r][:, :, :], xbcT[s, 8:12, :, t0:t0 + 128].rearrange("f p t -> p f t"), writes=[bcT[r]])
                P.dma("sp", zt[r][:, :], tm1[s, t0:t0 + 128, 0:1024], writes=[zt[r]])

            if pre >= 6:
                loads(0, ci % 2)
            for ch in range(n_ch if pre >= 6 else 0):
                r = ci % 2
                ci += 1
                if ch + 1 < n_ch:
                    loads(ch + 1, ci % 2)
                c16 = slice(ch * 16, ch * 16 + 16)
                if upto < 1:
                    continue
                for g in range(2):
                    P.op("pe", lambda e: e.matmul(cbp[:, g * 128:(g + 1) * 128], lhsT=bcT[r][:, g, :], rhs=bcT[r][:, 2 + g, :], start=True, stop=True),
                         reads=[bcT[r]], writes=[cbp])
                P.op("dve", lambda e: e.tensor_tensor(out=v3(cbm[:, :], 2), in0=v3(cbp[:, 0:256], 2),
                                                      in1=tri[:, :].unsqueeze(1).to_broadcast([128, 2, 128]), op=ALU.mult), reads=[cbp, tri], writes=[cbm])
                if upto < 2:
                    continue
                for i in range(2):
                    P.op("dve", lambda e: e.tensor_tensor(out=v3(Rm[i][:, :], 16), in0=tri[:, :].unsqueeze(1).to_broadcast([128, 16, 128]),
                                                          in1=dsp[i][:, c16].unsqueeze(2).to_broadcast([128, 16, 128]), op=ALU.mult),
                         reads=[tri, dsp[i]], writes=[Rm[i]])
                for g in range(2):
                    for q in range(2):
                        for i in range(2):
                            P.op("pe", lambda e: e.matmul(abc[:, q * 512:(q + 1) * 512], lhsT=ones[:, :],
                                                          rhs=Rm[i][:, g * 1024 + q * 512:g * 1024 + (q + 1) * 512],
                                                          start=(i == 0), stop=(i == 1)), reads=[ones, Rm[i]], writes=[abc])
                    for hl in range(8):
                        h = 8 * g + hl
                        P.op("dve", lambda e: e.tensor_scalar(out=exa[:, h * 128:(h + 1) * 128], in0=abc[:, hl * 128:(hl + 1) * 128],
                                                              scalar1=nac[:, ch * 16 + h:ch * 16 + h + 1], scalar2=None, op0=ALU.add),
                             reads=[abc, nac], writes=[exa])
                if upto < 3:
                    continue
                P.op("dve", lambda e: e.tensor_scalar(out=exa[:, :], in0=exa[:, :], scalar1=0.0, scalar2=None, op0=ALU.min), reads=[exa], writes=[exa])
                P.op("act", lambda e: e.activation(out=dec[:, :], in_=exa[:, :], func=AF.Exp), reads=[exa], writes=[dec])
                if upto < 4:
                    continue
                for g in range(2):
                    P.op("dve", lambda e: e.tensor_tensor(out=v3(scT[:, g * 1024:(g + 1) * 1024], 8), in0=v3(dec[:, g * 1024:(g + 1) * 1024], 8),
                                                          in1=cbm[:, g * 128:(g + 1) * 128].unsqueeze(1).to_broadcast([128, 8, 128]), op=ALU.mult),
                         reads=[dec, cbm], writes=[scT])
                if upto < 5:
                    continue
                for f in range(8):
                    P.op("pe", lambda e: e.transpose(out=tpb[:, f * 128:(f + 1) * 128], in_=xT[r][:, f, :], identity=P.ident[:, :]),
                         reads=[xT[r], P.ident], writes=[tpb])
                P.op("act", lambda e: e.copy(out=xtm[:, :], in_=tpb[:, :]), reads=[tpb], writes=[xtm])
                P.op("dve", lambda e: e.tensor_tensor(out=v3(xdt[:, :], 16), in0=v3(xtm[:, :], 16),
                                                      in1=dt_all[:, c16].unsqueeze(2).to_broadcast([128, 16, 64]), op=ALU.mult),
                     reads=[xtm, dt_all], writes=[xdt])
                if upto < 6:
                    continue
                for h in range(16):
                    P.op("pe", lambda e: e.matmul(yps[h // 8][:, (h % 8) * 64:(h % 8 + 1) * 64], lhsT=scT[:, h * 128:(h + 1) * 128],
                                                  rhs=xdt[:, h * 64:(h + 1) * 64], start=True, stop=True), reads=[scT, xdt], writes=[yps[h // 8]])
                for g in range(2):
                    P.op("pe", lambda e: e.matmul(yip[g][:, :], lhsT=bcT[r][:, 2 + g, :], rhs=state_bf[:, g * 512:(g + 1) * 512], start=True, stop=True),
                         reads=[bcT[r], state_bf], writes=[yip[g]])
                if upto < 7:
                    continue
                for g in range(2):
                    hs = slice(g * 512, (g + 1) * 512)
                    P.op("dve", lambda e: e.tensor_tensor(out=v3(ytmp[:, hs], 8), in0=v3(yip[g][:, :], 8),
                                                          in1=eac[:, ch * 16 + 8 * g:ch * 16 + 8 * g + 8].unsqueeze(2).to_broadcast([128, 8, 64]), op=ALU.mult),
                         reads=[yip[g], eac], writes=[ytmp])
                    P.op("dve", lambda e: e.tensor_tensor(out=y[:, hs], in0=yps[g][:, :], in1=ytmp[:, hs], op=ALU.add), reads=[yps[g], ytmp], writes=[y])
                if upto < 8:
                    continue
                P.op("pool", lambda e: e.tensor_tensor(out=t2[:, :], in0=xtm[:, :], in1=dsk[:, :], op=ALU.mult), reads=[xtm, dsk], writes=[t2])
                P.op("pool", lambda e: e.tensor_tensor(out=y[:, :], in0=y[:, :], in1=t2[:, :], op=ALU.add), reads=[y, t2], writes=[y])
                P.op("act", lambda e: e.activation(out=sz[:, :], in_=zt[r][:, :], func=AF.Silu), reads=[zt[r]], writes=[sz])
                P.op("dve", lambda e: e.tensor_tensor(out=y[:, :], in0=y[:, :], in1=sz[:, :], op=ALU.mult), reads=[y, sz], writes=[y])
                if upto < 9:
                    continue
                for g in range(2):
                    P.op("act", lambda e: e.activation(out=junk[:, :], in_=y[:, g * 512:(g + 1) * 512], func=AF.Square, accum_out=ss[:, g:g + 1]),
                         reads=[y], writes=[junk, ss])
                rstd_from_ss(P, ss, lnv, rstd, 2, 512)
                for g in range(2):
                    hs = slice(g * 512, (g + 1) * 512)
                    P.op("dve", lambda e: e.scalar_tensor_tensor(out=yn[:, hs], in0=y[:, hs], scalar=rstd[:, g:g + 1], in1=gn[:, hs],
                                                                 op0=ALU.mult, op1=ALU.mult), reads=[y, rstd, gn], writes=[yn])
                if upto < 10:
                    continue
                P.op("dve", lambda e: e.tensor_tensor(out=v3(xdtt[:, :], 16), in0=v3(xdt[:, :], 16),
                                                      in1=v3(dec[:, :], 16)[:, :, 127:128].to_broadcast([128, 16, 64]), op=ALU.mult),
                     reads=[xdt, dec], writes=[xdtt])
                for g in range(2):
                    P.op("pe", lambda e: e.transpose(out=tpb[:, g * 128:(g + 1) * 128], in_=bcT[r][:, g, :], identity=P.ident[:, :]),
                         reads=[bcT[r], P.ident], writes=[tpb])
                P.op("act", lambda e: e.copy(out=btm[:, :], in_=tpb[:, 0:256]), reads=[tpb], writes=[btm])
                for g in range(2):
                    P.op("pe", lambda e: e.matmul(yip[g][:, :], lhsT=btm[:, g * 128:(g + 1) * 128], rhs=xdtt[:, g * 512:(g + 1) * 512], start=True, stop=True),
                         reads=[btm, xdtt], writes=[yip[g]])
                for g in range(2):
                    hs = slice(g * 512, (g + 1) * 512)
                    P.op("pool", lambda e: e.tensor_tensor(out=v3(stt[:, hs], 8), in0=v3(state[:, hs], 8),
                                                           in1=eal[:, ch * 16 + 8 * g:ch * 16 + 8 * g + 8].unsqueeze(2).to_broadcast([128, 8, 64]), op=ALU.mult),
                         reads=[state, eal], writes=[stt])
                    P.op("dve", lambda e: e.tensor_tensor(out=state[:, hs], in0=yip[g][:, :], in1=stt[:, hs], op=ALU.add), reads=[yip[g], stt], writes=[state])
                P.op("act", lambda e: e.copy(out=state_bf[:, :], in_=state[:, :]), reads=[state], writes=[state_bf])
                if upto < 11:
                    continue
                yr = (ci // 4) % 2 if False else ((s * 32 + ch) // 4) % 2
                for f in range(8):
                    P.op("pe", lambda e: e.transpose(out=tpb[:, f * 128:(f + 1) * 128], in_=yn[:, f * 128:(f + 1) * 128], identity=P.ident[:, :]),
                         reads=[yn, P.ident], writes=[tpb])
                P.op("act", lambda e: e.copy(out=yT[yr][:, :, (ch % 4) * 128:(ch % 4 + 1) * 128], in_=v3(tpb[:, :], 8)), reads=[tpb], writes=[yT[yr]])
                if ch % 4 == 3:
                    tb = ch // 4
                    P.dma("sp", mixT[s, 0:1024, tb * TB:(tb + 1) * TB].rearrange("(f p) t -> p f t", p=128), yT[yr][:, :, :], reads=[yT[yr]])
    P.barrier()
```

```python
from contextlib import ExitStack
import math
import numpy as np
import ml_dtypes
import concourse.bass as bass
import concourse.mybir as mybir
from concourse.bass_utils import run_bass_kernel_spmd

F32 = mybir.dt.float32
BF16 = mybir.dt.bfloat16
AF = mybir.ActivationFunctionType
ALU = mybir.AluOpType
AX = mybir.AxisListType

NCORES = 8
SPC = 2
S = 4096
D = 1024
DFF = 2816
NFT = DFF // 128
EPS = 1e-6
TB = 512
NTB = S // TB
NEG = -60000.0


class Buf:
    __slots__ = ("name", "w", "r")

    def __init__(self, name=""):
        self.name = name
        self.w = None
        self.r = {}


class Tile:
    __slots__ = ("t", "b")

    def __init__(self, t, name):
        self.t = t
        self.b = Buf(name)

    def __getitem__(self, idx):
        return self.t[idx]


class Prog:
    ENG = ("pe", "act", "dve", "pool", "sp")

    def __init__(self, nc, stack):
        self.nc = nc
        self.stack = stack
        self.eobj = {"pe": nc.tensor, "act": nc.scalar, "dve": nc.vector,
                     "pool": nc.gpsimd, "sp": nc.sync}
        self.esem = {}
        self.ecnt = {}
        for e in ("pe", "act", "dve", "pool"):
            self.esem[e] = stack.enter_context(nc.semaphore("s_" + e))
            self.ecnt[e] = 0
        self.waited = {e: {} for e in self.ENG}
        self.rings = {}
        for q, n in (("sp", 24), ("pool", 12), ("act", 8)):
            sems = [stack.enter_context(nc.semaphore(f"r_{q}{i}")) for i in range(n)]
            self.rings[q] = {"sems": sems, "n": 0}
        self.ninstr = 0

    def _need(self, eng, toks):
        best = {}
        for tk in toks:
            if tk is None:
                continue
            key, sem, val = tk
            if key == "pe" and eng == "pe":
                continue
            if self.waited[eng].get(key, 0) >= val:
                continue
            if key not in best or best[key][2] < val:
                best[key] = tk
        for key, (k, sem, val) in best.items():
            self.eobj[eng].wait_ge(sem, val)
            self.waited[eng][key] = val
            self.ninstr += 1

    def _deps(self, reads, writes):
        toks = []
        for b in reads:
            toks.append(b.w)
        for b in writes:
            toks.append(b.w)
            toks.extend(b.r.values())
        return toks

    def _record(self, tok, reads, writes):
        for b in reads:
            old = b.r.get(tok[0])
            if old is None or old[2] < tok[2]:
                b.r[tok[0]] = tok
        for b in writes:
            b.w = tok
            b.r = {}

    @staticmethod
    def _bufs(lst):
        return [x.b if isinstance(x, Tile) else x for x in lst]

    def op(self, eng, fn, reads=(), writes=()):
        reads = self._bufs(reads)
        writes = self._bufs(writes)
        self._need(eng, self._deps(reads, writes))
        ins = fn(self.eobj[eng])
        self.ecnt[eng] += 1
        ins.then_inc(self.esem[eng], 1)
        tok = (eng, self.esem[eng], self.ecnt[eng])
        self._record(tok, reads, writes)
        self.ninstr += 1
        return ins

    def dma(self, q, out, in_, reads=(), writes=(), **kw):
        reads = self._bufs(reads)
        writes = self._bufs(writes)
        ring = self.rings[q]
        n = ring["n"]
        R = len(ring["sems"])
        sem = ring["sems"][n % R]
        key = f"ring_{q}{n % R}"
        prev = 16 * (n // R)
        toks = self._deps(reads, writes)
        if prev > 0:
            toks.append((key, sem, prev))
        self._need(q, toks)
        ins = self.eobj[q].dma_start(out=out, in_=in_, **kw)
        ins.then_inc(sem, 16)
        ring["n"] = n + 1
        tok = (key, sem, prev + 16)
        self._record(tok, reads, writes)
        self.ninstr += 1
        return ins

    def barrier(self):
        toks = []
        for e in ("pe", "act", "dve", "pool"):
            if self.ecnt[e] > 0:
                toks.append((e, self.esem[e], self.ecnt[e]))
        for q, ring in self.rings.items():
            R = len(ring["sems"])
            for i in range(min(R, ring["n"])):
                cnt = (ring["n"] - 1 - i) // R + 1
                toks.append((f"ring_{q}{i}", ring["sems"][i], 16 * cnt))
        for e in self.ENG:
            self._need(e, toks)

    def sb(self, ctx, name, shape, dt):
        self.uid = getattr(self, "uid", 0) + 1
        name = f"sb{self.uid}_{name}"
        return Tile(ctx.enter_context(self.nc.sbuf_tensor(name, list(shape), dt)), name)

    def ps(self, ctx, name, shape, dt=F32):
        self.uid = getattr(self, "uid", 0) + 1
        name = f"ps{self.uid}_{name}"
        return Tile(ctx.enter_context(self.nc.psum_tensor(name, list(shape), dt)), name)


def bcast_rows(ap_1d, nparts):
    return ap_1d.partition_broadcast(nparts)


def _rope_np(dim):
    inv = (np.float32(10000.0) ** (-np.arange(0, dim, 2, dtype=np.float32) / np.float32(dim))).astype(np.float32)
    ang = (np.arange(S, dtype=np.float32)[:, None] * inv[None, :]).astype(np.float32)
    return np.cos(ang).astype(np.float32).T.copy(), np.sin(ang).astype(np.float32).T.copy()


def rope_tables_host():
    c64, s64 = _rope_np(64)
    c128, s128 = _rope_np(128)
    tab = np.zeros((3, 2, 128, S), np.float32)
    tab[0, 0] = np.tile(c64, (4, 1)); tab[0, 1] = np.tile(s64, (4, 1))
    tab[1, 0] = np.tile(c128, (2, 1)); tab[1, 1] = np.tile(s128, (2, 1))
    tab[2, 0, 0:64] = c128; tab[2, 1, 0:64] = s128
    tab[2, 0, 64:96] = c64; tab[2, 1, 64:96] = s64
    return tab


def pair_cols_h64(base, p):
    A = [base + (4 * p + m) * 64 + d for m in range(4) for d in range(32)]
    return A, [c + 32 for c in A]


def pair_cols_h128(base, p):
    A = [base + (2 * p + m) * 128 + d for m in range(2) for d in range(64)]
    return A, [c + 64 for c in A]


E_AQ, E_AK, E_AV, E_BQ, E_BK, E_BV, E_IQ, E_IK, E_IW = 0, 512, 1024, 1536, 2048, 2176, 2304, 2816, 2880


def l0_layout():
    fm_cols, types = [], []
    for base in (E_AQ, E_AK, E_IQ):
        for p in range(2):
            A, B = pair_cols_h64(base, p)
            fm_cols += A + B
            types.append(0)
    for p in range(2):
        A, B = pair_cols_h128(E_BQ, p)
        fm_cols += A + B
        types.append(1)
    A = [E_BK + d for d in range(64)] + [E_IK + d for d in range(32)] + [0] * 32
    B = [E_BK + 64 + d for d in range(64)] + [E_IK + 32 + d for d in range(32)] + [0] * 32
    fm_cols += A + B
    types.append(2)
    tm_cols = list(range(E_AV, E_AV + 512)) + list(range(E_BV, E_BV + 128)) + list(range(E_IW, E_IW + 8))
    return np.array(fm_cols), types, np.array(tm_cols)


def load_weight_bf16(P, ctx, name, w_ap, K, N, wt=None):
    kc = K // 128
    if wt is None:
        wt = P.sb(ctx, name, [128, kc, N], BF16)
    src = w_ap.rearrange("(c p) n -> p c n", p=128)
    step = max(1, 2048 // N) if N <= 2048 else 1
    for c0 in range(0, kc, step):
        c1 = min(kc, c0 + step)
        if N <= 2048:
            P.dma("pool", wt[:, c0:c1, :], src[:, c0:c1, :], writes=[wt])
        else:
            for n0 in range(0, N, 2048):
                n1 = min(N, n0 + 2048)
                P.dma("pool", wt[:, c0:c1, n0:n1], src[:, c0:c1, n0:n1], writes=[wt])
    return wt


def rms_block(P, xt, ss, junk, lnv, rstd, ntile):
    for j in range(ntile):
        P.op("act", lambda e, j=j: e.activation(out=junk[:, :], in_=xt[j][:, :], func=AF.Square,
                                                  accum_out=ss[:, j:j + 1]),
             reads=[xt[j]], writes=[junk, ss])
    P.op("act", lambda e: e.activation(out=lnv[:, 0:ntile], in_=ss[:, 0:ntile], func=AF.Ln,
                                        scale=1.0 / D, bias=P.eps_t[:, 0:1]),
         reads=[ss, P.eps_t], writes=[lnv])
    P.op("act", lambda e: e.activation(out=rstd[:, 0:ntile], in_=lnv[:, 0:ntile], func=AF.Exp, scale=-0.5),
         reads=[lnv], writes=[rstd])


def phase_inproj(P, nc, cfg):
    x_ap, g_ap = cfg["x"], cfg["g"]
    nfm, ntm = cfg["nfm"], cfg["ntm"]
    with ExitStack() as ctx:
        wfm = load_weight_bf16(P, ctx, "wfm", cfg["wfm"], D, nfm * 128)
        wtm = load_weight_bf16(P, ctx, "wtm", cfg["wtm"], D, ntm)
        gbc = P.sb(ctx, "gbc", [128, D], F32)
        P.dma("sp", gbc[:, :], g_ap.partition_broadcast(128), writes=[gbc])
        xt = [[P.sb(ctx, f"xt{r}_{j}", [128, D], F32) for j in range(4)] for r in range(2)]
        hn = [P.sb(ctx, f"hn{j}", [128, D], BF16) for j in range(4)]
        hnT = [P.sb(ctx, f"hnT{r}", [128, 8, TB], BF16) for r in range(2)]
        junk = P.sb(ctx, "junk", [128, D], BF16)
        ss = [P.sb(ctx, f"ss{r}", [128, 4], F32) for r in range(2)]
        lnv = P.sb(ctx, "lnv", [128, 4], F32)
        rstd = [P.sb(ctx, f"rstd{r}", [128, 4], F32) for r in range(2)]
        ntab = len(set(t for t in cfg["types"] if t is not None))
        tabs = {}
        for ty in sorted(set(t for t in cfg["types"] if t is not None)):
            tabs[ty] = [[P.sb(ctx, f"tab{ty}_{cs}_{r}", [128, TB], F32) for cs in range(2)] for r in range(2)]
        tmp = [[P.sb(ctx, f"rt{r}_{i}", [128, TB], F32) for i in range(4)] for r in range(2)]
        oA = [P.sb(ctx, f"oA{r}", [128, TB], BF16) for r in range(3)]
        oB = [P.sb(ctx, f"oB{r}", [128, TB], BF16) for r in range(3)]
        tp = [P.ps(ctx, f"tp{r}", [128, TB], BF16) for r in range(2)]
        psA = [P.ps(ctx, f"psA{r}", [128, TB]) for r in range(2)]
        psB = [P.ps(ctx, f"psB{r}", [128, TB]) for r in range(2)]
        pst = [P.ps(ctx, f"pst{r}", [128, TB]) for r in range(2)]
        extra = cfg["alloc"](P, ctx) if "alloc" in cfg else None

        blocks = [(s, tb) for s in range(SPC) for tb in range(NTB)]

        def issue_loads(bi):
            s, tb = blocks[bi]
            r = bi % 2
            for j in range(4):
                t0 = tb * TB + j * 128
                P.dma("sp", xt[r][j][:, :], x_ap[s, t0:t0 + 128, :], writes=[xt[r][j]])
            for ty, tt in tabs.items():
                for cs in range(2):
                    P.dma("sp", tt[r][cs][:, :], cfg["rope"][ty, cs, :, tb * TB:(tb + 1) * TB], writes=[tt[r][cs]])

        issue_loads(0)
        ocnt = 0
        for bi, (s, tb) in enumerate(blocks):
            r = bi % 2
            if bi + 1 < len(blocks):
                issue_loads(bi + 1)
            rms_block(P, xt[r], ss[r], junk, lnv, rstd[r], 4)
            for j in range(4):
                P.op("dve", lambda e, j=j: e.scalar_tensor_tensor(
                    out=hn[j][:, :], in0=xt[r][j][:, :], scalar=rstd[r][:, j:j + 1], in1=gbc[:, :],
                    op0=ALU.mult, op1=ALU.mult), reads=[xt[r][j], rstd[r], gbc], writes=[hn[j]])
            for c in range(8):
                tpc = tp[c % 2]
                for j in range(4):
                    P.op("pe", lambda e, j=j, c=c, tpc=tpc: e.transpose(
                        out=tpc[:, j * 128:(j + 1) * 128], in_=hn[j][:, c * 128:(c + 1) * 128],
                        identity=P.ident[:, :]), reads=[hn[j], P.ident], writes=[tpc])
                P.op("act", lambda e, c=c, tpc=tpc: e.copy(out=hnT[r][:, c, :], in_=tpc[:, :]),
                     reads=[tpc], writes=[hnT[r]])
            ft = 0
            pi = 0
            while ft < nfm:
                ty = cfg["types"][pi]
                if ty is None:
                    pr = pi % 2
                    for c in range(8):
                        P.op("pe", lambda e, c=c, ft=ft, pr=pr: e.matmul(
                            psA[pr][:, :], lhsT=wfm[:, c, ft * 128:(ft + 1) * 128], rhs=hnT[r][:, c, :],
                            start=(c == 0), stop=(c == 7)), reads=[wfm, hnT[r]], writes=[psA[pr]])
                    cfg["plain_handler"](P, extra, s, tb, ft, psA[pr])
                    ft += 1
                    pi += 1
                    continue
                pr = pi % 2
                for half, ps in ((0, psA[pr]), (1, psB[pr])):
                    for c in range(8):
                        P.op("pe", lambda e, c=c, ps=ps, f=ft + half: e.matmul(
                            ps[:, :], lhsT=wfm[:, c, f * 128:(f + 1) * 128], rhs=hnT[r][:, c, :],
                            start=(c == 0), stop=(c == 7)), reads=[wfm, hnT[r]], writes=[ps])
                cosT, sinT = tabs[ty][r]
                t1, t2, t3, t4 = tmp[pr]
                A, B = psA[pr], psB[pr]
                for (o, a, b) in ((t1, A, cosT), (t2, B, sinT), (t3, B, cosT), (t4, A, sinT)):
                    P.op("dve", lambda e, o=o, a=a, b=b: e.tensor_tensor(out=o[:, :], in0=a[:, :], in1=b[:, :], op=ALU.mult),
                         reads=[a, b], writes=[o])
                oa, ob = oA[ocnt % 3], oB[ocnt % 3]
                ocnt += 1
                if cfg.get("rope_f32") and cfg["rope_f32"](pi):
                    cfg["rope_f32_handler"](P, extra, s, tb, pi, ft, t1, t2, t3, t4, oa, ob)
                else:
                    P.op("pool", lambda e, oa=oa: e.tensor_tensor(out=oa[:, :], in0=t1[:, :], in1=t2[:, :], op=ALU.subtract),
                         reads=[t1, t2], writes=[oa])
                    P.op("pool", lambda e, ob=ob: e.tensor_tensor(out=ob[:, :], in0=t3[:, :], in1=t4[:, :], op=ALU.add),
                         reads=[t3, t4], writes=[ob])
                P.dma("sp", cfg["fmT"][s, ft, :, tb * TB:(tb + 1) * TB], oa[:, :], reads=[oa])
                P.dma("sp", cfg["fmT"][s, ft + 1, :, tb * TB:(tb + 1) * TB], ob[:, :], reads=[ob])
                ft += 2
                pi += 1
            cfg["tm_handler"](P, extra, s, tb, r, hnT[r], wtm, pst)
    P.barrier()


def l0_alloc(P, ctx):
    ex = {}
    ex["tmo"] = [P.sb(ctx, f"tmo{r}", [128, 640], BF16) for r in range(2)]
    ex["tmw"] = [P.sb(ctx, f"tmw{r}", [128, 8], F32) for r in range(2)]
    ex["cnt"] = 0
    return ex


def make_l0_tm_handler(tmv, tmw):
    def handler(P, ex, s, tb, r, hnT, wtm, pst):
        for j in range(4):
            t0 = tb * TB + j * 128
            for c in range(8):
                P.op("pe", lambda e: e.matmul(pst[0][:, :], lhsT=hnT[:, c, j * 128:(j + 1) * 128], rhs=wtm[:, c, 0:512],
                                              start=(c == 0), stop=(c == 7)), reads=[hnT, wtm], writes=[pst[0]])
            for c in range(8):
                P.op("pe", lambda e: e.matmul(pst[1][:, 0:136], lhsT=hnT[:, c, j * 128:(j + 1) * 128], rhs=wtm[:, c, 512:648],
                                              start=(c == 0), stop=(c == 7)), reads=[hnT, wtm], writes=[pst[1]])
            k = ex["cnt"] % 2
            ex["cnt"] += 1
            o, w = ex["tmo"][k], ex["tmw"][k]
            P.op("act", lambda e: e.copy(out=o[:, 0:512], in_=pst[0][:, :]), reads=[pst[0]], writes=[o])
            P.op("act", lambda e: e.copy(out=o[:, 512:640], in_=pst[1][:, 0:128]), reads=[pst[1]], writes=[o])
            P.op("act", lambda e: e.copy(out=w[:, :], in_=pst[1][:, 128:136]), reads=[pst[1]], writes=[w])
            P.dma("sp", tmv[s, t0:t0 + 128, :], o[:, :], reads=[o])
            P.dma("sp", tmw[s, :, t0 // 128, :], w[:, :], reads=[w])
    return handler


def build_program(phases, debug=False, h2_input=False):
    nc = bass.Bass("TRN2", target_bir_lowering=False)
    dbg_names = set(debug) if isinstance(debug, (list, tuple, set)) else None

    def din(name, shape, dt=F32):
        return nc.dram_tensor(name, list(shape), dt, kind="ExternalInput").ap()

    def dsc(name, shape, dt):
        ext = debug and (dbg_names is None or name in dbg_names)
        return nc.dram_tensor(name, list(shape), dt, kind="ExternalOutput" if ext else "Internal").ap()

    spec = {}

    spec["x"] = (din, ("x", [SPC, S, D],))
    spec["norms"] = (din, ("norms", [8, D]))
    spec["rope"] = (din, ("rope", [3, 2, 128, S],))
    spec["ident"] = (din, ("ident", [128, 128], BF16,))
    spec["wfm0"] = (din, ("wfm0", [D, 18 * 128],))
    spec["wtm0"] = (din, ("wtm0", [D, 648],))
    spec["fm0"] = (dsc, ("fm0", [SPC, 18, 128, S], BF16,))
    spec["tmv0"] = (dsc, ("tmv0", [SPC, S, 640], BF16,))
    spec["tmw0"] = (dsc, ("tmw0", [SPC, 128, 32, 8], F32,))
    spec["mixT0"] = (dsc, ("mixT0", [SPC, 1024, S], BF16,))
    spec["diff_lambda"] = (din, ("diff_lambda", [4, 64],))
    spec["diff_subln"] = (din, ("diff_subln", [128],))
    spec["tri"] = (din, ("tri", [128, 128], BF16,))
    spec["dmask"] = (din, ("dmask", [4, 128, TB],))
    spec["pow2"] = (din, ("pow2", [NIT],))
    spec["wout0"] = (din, ("wout0", [1024, D],))
    spec["wg"] = (din, ("wg", [2, D, DFF],))
    spec["wu"] = (din, ("wu", [2, D, DFF],))
    spec["wd"] = (din, ("wd", [2, DFF, D],))
    spec["h1"] = (dsc, ("h1", [SPC, S, D], F32,))
    spec["h2"] = (dsc, ("h2", [SPC, S, D], F32,))
    spec["wfm1"] = (din, ("wfm1", [D, 20 * 128],))
    spec["wtm1"] = (din, ("wtm1", [D, 1552],))
    spec["wout1"] = (din, ("wout1", [1536, D],))
    spec["convw"] = (din, ("convw", [1536, 4],))
    spec["convb"] = (din, ("convb", [128, 12],))
    spec["pm"] = (din, ("pm", [256],))
    spec["oh"] = (din, ("oh", [256],))
    spec["blk1h"] = (din, ("blk1h", [16, S], BF16,))
    spec["triU"] = (din, ("triU", [128, 128],))
    spec["sel16"] = (din, ("sel16", [16, 16 * 128],))
    spec["ssm_norm"] = (din, ("ssm_norm", [1024],))
    spec["dsk"] = (din, ("dsk", [1024],))
    spec["dt_bias"] = (din, ("dt_bias", [16],))
    spec["a_log"] = (din, ("a_log", [16],))
    spec["fm1"] = (dsc, ("fm1", [SPC, 20, 128, S], BF16,))
    spec["xbcT"] = (dsc, ("xbcT", [SPC, 12, 128, S], BF16,))
    spec["mqf"] = (dsc, ("mqf", [SPC, 4, 128, S], F32,))
    spec["kmean"] = (dsc, ("kmean", [SPC, 4, 128, 16], F32,))
    spec["tm1"] = (dsc, ("tm1", [SPC, S, 1536], BF16,))
    spec["dtraw"] = (dsc, ("dtraw", [SPC, 128, 32, 16], F32,))
    spec["negT"] = (dsc, ("negT", [SPC, 128, S], BF16,))
    spec["mixT1"] = (dsc, ("mixT1", [SPC, 1536, S], BF16,))
    spec["h3"] = (dsc, ("h3", [SPC, S, D], F32,))
    if h2_input:
        spec["h2"] = (din, ("h2", [SPC, S, D]))
    if not debug:
        spec["out"] = (lambda name, shape, dt: nc.dram_tensor(name, list(shape), dt, kind="ExternalOutput").ap(), ("out", [SPC, S, D], F32))

    class Lazy(dict):
        def __missing__(self, key):
            fn, args = spec[key]
            v = fn(*args)
            self[key] = v
            return v

    T = Lazy()
    T["dbg"] = DBG
    with ExitStack() as stack:
        P = Prog(nc, stack)
        P.ident = P.sb(stack, "ident_sb", [128, 128], BF16)
        P.eps_t = P.sb(stack, "eps_t", [128, 1], F32)
        P.one_t = P.sb(stack, "one_t", [128, 1], F32)
        P.dma("sp", P.ident[:, :], T["ident"], writes=[P.ident])
        P.op("dve", lambda e: e.memset(P.eps_t[:, :], EPS), writes=[P.eps_t])
        P.op("dve", lambda e: e.memset(P.one_t[:, :], 1.0), writes=[P.one_t])
        _, types0, _ = l0_layout()
        if "A0" in phases:
            phase_inproj(P, nc, dict(x=T["x"], g=T["norms"][0], wfm=T["wfm0"], wtm=T["wtm0"], nfm=18, ntm=648,
                                     types=types0, rope=T["rope"], fmT=T["fm0"], alloc=l0_alloc,
                                     tm_handler=make_l0_tm_handler(T["tmv0"], T["tmw0"])))
        if "B0" in phases:
            phase_diff(P, nc, T, 0.8 - 0.6 * math.exp(-0.3 * 0))
        if "B1" in phases:
            phase_dsa(P, nc, T)
        if "C0" in phases:
            phase_outproj(P, nc, T["mixT0"], T["wout0"], 1024, T["norms"][1], T["x"], T["h1"],
                          (T["wg"][0], T["wu"][0], T["wd"][0], T["norms"][2], T["norms"][3], T["h1"], T["h2"]))
        if "A1" in phases:
            phase_inproj(P, nc, make_l1_cfg(T))
        if "B2" in phases:
            phase_ssd(P, nc, T)
        if "B3" in phases:
            phase_moba_gate(P, nc, T)
            phase_moba_attn(P, nc, T)
        if "C1" in phases:
            phase_outproj(P, nc, T["mixT1"], T["wout1"], 1536, T["norms"][5], T["h2"], T["h3"],
                          (T["wg"][1], T["wu"][1], T["wd"][1], T["norms"][6], T["norms"][7], T["h3"], T["h3"] if debug else T["out"]))
        P.barrier()
        nc.used_inputs = set(k for k in T.keys() if k in spec and spec[k][0] is din)
        print("instructions:", P.ninstr, {e: P.ecnt[e] for e in P.ecnt})
    return nc


def host_inputs(inputs):
    x = np.ascontiguousarray(inputs["x"], dtype=np.float32)
    fm_cols, _, tm_cols = l0_layout()
    w_in0 = np.asarray(inputs["even_w_in"][0], np.float32)
    fm_cols1, _, tm_cols1 = l1_layout()
    w_in1 = np.asarray(inputs["odd_w_in"][0], np.float32)
    norms = np.stack([inputs["norm_mix_pre"][0], inputs["norm_mix_post"][0], inputs["norm_ffn_pre"][0], inputs["norm_ffn_post"][0],
                      inputs["norm_mix_pre"][1], inputs["norm_mix_post"][1], inputs["norm_ffn_pre"][1], inputs["norm_ffn_post"][1]]).astype(np.float32)
    common = {
        "norms": norms,
        "rope": rope_tables_host(),
        "ident": np.eye(128, dtype=np.float32).astype(ml_dtypes.bfloat16),
        "wfm0": np.ascontiguousarray(w_in0[:, fm_cols]),
        "wtm0": np.ascontiguousarray(w_in0[:, tm_cols]),
        "diff_lambda": np.asarray(inputs["diff_lambda"][0], np.float32),
        "diff_subln": np.asarray(inputs["diff_subln"][0], np.float32),
        "dmask": np.stack([np.where(np.arange(TB)[None, :] <= 128 * qt + np.arange(128)[:, None], 0.0, NEG) for qt in range(4)]).astype(np.float32),
        "pow2": (0.5 ** np.arange(1, NIT + 1)).astype(np.float32),
        "wout0": np.asarray(inputs["even_w_out"][0], np.float32),
        "wg": np.asarray(inputs["ffn_gate"], np.float32),
        "wu": np.asarray(inputs["ffn_up"], np.float32),
        "wd": np.asarray(inputs["ffn_down"], np.float32),
        "wfm1": np.ascontiguousarray(w_in1[:, fm_cols1]),
        "wtm1": np.ascontiguousarray(w_in1[:, tm_cols1]),
        "wout1": np.asarray(inputs["odd_w_out"][0], np.float32),
        "convw": np.ascontiguousarray(np.asarray(inputs["ssm_conv_w"][0], np.float32).T),
        "convb": np.ascontiguousarray(np.asarray(inputs["ssm_conv_b"][0], np.float32).reshape(12, 128).T),
        "pm": np.where(np.arange(16)[None, :] < np.arange(16)[:, None], 0.0, NEG).astype(np.float32).reshape(256),
        "oh": np.eye(16, dtype=np.float32).reshape(256),
        "blk1h": (np.arange(S)[None, :] // 256 == np.arange(16)[:, None]).astype(np.float32).astype(ml_dtypes.bfloat16),
        "triU": np.triu(np.ones((128, 128), np.float32)),
        "sel16": np.repeat(np.eye(16, dtype=np.float32)[:, :, None], 128, axis=2).reshape(16, 16 * 128),
        "ssm_norm": np.asarray(inputs["ssm_norm"][0], np.float32),
        "dsk": np.repeat(np.asarray(inputs["ssm_d"][0], np.float32), 64),
        "dt_bias": np.asarray(inputs["ssm_dt_bias"][0], np.float32),
        "a_log": np.asarray(inputs["ssm_a_log"][0], np.float32),
        "tri": np.triu(np.ones((128, 128), np.float32)).astype(ml_dtypes.bfloat16),
    }
    maps = []
    for c in range(NCORES):
        m = dict(common)
        m["x"] = x[c * SPC:(c + 1) * SPC]
        maps.append(m)
    return maps


def filter_maps(nc, maps):
    used = nc.used_inputs
    return [{k: v for k, v in m.items() if k in used} for m in maps]


DBG = {}
ALL_PHASES = ["A0", "B0", "B1", "C0", "A1", "B2", "B3", "C1"]


def kernel(**inputs):
    nc = build_program(ALL_PHASES)
    maps = filter_maps(nc, host_inputs(inputs))
    res = run_bass_kernel_spmd(nc, maps, core_ids=list(range(NCORES)))
    return np.concatenate([r["out"] for r in res.results], axis=0)


def attn_core(P, A, qT, kT_of, v1_of, qb, E, scale, rd, mask_of=None, tri=None, diag_only_tri=True):
    nkt = 4 * (qb + 1)
    started = [False, False]

    def stage1(kt):
        r = kt - 4 * qb
        st = A["st"][A["i"] % 2]
        pt = A["pt"][A["i"] % len(A["pt"])]
        A["i"] += 1
        P.op("pe", lambda e: e.matmul(st[:, :], lhsT=kT_of(kt), rhs=qT, start=True, stop=True),
             reads=rd, writes=[st])
        P.op("act", lambda e: e.activation(out=pt[:, :], in_=st[:, :], func=AF.Exp, scale=scale),
             reads=[st], writes=[pt])
        if mask_of is not None:
            m_ap, m_t = mask_of(kt)
            P.op("pool", lambda e: e.tensor_tensor(out=pt[:, :], in0=pt[:, :], in1=m_ap, op=ALU.mult),
                 reads=[pt, m_t], writes=[pt])
        elif r >= 0:
            P.op("pool", lambda e: e.tensor_tensor(out=pt[:, r * 128:(r + 1) * 128], in0=pt[:, r * 128:(r + 1) * 128],
                                                   in1=tri[:, :], op=ALU.mult), reads=[pt, tri], writes=[pt])
        return pt

    def stage2(kt, pt):
        r = kt - 4 * qb
        for qt in range(4):
            if r > qt:
                continue
            acc = A["acc"][qt // 2]
            first = not started[qt // 2]
            started[qt // 2] = True
            last = (kt == 4 * qb + qt)
            P.op("pe", lambda e: e.matmul(acc[:, qt % 2, 0:E + 1], lhsT=pt[:, qt * 128:(qt + 1) * 128], rhs=v1_of(kt),
                                          start=first, stop=last), reads=[pt] + rd, writes=[acc])

    prev = None
    for kt in range(nkt):
        cur = stage1(kt)
        if prev is not None:
            stage2(*prev)
        prev = (kt, cur)
    stage2(*prev)


def q_rows_h64(fm, s, base_tile, m):
    p, ml = m // 4, m % 4
    return fm[s, base_tile + 2 * p, 32 * ml:32 * ml + 32, :], fm[s, base_tile + 2 * p + 1, 32 * ml:32 * ml + 32, :]


def compute_lambda(P, ctx, dl_ap, lam_init):
    lf = P.sb(ctx, "lf", [128, 4, 64], F32)
    P.dma("sp", lf[:, :, :], dl_ap.rearrange("a d -> (a d)").partition_broadcast(128).rearrange("p (a d) -> p a d", a=4), writes=[lf])
    pr = P.sb(ctx, "lpr", [128, 2, 64], F32)
    sm = P.sb(ctx, "lsm", [128, 2], F32)
    ex = P.sb(ctx, "lex", [128, 2], F32)
    nl = P.sb(ctx, "nlam", [128, 1], F32)
    P.op("dve", lambda e: e.tensor_tensor(out=pr[:, 0, :], in0=lf[:, 0, :], in1=lf[:, 1, :], op=ALU.mult), reads=[lf], writes=[pr])
    P.op("dve", lambda e: e.tensor_tensor(out=pr[:, 1, :], in0=lf[:, 2, :], in1=lf[:, 3, :], op=ALU.mult), reads=[lf, pr], writes=[pr])
    P.op("dve", lambda e: e.tensor_reduce(out=sm[:, :], in_=pr[:, :, :], axis=AX.X, op=ALU.add), reads=[pr], writes=[sm])
    P.op("act", lambda e: e.activation(out=ex[:, :], in_=sm[:, :], func=AF.Exp), reads=[sm], writes=[ex])
    P.op("dve", lambda e: e.tensor_tensor(out=nl[:, :], in0=ex[:, 1:2], in1=ex[:, 0:1], op=ALU.subtract), reads=[ex], writes=[nl])
    P.op("dve", lambda e: e.tensor_scalar(out=nl[:, :], in0=nl[:, :], scalar1=-lam_init, scalar2=None, op0=ALU.add), reads=[nl], writes=[nl])
    return nl


def phase_diff(P, nc, T, lam_init):
    fm, tmv, mixT = T["fm0"], T["tmv0"], T["mixT0"]
    with ExitStack() as ctx:
        nlam = compute_lambda(P, ctx, T["diff_lambda"], lam_init)
        g2 = P.sb(ctx, "g2", [128, 128], F32)
        P.dma("sp", g2[:, :], T["diff_subln"].partition_broadcast(128), writes=[g2])
        P.op("dve", lambda e: e.tensor_scalar(out=g2[:, :], in0=g2[:, :], scalar1=1.0 - lam_init, scalar2=None, op0=ALU.mult),
             reads=[g2], writes=[g2])
        tri = P.sb(ctx, "tri", [128, 128], BF16)
        P.dma("sp", tri[:, :], T["tri"], writes=[tri])
        kT = [P.sb(ctx, f"kT{r}", [128, S], BF16) for r in range(2)]
        v1 = [P.sb(ctx, f"v1{r}", [128, 32, 129], BF16) for r in range(2)]
        for r in range(2):
            P.op("pool", lambda e: e.memset(v1[r][:, :, 128:129], 1.0), writes=[v1[r]])
        qT = [P.sb(ctx, f"qT{r}", [128, TB], BF16) for r in range(2)]
        A = {"st": [P.ps(ctx, f"st{r}", [128, TB]) for r in range(2)],
             "pt": [P.sb(ctx, f"pt{r}", [128, TB], BF16) for r in range(3)], "i": 0}
        accs = [[P.ps(ctx, f"acc{m}_{r}", [128, 2, 256]) for r in range(2)] for m in range(2)]
        tp = P.ps(ctx, "tpo", [128, TB], BF16)
        o1 = [P.sb(ctx, f"o1_{r}", [128, 4, 128], F32) for r in range(2)]
        rc = [P.sb(ctx, f"rc{r}", [128, 8], F32) for r in range(2)]
        ss = P.sb(ctx, "dss", [128, 4], F32)
        lnv = P.sb(ctx, "dlnv", [128, 4], F32)
        rstd = P.sb(ctx, "drstd", [128, 4], F32)
        junk = P.sb(ctx, "djunk", [128, 128], BF16)
        ob = [P.sb(ctx, f"ob{r}", [128, 4, 128], BF16) for r in range(2)]
        oT = [P.sb(ctx, f"oT{r}", [128, TB], BF16) for r in range(2)]
        it = 0
        hi = 0
        for s in range(SPC):
            for h in range(4):
                kr = hi % 2
                hi += 1
                hh = h % 2
                p = h // 2
                srcs = [(4 + 2 * p, 64 * hh), (5 + 2 * p, 64 * hh), (4 + 2 * p, 64 * hh + 32), (5 + 2 * p, 64 * hh + 32)]
                for i, (tile, row) in enumerate(srcs):
                    P.dma("sp", kT[kr][32 * i:32 * i + 32, :], fm[s, tile, row:row + 32, :], writes=[kT[kr]])
                for half in range(2):
                    P.dma("sp", v1[kr][:, 16 * half:16 * half + 16, 0:128],
                          tmv[s, 2048 * half:2048 * (half + 1), h * 128:(h + 1) * 128].rearrange("(kt p) e -> p kt e", p=128),
                          writes=[v1[kr]])
                for qb in range(NTB):
                    qr = it % 2
                    it += 1
                    qsrcs = [(2 * p, 64 * hh), (2 * p + 1, 64 * hh), (2 * p, 64 * hh + 32), (2 * p + 1, 64 * hh + 32)]
                    for i, (tile, row) in enumerate(qsrcs):
                        P.dma("sp", qT[qr][32 * i:32 * i + 32, :], fm[s, tile, row:row + 32, qb * TB:(qb + 1) * TB], writes=[qT[qr]])
                    for m in range(2):
                        A["acc"] = accs[m]
                        attn_core(P, A, qT[qr][64 * m:64 * m + 64, :],
                                  lambda kt: kT[kr][64 * m:64 * m + 64, kt * 128:(kt + 1) * 128],
                                  lambda kt: v1[kr][:, kt, :], qb, 128, 0.125, [qT[qr], kT[kr], v1[kr]], tri=tri)
                        for qt in range(4):
                            acc = accs[m][qt // 2]
                            P.op("dve", lambda e: e.reciprocal(out=rc[qr][:, 4 * m + qt:4 * m + qt + 1], in_=acc[:, qt % 2, 128:129]),
                                 reads=[acc], writes=[rc[qr]])
                        if m == 1:
                            P.op("dve", lambda e: e.tensor_scalar(out=rc[qr][:, 4:8], in0=rc[qr][:, 4:8], scalar1=nlam[:, 0:1], scalar2=None,
                                                                  op0=ALU.mult), reads=[rc[qr], nlam], writes=[rc[qr]])
                        for qt in range(4):
                            acc = accs[m][qt // 2]
                            if m == 0:
                                P.op("dve", lambda e: e.tensor_scalar(out=o1[qr][:, qt, :], in0=acc[:, qt % 2, 0:128],
                                                                      scalar1=rc[qr][:, qt:qt + 1], scalar2=None, op0=ALU.mult),
                                     reads=[acc, rc[qr]], writes=[o1[qr]])
                            else:
                                P.op("dve", lambda e: e.scalar_tensor_tensor(out=o1[qr][:, qt, :], in0=acc[:, qt % 2, 0:128],
                                                                             scalar=rc[qr][:, 4 + qt:5 + qt], in1=o1[qr][:, qt, :],
                                                                             op0=ALU.mult, op1=ALU.add),
                                     reads=[acc, rc[qr], o1[qr]], writes=[o1[qr]])
                    for qt in range(4):
                        P.op("act", lambda e: e.activation(out=junk[:, :], in_=o1[qr][:, qt, :], func=AF.Square, accum_out=ss[:, qt:qt + 1]),
                             reads=[o1[qr]], writes=[junk, ss])
                    P.op("act", lambda e: e.activation(out=lnv[:, :], in_=ss[:, :], func=AF.Ln, scale=1.0 / 128, bias=P.eps_t[:, 0:1]),
                         reads=[ss, P.eps_t], writes=[lnv])
                    P.op("act", lambda e: e.activation(out=rstd[:, :], in_=lnv[:, :], func=AF.Exp, scale=-0.5), reads=[lnv], writes=[rstd])
                    for qt in range(4):
                        P.op("dve", lambda e: e.scalar_tensor_tensor(out=ob[qr][:, qt, :], in0=o1[qr][:, qt, :], scalar=rstd[:, qt:qt + 1],
                                                                     in1=g2[:, :], op0=ALU.mult, op1=ALU.mult),
                             reads=[o1[qr], rstd, g2], writes=[ob[qr]])
                        P.op("pe", lambda e: e.transpose(out=tp[:, qt * 128:(qt + 1) * 128], in_=ob[qr][:, qt, :], identity=P.ident[:, :]),
                             reads=[ob[qr], P.ident], writes=[tp])
                    P.op("act", lambda e: e.copy(out=oT[qr][:, :], in_=tp[:, :]), reads=[tp], writes=[oT[qr]])
                    P.dma("sp", mixT[s, h * 128:(h + 1) * 128, qb * TB:(qb + 1) * TB], oT[qr][:, :], reads=[oT[qr]])
    P.barrier()


def rstd_from_ss(P, ss, lnv, rstd, n, dim):
    P.op("act", lambda e: e.activation(out=lnv[:, 0:n], in_=ss[:, 0:n], func=AF.Ln, scale=1.0 / dim, bias=P.eps_t[:, 0:1]),
         reads=[ss, P.eps_t], writes=[lnv])
    P.op("act", lambda e: e.activation(out=rstd[:, 0:n], in_=lnv[:, 0:n], func=AF.Exp, scale=-0.5), reads=[lnv], writes=[rstd])


def phase_outproj(P, nc, mixT, wout_ap, kmix, g_ap, h_in, h_out, ffn_args):
    kc = kmix // 128
    with ExitStack() as octx:
      wg_t = P.sb(octx, "wg", [128, 8, DFF], BF16)
      wu_t = P.sb(octx, "wu", [128, 8, DFF], BF16)
      with ExitStack() as ctx:
        wout = load_weight_bf16(P, ctx, "wout", wout_ap, kmix, D)
        pre = {"wg": load_weight_bf16(P, ctx, "wg", ffn_args[0], D, DFF, wt=wg_t),
               "wu": load_weight_bf16(P, ctx, "wu", ffn_args[1], D, DFF, wt=wu_t)}
        gbc = P.sb(ctx, "gbc", [128, D], F32)
        P.dma("sp", gbc[:, :], g_ap.partition_broadcast(128), writes=[gbc])
        mt = [P.sb(ctx, f"mt{r}", [128, kc, TB], BF16) for r in range(2)]
        ht = [[P.sb(ctx, f"ht{r}_{j}", [128, D], F32) for j in range(4)] for r in range(2)]
        mo = [P.sb(ctx, f"mo{r}", [128, D], F32) for r in range(2)]
        junk = P.sb(ctx, "junk", [128, D], BF16)
        ss = [P.sb(ctx, f"ss{r}", [128, 1], F32) for r in range(2)]
        lnv = P.sb(ctx, "lnv", [128, 1], F32)
        rstd = [P.sb(ctx, f"rstd{r}", [128, 1], F32) for r in range(2)]
        ps = [P.ps(ctx, f"pso{r}", [128, TB]) for r in range(4)]
        blocks = [(s, tb) for s in range(SPC) for tb in range(NTB)]

        def loads(bi):
            s, tb = blocks[bi]
            r = bi % 2
            P.dma("sp", mt[r][:, :, :], mixT[s, :, tb * TB:(tb + 1) * TB].rearrange("(c p) t -> p c t", p=128), writes=[mt[r]])
            for j in range(4):
                t0 = tb * TB + j * 128
                P.dma("sp", ht[r][j][:, :], h_in[s, t0:t0 + 128, :], writes=[ht[r][j]])

        loads(0)
        k = 0
        for bi, (s, tb) in enumerate(blocks):
            r = bi % 2
            if bi + 1 < len(blocks):
                loads(bi + 1)
            for j in range(4):
                t0 = tb * TB + j * 128
                kk = k % 2
                k += 1
                for half in range(2):
                    pp = ps[2 * kk + half]
                    for c in range(kc):
                        P.op("pe", lambda e: e.matmul(pp[:, :], lhsT=mt[r][:, c, j * 128:(j + 1) * 128],
                                                      rhs=wout[:, c, half * 512:(half + 1) * 512], start=(c == 0), stop=(c == kc - 1)),
                             reads=[mt[r], wout], writes=[pp])
                    P.op("act", lambda e: e.copy(out=mo[kk][:, half * 512:(half + 1) * 512], in_=pp[:, :]), reads=[pp], writes=[mo[kk]])
                P.op("act", lambda e: e.activation(out=junk[:, :], in_=mo[kk][:, :], func=AF.Square, accum_out=ss[kk][:, 0:1]),
                     reads=[mo[kk]], writes=[junk, ss[kk]])
                rstd_from_ss(P, ss[kk], lnv, rstd[kk], 1, D)
                P.op("dve", lambda e: e.scalar_tensor_tensor(out=mo[kk][:, :], in0=mo[kk][:, :], scalar=rstd[kk][:, 0:1], in1=gbc[:, :],
                                                             op0=ALU.mult, op1=ALU.mult), reads=[mo[kk], rstd[kk], gbc], writes=[mo[kk]])
                P.op("pool", lambda e: e.tensor_tensor(out=ht[r][j][:, :], in0=ht[r][j][:, :], in1=mo[kk][:, :], op=ALU.add),
                     reads=[ht[r][j], mo[kk]], writes=[ht[r][j]])
                P.dma("sp", h_out[s, t0:t0 + 128, :], ht[r][j][:, :], reads=[ht[r][j]])
      P.barrier()
      phase_ffn(P, nc, *ffn_args, pre=pre)


FB = 256


def phase_ffn(P, nc, wg_ap, wu_ap, wd_ap, gpre_ap, gpost_ap, h_in, h_out, pre=None):
    with ExitStack() as ctx:
        wg, wu = pre["wg"], pre["wu"]
        wd = load_weight_bf16(P, ctx, "wd", wd_ap, DFF, D)
        gpre = P.sb(ctx, "gpre", [128, D], F32)
        gpost = P.sb(ctx, "gpost", [128, D], F32)
        P.dma("sp", gpre[:, :], gpre_ap.partition_broadcast(128), writes=[gpre])
        P.dma("sp", gpost[:, :], gpost_ap.partition_broadcast(128), writes=[gpost])
        ht = [[P.sb(ctx, f"ht{r}_{j}", [128, D], F32) for j in range(2)] for r in range(2)]
        hn = [P.sb(ctx, f"hn{j}", [128, D], BF16) for j in range(2)]
        hnT = P.sb(ctx, "hnT", [128, 8, FB], BF16)
        actT = P.sb(ctx, "actT", [128, NFT, FB], BF16)
        sg = [P.sb(ctx, f"sg{r}", [128, FB], F32) for r in range(2)]
        mo = [P.sb(ctx, f"mo{r}", [128, D], F32) for r in range(2)]
        junk = P.sb(ctx, "junk", [128, D], BF16)
        ss = [P.sb(ctx, f"ss{r}", [128, 2], F32) for r in range(2)]
        lnv = P.sb(ctx, "lnv", [128, 2], F32)
        rstd = [P.sb(ctx, f"rstd{r}", [128, 2], F32) for r in range(2)]
        ss2 = [P.sb(ctx, f"ss2{r}", [128, 1], F32) for r in range(2)]
        rstd2 = [P.sb(ctx, f"rstd2{r}", [128, 1], F32) for r in range(2)]
        tp = [P.ps(ctx, f"tp{r}", [128, FB], BF16) for r in range(2)]
        psg = [P.ps(ctx, f"psg{r}", [128, FB]) for r in range(2)]
        psu = [P.ps(ctx, f"psu{r}", [128, FB]) for r in range(2)]
        psd = [P.ps(ctx, f"psd{r}", [128, TB]) for r in range(2)]
        nb = S // FB
        blocks = [(s, tb) for s in range(SPC) for tb in range(nb)]

        def loads(bi):
            s, tb = blocks[bi]
            r = bi % 2
            for j in range(2):
                t0 = tb * FB + j * 128
                P.dma("sp", ht[r][j][:, :], h_in[s, t0:t0 + 128, :], writes=[ht[r][j]])

        loads(0)
        k = 0
        for bi, (s, tb) in enumerate(blocks):
            r = bi % 2
            if bi + 1 < len(blocks):
                loads(bi + 1)
            for j in range(2):
                P.op("act", lambda e: e.activation(out=junk[:, :], in_=ht[r][j][:, :], func=AF.Square, accum_out=ss[r][:, j:j + 1]),
                     reads=[ht[r][j]], writes=[junk, ss[r]])
            rstd_from_ss(P, ss[r], lnv, rstd[r], 2, D)
            for j in range(2):
                P.op("dve", lambda e: e.scalar_tensor_tensor(out=hn[j][:, :], in0=ht[r][j][:, :], scalar=rstd[r][:, j:j + 1], in1=gpre[:, :],
                                                             op0=ALU.mult, op1=ALU.mult), reads=[ht[r][j], rstd[r], gpre], writes=[hn[j]])
            for c in range(8):
                tpc = tp[c % 2]
                for j in range(2):
                    P.op("pe", lambda e: e.transpose(out=tpc[:, j * 128:(j + 1) * 128], in_=hn[j][:, c * 128:(c + 1) * 128],
                                                     identity=P.ident[:, :]), reads=[hn[j], P.ident], writes=[tpc])
                P.op("dve", lambda e: e.tensor_copy(out=hnT[:, c, :], in_=tpc[:, :]), reads=[tpc], writes=[hnT])
            for f in range(NFT):
                fr = f % 2
                for c in range(8):
                    P.op("pe", lambda e: e.matmul(psg[fr][:, :], lhsT=wg[:, c, f * 128:(f + 1) * 128], rhs=hnT[:, c, :],
                                                  start=(c == 0), stop=(c == 7)), reads=[wg, hnT], writes=[psg[fr]])
                for c in range(8):
                    P.op("pe", lambda e: e.matmul(psu[fr][:, :], lhsT=wu[:, c, f * 128:(f + 1) * 128], rhs=hnT[:, c, :],
                                                  start=(c == 0), stop=(c == 7)), reads=[wu, hnT], writes=[psu[fr]])
                P.op("act", lambda e: e.activation(out=sg[fr][:, :], in_=psg[fr][:, :], func=AF.Silu), reads=[psg[fr]], writes=[sg[fr]])
                P.op("dve", lambda e: e.tensor_tensor(out=actT[:, f, :], in0=psu[fr][:, :], in1=sg[fr][:, :], op=ALU.mult),
                     reads=[psu[fr], sg[fr]], writes=[actT])
            for j in range(2):
                t0 = tb * FB + j * 128
                kk = k % 2
                k += 1
                for half in range(2):
                    pp = psd[half]
                    for f in range(NFT):
                        P.op("pe", lambda e: e.matmul(pp[:, :], lhsT=actT[:, f, j * 128:(j + 1) * 128],
                                                      rhs=wd[:, f, half * 512:(half + 1) * 512], start=(f == 0), stop=(f == NFT - 1)),
                             reads=[actT, wd], writes=[pp])
                    P.op("act", lambda e: e.copy(out=mo[kk][:, half * 512:(half + 1) * 512], in_=pp[:, :]), reads=[pp], writes=[mo[kk]])
                P.op("act", lambda e: e.activation(out=junk[:, :], in_=mo[kk][:, :], func=AF.Square, accum_out=ss2[kk][:, 0:1]),
                     reads=[mo[kk]], writes=[junk, ss2[kk]])
                rstd_from_ss(P, ss2[kk], lnv, rstd2[kk], 1, D)
                P.op("dve", lambda e: e.scalar_tensor_tensor(out=mo[kk][:, :], in0=mo[kk][:, :], scalar=rstd2[kk][:, 0:1], in1=gpost[:, :],
                                                             op0=ALU.mult, op1=ALU.mult), reads=[mo[kk], rstd2[kk], gpost], writes=[mo[kk]])
                P.op("pool", lambda e: e.tensor_tensor(out=ht[r][j][:, :], in0=ht[r][j][:, :], in1=mo[kk][:, :], op=ALU.add),
                     reads=[ht[r][j], mo[kk]], writes=[ht[r][j]])
                P.dma("sp", h_out[s, t0:t0 + 128, :], ht[r][j][:, :], reads=[ht[r][j]])
    P.barrier()


NIT = 12
TOPK = 256


def phase_dsa(P, nc, T):
    fm, tmv, tmw, mixT = T["fm0"], T["tmv0"], T["tmw0"], T["mixT0"]
    dbg = T.get("dbg", {})
    n_s, n_qb, stages = dbg.get("n_s", SPC), dbg.get("n_qb", NTB), dbg.get("stages", "idx,bis,tr,att")
    with ExitStack() as ctx:
        dmask = P.sb(ctx, "dmask", [128, 4, TB], F32)
        P.dma("sp", dmask[:, :, :], T["dmask"].rearrange("a p k -> p a k"), writes=[dmask])
        pow2 = P.sb(ctx, "pow2", [128, NIT], F32)
        P.dma("sp", pow2[:, :], T["pow2"].partition_broadcast(128), writes=[pow2])
        bkT = P.sb(ctx, "bkT", [128, S], BF16)
        ikT = P.sb(ctx, "ikT", [128, S], BF16)
        bv1 = P.sb(ctx, "bv1", [128, 32, 129], BF16)
        P.op("pool", lambda e: e.memset(bv1[:, :, 128:129], 1.0), writes=[bv1])
        iw = P.sb(ctx, "iw", [128, 32, 8], F32)
        iqT = [[P.sb(ctx, f"iqT{r}_{i}", [128, TB], BF16) for i in range(4)] for r in range(2)]
        bqT = [[P.sb(ctx, f"bqT{r}_{i}", [128, TB], BF16) for i in range(4)] for r in range(2)]
        score = [P.sb(ctx, f"score{r}", [128, S], F32) for r in range(2)]
        mask = [P.sb(ctx, f"mask{i}", [128, S], BF16) for i in range(4)]
        maskT = [P.sb(ctx, f"maskT{r}", [128, 32, TB], BF16) for r in range(2)]
        junk = P.sb(ctx, "junk", [128, S], BF16)
        rl = [P.sb(ctx, f"rl{r}", [128, TB], F32) for r in range(4)]
        sm = {n: P.sb(ctx, "sm_" + n, [128, 1], F32) for n in ("hi", "lo", "rng", "th", "cand", "cnt", "m")}
        steps = P.sb(ctx, "steps", [128, NIT], F32)
        A = {"st": [P.ps(ctx, f"st{r}", [128, TB]) for r in range(2)],
             "pt": [P.sb(ctx, f"pt{r}", [128, TB], BF16) for r in range(3)], "i": 0}
        accs = [[P.ps(ctx, f"acc{m}_{r}", [128, 2, 256]) for r in range(2)] for m in range(2)]
        tpx = P.ps(ctx, "tpx", [128, TB], BF16)
        ist = A["st"] + [P.ps(ctx, "st_x", [128, TB])]
        ii = 0
        rc = P.sb(ctx, "rc", [128, 4], F32)
        ob = [P.sb(ctx, f"ob{r}", [128, 4, 128], BF16) for r in range(2)]
        oT = [P.sb(ctx, f"oT{r}", [128, TB], BF16) for r in range(2)]
        st8 = {"hcnt": 0, "sc_i": 0, "ii": 0}
        lnl = P.sb(ctx, "lnl", [128, 4], F32)

        def load_idx_side(s):
            for rep in range(2):
                P.dma("sp", ikT[64 * rep:64 * rep + 32, :], fm[s, 16, 64:96, :], writes=[ikT])
                P.dma("sp", ikT[64 * rep + 32:64 * rep + 64, :], fm[s, 17, 64:96, :], writes=[ikT])
            P.dma("sp", iw[:, :, :], tmw[s, :, :, :], writes=[iw])

        def load_att_side(s):
            P.dma("sp", bkT[0:64, :], fm[s, 16, 0:64, :], writes=[bkT])
            P.dma("sp", bkT[64:128, :], fm[s, 17, 0:64, :], writes=[bkT])
            for half in range(2):
                P.dma("sp", bv1[:, 16 * half:16 * half + 16, 0:128],
                      tmv[s, 2048 * half:2048 * (half + 1), 512:640].rearrange("(kt p) e -> p kt e", p=128), writes=[bv1])

        def front_a(s, qb, r):
            c0, c1 = qb * TB, (qb + 1) * TB
            for i in range(4):
                p, hl0 = (2 * i) // 4, (2 * i) % 4
                for k2 in range(2):
                    hl = hl0 + k2
                    P.dma("sp", iqT[r][i][64 * k2:64 * k2 + 32, :], fm[s, 8 + 2 * p, 32 * hl:32 * hl + 32, c0:c1], writes=[iqT[r][i]])
                    P.dma("sp", iqT[r][i][64 * k2 + 32:64 * k2 + 64, :], fm[s, 9 + 2 * p, 32 * hl:32 * hl + 32, c0:c1], writes=[iqT[r][i]])
            for h in range(4):
                p, hl = h // 2, h % 2
                P.dma("sp", bqT[r][h][0:64, :], fm[s, 12 + 2 * p, 64 * hl:64 * hl + 64, c0:c1], writes=[bqT[r][h]])
                P.dma("sp", bqT[r][h][64:128, :], fm[s, 13 + 2 * p, 64 * hl:64 * hl + 64, c0:c1], writes=[bqT[r][h]])
            for qt in range(4):
                gq = 4 * qb + qt
                n = 128 * (gq + 1)
                sc = score[st8["sc_i"] % 2]
                st8["sc_i"] += 1
                for kc in range(qb + 1):
                    for h in range(8):
                        st = ist[st8["ii"] % 3]
                        rlb = rl[st8["ii"] % 4]
                        st8["ii"] += 1
                        ro = 64 * (h % 2)
                        P.op("pe", lambda e: e.matmul(st[:, :], lhsT=iqT[r][h // 2][ro:ro + 64, qt * 128:(qt + 1) * 128],
                                                      rhs=ikT[ro:ro + 64, kc * TB:(kc + 1) * TB], start=True, stop=True),
                             reads=[iqT[r][h // 2], ikT], writes=[st])
                        P.op("act", lambda e: e.activation(out=rlb[:, :], in_=st[:, :], func=AF.Relu), reads=[st], writes=[rlb])
                        if h == 0:
                            P.op("dve", lambda e: e.tensor_scalar(out=sc[:, kc * TB:(kc + 1) * TB], in0=rlb[:, :], scalar1=iw[:, gq, 0:1],
                                                                  scalar2=None, op0=ALU.mult), reads=[rlb, iw], writes=[sc])
                        else:
                            P.op("dve", lambda e: e.scalar_tensor_tensor(out=sc[:, kc * TB:(kc + 1) * TB], in0=rlb[:, :],
                                                                         scalar=iw[:, gq, h:h + 1], in1=sc[:, kc * TB:(kc + 1) * TB],
                                                                         op0=ALU.mult, op1=ALU.add), reads=[rlb, iw, sc], writes=[sc])
                P.op("pool", lambda e: e.tensor_tensor(out=sc[:, c0:c1], in0=sc[:, c0:c1], in1=dmask[:, qt, :], op=ALU.add),
                     reads=[sc, dmask], writes=[sc])
                th = sm["th"]
                if gq < 2:
                    P.op("dve", lambda e: e.memset(th[:, :], NEG / 2), writes=[th])
                else:
                    P.op("dve", lambda e: e.tensor_reduce(out=sm["hi"][:, :], in_=sc[:, 0:n], axis=AX.X, op=ALU.max), reads=[sc], writes=[sm["hi"]])
                    P.op("dve", lambda e: e.tensor_reduce(out=th[:, :], in_=sc[:, 0:128 * gq], axis=AX.X, op=ALU.min), reads=[sc], writes=[th])
                    P.op("dve", lambda e: e.tensor_tensor(out=sm["rng"][:, :], in0=sm["hi"][:, :], in1=th[:, :], op=ALU.subtract),
                         reads=[sm["hi"], th], writes=[sm["rng"]])
                    P.op("dve", lambda e: e.tensor_scalar(out=steps[:, :], in0=pow2[:, :], scalar1=sm["rng"][:, 0:1], scalar2=None, op0=ALU.mult),
                         reads=[pow2, sm["rng"]], writes=[steps])
                    P.op("dve", lambda e: e.tensor_tensor(out=sm["cand"][:, :], in0=th[:, :], in1=steps[:, 0:1], op=ALU.add),
                         reads=[th, steps], writes=[sm["cand"]])
                    for j in range(NIT):
                        P.op("dve", lambda e: e.tensor_scalar(out=junk[:, 0:n], in0=sc[:, 0:n], scalar1=sm["cand"][:, 0:1], scalar2=None,
                                                              op0=ALU.is_ge, op1=ALU.add, accum_out=sm["cnt"][:, 0:1]),
                             reads=[sc, sm["cand"]], writes=[junk, sm["cnt"]])
                        P.op("dve", lambda e: e.tensor_scalar(out=sm["m"][:, :], in0=sm["cnt"][:, :], scalar1=float(TOPK), scalar2=-0.5,
                                                              op0=ALU.is_ge, op1=ALU.add), reads=[sm["cnt"]], writes=[sm["m"]])
                        P.op("dve", lambda e: e.scalar_tensor_tensor(out=sm["cand"][:, :], in0=sm["m"][:, :], scalar=steps[:, j:j + 1],
                                                                     in1=sm["cand"][:, :], op0=ALU.mult, op1=ALU.add),
                             reads=[sm["m"], steps, sm["cand"]], writes=[sm["cand"]])
                    P.op("dve", lambda e: e.scalar_tensor_tensor(out=th[:, :], in0=sm["rng"][:, :], scalar=-(0.5 ** (NIT + 1)),
                                                                 in1=sm["cand"][:, :], op0=ALU.mult, op1=ALU.add),
                         reads=[sm["rng"], sm["cand"]], writes=[th])
                P.op("dve", lambda e: e.tensor_scalar(out=mask[qt][:, 0:n], in0=sc[:, 0:n], scalar1=th[:, 0:1], scalar2=None, op0=ALU.is_ge),
                     reads=[sc, th], writes=[mask[qt]])

        def front_b(s, qb, r):
            mT = maskT[r]
            for kt in range(4 * qb + 4):
                for qt in range(4):
                    if kt <= 4 * qb + qt:
                        P.op("pe", lambda e: e.transpose(out=tpx[:, qt * 128:(qt + 1) * 128], in_=mask[qt][:, kt * 128:(kt + 1) * 128],
                                                         identity=P.ident[:, :]), reads=[mask[qt], P.ident], writes=[tpx])
                P.op("act", lambda e: e.copy(out=mT[:, kt, :], in_=tpx[:, :]), reads=[tpx], writes=[mT])

        def back(s, qb, r):
            c0, c1 = qb * TB, (qb + 1) * TB
            mT = maskT[r]
            for h in range(4):
                A["acc"] = accs[st8["hcnt"] % 2]
                orr = st8["hcnt"] % 2
                st8["hcnt"] += 1
                attn_core(P, A, bqT[r][h][:, :], lambda kt: bkT[:, kt * 128:(kt + 1) * 128], lambda kt: bv1[:, kt, :],
                          qb, 128, 128 ** -0.5, [bqT[r][h], bkT, bv1], mask_of=lambda kt: (mT[:, kt, :], mT))
                for qt in range(4):
                    acc = A["acc"][qt // 2]
                    P.op("act", lambda e: e.activation(out=lnl[:, qt:qt + 1], in_=acc[:, qt % 2, 128:129], func=AF.Ln), reads=[acc], writes=[lnl])
                P.op("act", lambda e: e.activation(out=rc[:, :], in_=lnl[:, :], func=AF.Exp, scale=-1.0), reads=[lnl], writes=[rc])
                for qt in range(4):
                    acc = A["acc"][qt // 2]
                    P.op("act", lambda e: e.activation(out=ob[orr][:, qt, :], in_=acc[:, qt % 2, 0:128], func=AF.Identity, scale=rc[:, qt:qt + 1]),
                         reads=[acc, rc], writes=[ob[orr]])
                    P.op("pe", lambda e: e.transpose(out=tpx[:, qt * 128:(qt + 1) * 128], in_=ob[orr][:, qt, :], identity=P.ident[:, :]),
                         reads=[ob[orr], P.ident], writes=[tpx])
                P.op("act", lambda e: e.copy(out=oT[orr][:, :], in_=tpx[:, :]), reads=[tpx], writes=[oT[orr]])
                P.dma("sp", mixT[s, 512 + h * 128:512 + (h + 1) * 128, c0:c1], oT[orr][:, :], reads=[oT[orr]])

        blocks = [(s, qb) for s in range(n_s) for qb in range(n_qb)]
        load_idx_side(blocks[0][0])
        front_a(*blocks[0], 0)
        front_b(*blocks[0], 0)
        for i, (s, qb) in enumerate(blocks):
            if qb == 0:
                load_att_side(s)
            if i + 1 < len(blocks):
                s2, qb2 = blocks[i + 1]
                if qb2 == 0:
                    load_idx_side(s2)
                front_a(s2, qb2, (i + 1) % 2)
            back(s, qb, i % 2)
            if i + 1 < len(blocks):
                front_b(*blocks[i + 1], (i + 1) % 2)
    P.barrier()


O_Z, O_XBC, O_DT, O_MQ, O_MK, O_MV = 0, 1024, 2560, 2576, 3088, 3600


def l1_layout():
    fm_cols = list(range(O_XBC, O_XBC + 1536))
    types = [None] * 12
    for base in (O_MQ, O_MK):
        for p in range(2):
            A, B = pair_cols_h64(base, p)
            fm_cols += A + B
            types.append(0)
    tm_cols = list(range(O_Z, O_Z + 1024)) + list(range(O_MV, O_MV + 512)) + list(range(O_DT, O_DT + 16))
    return np.array(fm_cols), types, np.array(tm_cols)


def make_l1_cfg(T):
    def alloc(P, ctx):
        ex = {}
        ex["xb"] = [P.sb(ctx, f"xb{f}", [128, 3 + TB], F32) for f in range(12)]
        ex["cacc"] = [P.sb(ctx, f"cacc{r}", [128, TB], F32) for r in range(2)]
        ex["co"] = [P.sb(ctx, f"co{r}", [128, TB], BF16) for r in range(2)]
        ex["convw"] = P.sb(ctx, "convw", [128, 12, 4], F32)
        ex["convb"] = P.sb(ctx, "convb", [128, 12], F32)
        P.dma("sp", ex["convw"][:, :, :], T["convw"].rearrange("(f p) j -> p f j", p=128), writes=[ex["convw"]])
        P.dma("sp", ex["convb"][:, :], T["convb"], writes=[ex["convb"]])
        ex["fa"] = [P.sb(ctx, f"fa{r}", [128, TB], F32) for r in range(2)]
        ex["fb"] = [P.sb(ctx, f"fb{r}", [128, TB], F32) for r in range(2)]
        ex["km"] = [P.sb(ctx, f"km{r}", [128, 2, 2], F32) for r in range(2)]
        ex["tmo"] = [P.sb(ctx, f"tmo{r}", [128, 1536], BF16) for r in range(2)]
        ex["tmw"] = [P.sb(ctx, f"tmw{r}", [128, 16], F32) for r in range(2)]
        ex["cnt"] = 0
        ex["c2"] = 0
        ex["c3"] = 0
        return ex

    def plain(P, ex, s, tb, ft, ps):
        xb = ex["xb"][ft]
        k = ex["c2"] % 2
        ex["c2"] += 1
        acc, co = ex["cacc"][k], ex["co"][k]
        cw = ex["convw"]
        if tb == 0:
            P.op("pool", lambda e: e.memset(xb[:, 0:3], 0.0), writes=[xb])
        P.op("act", lambda e: e.copy(out=xb[:, 3:3 + TB], in_=ps[:, :]), reads=[ps], writes=[xb])
        P.op("dve", lambda e: e.tensor_scalar(out=acc[:, :], in0=xb[:, 0:TB], scalar1=cw[:, ft, 0:1], scalar2=None, op0=ALU.mult),
             reads=[xb, cw], writes=[acc])
        for j in range(1, 4):
            P.op("dve", lambda e: e.scalar_tensor_tensor(out=acc[:, :], in0=xb[:, j:j + TB], scalar=cw[:, ft, j:j + 1], in1=acc[:, :],
                                                         op0=ALU.mult, op1=ALU.add), reads=[xb, cw, acc], writes=[acc])
        P.op("act", lambda e: e.activation(out=co[:, :], in_=acc[:, :], func=AF.Silu, bias=ex["convb"][:, ft:ft + 1]),
             reads=[acc, ex["convb"]], writes=[co])
        P.op("pool", lambda e: e.tensor_copy(out=xb[:, 0:3], in_=xb[:, TB:TB + 3]), reads=[xb], writes=[xb])
        P.dma("sp", T["xbcT"][s, ft, :, tb * TB:(tb + 1) * TB], co[:, :], reads=[co])

    def rope_f32(pi):
        return True

    def rope_handler(P, ex, s, tb, pi, ft, t1, t2, t3, t4, oa, ob):
        k = ex["c3"] % 2
        ex["c3"] += 1
        fa, fb = ex["fa"][k], ex["fb"][k]
        P.op("pool", lambda e: e.tensor_tensor(out=fa[:, :], in0=t1[:, :], in1=t2[:, :], op=ALU.subtract), reads=[t1, t2], writes=[fa])
        P.op("pool", lambda e: e.tensor_tensor(out=fb[:, :], in0=t3[:, :], in1=t4[:, :], op=ALU.add), reads=[t3, t4], writes=[fb])
        P.op("act", lambda e: e.copy(out=oa[:, :], in_=fa[:, :]), reads=[fa], writes=[oa])
        P.op("act", lambda e: e.copy(out=ob[:, :], in_=fb[:, :]), reads=[fb], writes=[ob])
        pr = pi - 12
        if pr < 2:
            P.dma("sp", T["mqf"][s, 2 * pr, :, tb * TB:(tb + 1) * TB], fa[:, :], reads=[fa])
            P.dma("sp", T["mqf"][s, 2 * pr + 1, :, tb * TB:(tb + 1) * TB], fb[:, :], reads=[fb])
        else:
            km = ex["km"][k]
            P.op("dve", lambda e: e.tensor_reduce(out=km[:, 0, :], in_=fa[:, :].rearrange("p (b t) -> p b t", b=2), axis=AX.X, op=ALU.add),
                 reads=[fa], writes=[km])
            P.op("dve", lambda e: e.tensor_reduce(out=km[:, 1, :], in_=fb[:, :].rearrange("p (b t) -> p b t", b=2), axis=AX.X, op=ALU.add),
                 reads=[fb, km], writes=[km])
            P.op("dve", lambda e: e.tensor_scalar(out=km[:, :, :], in0=km[:, :, :], scalar1=1.0 / 256, scalar2=None, op0=ALU.mult),
                 reads=[km], writes=[km])
            q = pr - 2
            P.dma("sp", T["kmean"][s, 2 * q, :, 2 * tb:2 * tb + 2], km[:, 0, :], reads=[km])
            P.dma("sp", T["kmean"][s, 2 * q + 1, :, 2 * tb:2 * tb + 2], km[:, 1, :], reads=[km])

    def tm_handler(P, ex, s, tb, r, hnT, wtm, pst):
        for j in range(4):
            t0 = tb * TB + j * 128
            k = ex["cnt"] % 2
            ex["cnt"] += 1
            o, w = ex["tmo"][k], ex["tmw"][k]
            for grp in range(4):
                n0 = grp * 512
                nw = 512 if grp < 3 else 16
                pp = pst[grp % 2]
                for c in range(8):
                    P.op("pe", lambda e: e.matmul(pp[:, 0:nw], lhsT=hnT[:, c, j * 128:(j + 1) * 128], rhs=wtm[:, c, n0:n0 + nw],
                                                  start=(c == 0), stop=(c == 7)), reads=[hnT, wtm], writes=[pp])
                if grp < 3:
                    P.op("act", lambda e: e.copy(out=o[:, n0:n0 + 512], in_=pp[:, :]), reads=[pp], writes=[o])
                else:
                    P.op("act", lambda e: e.copy(out=w[:, :], in_=pp[:, 0:16]), reads=[pp], writes=[w])
            P.dma("sp", T["tm1"][s, t0:t0 + 128, :], o[:, :], reads=[o])
            P.dma("sp", T["dtraw"][s, :, t0 // 128, :], w[:, :], reads=[w])

    _, types1, _ = l1_layout()
    return dict(x=T["h2"], g=T["norms"][4], wfm=T["wfm1"], wtm=T["wtm1"], nfm=20, ntm=1552, types=types1, rope=T["rope"],
                fmT=T["fm1"], alloc=alloc, tm_handler=tm_handler, plain_handler=plain, rope_f32=rope_f32, rope_f32_handler=rope_handler)


BIGQ = 240000.0


def phase_moba_gate(P, nc, T):
    with ExitStack() as ctx:
        pm = P.sb(ctx, "pm", [128, 16, 16], F32)
        oh = P.sb(ctx, "oh", [128, 16, 16], F32)
        P.dma("sp", pm[:, :, :], T["pm"].partition_broadcast(128).rearrange("p (a b) -> p a b", a=16), writes=[pm])
        P.dma("sp", oh[:, :, :], T["oh"].partition_broadcast(128).rearrange("p (a b) -> p a b", a=16), writes=[oh])
        kmbd = P.sb(ctx, "kmbd", [128, 4, 64], F32)
        qf = [P.sb(ctx, f"qf{r}", [128, 4, TB], F32) for r in range(2)]
        g2 = P.sb(ctx, "g2", [128, 8, 16], F32)
        mx = P.sb(ctx, "mx", [128, 8, 8], F32)
        sel = P.sb(ctx, "sel", [128, 8, 16], F32)
        negm = [P.sb(ctx, f"negm{r}", [128, 128], BF16) for r in range(2)]
        nT = [P.sb(ctx, f"nT{r}", [128, TB], BF16) for r in range(2)]
        gps = [P.ps(ctx, f"gps{r}", [128, 128]) for r in range(2)]
        tpx = P.ps(ctx, "tpx", [128, TB], BF16)
        bi = 0
        k = 0
        for s in range(SPC):
            P.op("dve", lambda e: e.memset(kmbd[:, :, :], 0.0), writes=[kmbd])
            for f in range(4):
                for hl in range(4):
                    P.dma("sp", kmbd[32 * hl:32 * hl + 32, f, 16 * hl:16 * hl + 16], T["kmean"][s, f, 32 * hl:32 * hl + 32, :], writes=[kmbd])
            for tb in range(NTB):
                r = bi % 2
                bi += 1
                P.dma("sp", qf[r][:, :, :], T["mqf"][s, :, :, tb * TB:(tb + 1) * TB].rearrange("f p t -> p f t"), writes=[qf[r]])
                for j in range(4):
                    own = (tb * 4 + j) // 2
                    kk = k % 2
                    k += 1
                    gp = gps[kk]
                    for p in range(2):
                        for ab in range(2):
                            P.op("pe", lambda e: e.matmul(gp[:, p * 64:(p + 1) * 64], lhsT=qf[r][:, 2 * p + ab, j * 128:(j + 1) * 128],
                                                          rhs=kmbd[:, 2 * p + ab, :], start=(ab == 0), stop=(ab == 1)),
                                 reads=[qf[r], kmbd], writes=[gp])
                    P.op("dve", lambda e: e.tensor_tensor(out=g2[:, :, :], in0=gp[:, :].rearrange("p (h n) -> p h n", h=8),
                                                          in1=pm[:, own, :].unsqueeze(1).to_broadcast([128, 8, 16]), op=ALU.add),
                         reads=[gp, pm], writes=[g2])
                    for h in range(8):
                        P.op("dve", lambda e: e.max(out=mx[:, h, :], in_=g2[:, h, :]), reads=[g2], writes=[mx])
                    P.op("dve", lambda e: e.tensor_tensor(out=sel[:, :, :], in0=g2[:, :, :], in1=mx[:, :, 2:3].to_broadcast([128, 8, 16]),
                                                          op=ALU.is_ge), reads=[g2, mx], writes=[sel])
                    P.op("dve", lambda e: e.tensor_tensor(out=sel[:, :, :], in0=sel[:, :, :],
                                                          in1=oh[:, own, :].unsqueeze(1).to_broadcast([128, 8, 16]), op=ALU.max),
                         reads=[sel, oh], writes=[sel])
                    P.op("dve", lambda e: e.tensor_scalar(out=negm[kk][:, :].rearrange("p (h n) -> p h n", h=8), in0=sel[:, :, :],
                                                          scalar1=-1.0, scalar2=BIGQ, op0=ALU.add, op1=ALU.mult), reads=[sel], writes=[negm[kk]])
                    P.op("pe", lambda e: e.transpose(out=tpx[:, j * 128:(j + 1) * 128], in_=negm[kk][:, :], identity=P.ident[:, :]),
                         reads=[negm[kk], P.ident], writes=[tpx])
                P.op("act", lambda e: e.copy(out=nT[r][:, :], in_=tpx[:, :]), reads=[tpx], writes=[nT[r]])
                P.dma("sp", T["negT"][s, :, tb * TB:(tb + 1) * TB], nT[r][:, :], reads=[nT[r]])
    P.barrier()


def phase_moba_attn(P, nc, T):
    fm, tm1, mixT = T["fm1"], T["tm1"], T["mixT1"]
    with ExitStack() as ctx:
        tri = P.sb(ctx, "tri", [128, 128], BF16)
        P.dma("sp", tri[:, :], T["tri"], writes=[tri])
        ka = [P.sb(ctx, f"ka{r}", [80, S], BF16) for r in range(4)]
        v1 = [P.sb(ctx, f"v1{r}", [128, 32, 65], BF16) for r in range(4)]
        for r in range(4):
            P.op("pool", lambda e: e.memset(v1[r][:, :, 64:65], 1.0), writes=[v1[r]])
            P.dma("sp", ka[r][64:80, :], T["blk1h"], writes=[ka[r]])
        qa = [P.sb(ctx, f"qa{r}", [80, TB], BF16) for r in range(4)]
        A = {"st": [P.ps(ctx, f"st{r}", [128, TB]) for r in range(2)],
             "pt": [P.sb(ctx, f"pt{r}", [128, TB], BF16) for r in range(3)], "i": 0}
        accs = [[P.ps(ctx, f"acc{m}_{r}", [128, 2, 256]) for r in range(2)] for m in range(2)]
        tpx = P.ps(ctx, "tpx", [128, TB], BF16)
        rc = P.sb(ctx, "rc", [128, 4], F32)
        ob = [P.sb(ctx, f"ob{r}", [128, 4, 128], BF16) for r in range(2)]
        oT = [P.sb(ctx, f"oT{r}", [128, TB], BF16) for r in range(2)]
        hpi = 0
        qi = 0
        for s in range(SPC):
            for hp in range(4):
                kb = 2 * (hpi % 2)
                hpi += 1
                for hh in range(2):
                    h = 2 * hp + hh
                    p, hl = h // 4, h % 4
                    P.dma("sp", ka[kb + hh][0:32, :], fm[s, 16 + 2 * p, 32 * hl:32 * hl + 32, :], writes=[ka[kb + hh]])
                    P.dma("sp", ka[kb + hh][32:64, :], fm[s, 17 + 2 * p, 32 * hl:32 * hl + 32, :], writes=[ka[kb + hh]])
                    for half in range(2):
                        P.dma("sp", v1[kb + hh][:, 16 * half:16 * half + 16, 0:64],
                              tm1[s, 2048 * half:2048 * (half + 1), 1024 + h * 64:1024 + (h + 1) * 64].rearrange("(kt p) e -> p kt e", p=128),
                              writes=[v1[kb + hh]])
                for qb in range(NTB):
                    qr = 2 * (qi % 2)
                    orr = qi % 2
                    qi += 1
                    c0, c1 = qb * TB, (qb + 1) * TB
                    for hh in range(2):
                        h = 2 * hp + hh
                        p, hl = h // 4, h % 4
                        q = qa[qr + hh]
                        P.dma("sp", q[0:32, :], fm[s, 12 + 2 * p, 32 * hl:32 * hl + 32, c0:c1], writes=[q])
                        P.dma("sp", q[32:64, :], fm[s, 13 + 2 * p, 32 * hl:32 * hl + 32, c0:c1], writes=[q])
                        P.dma("sp", q[64:80, :], T["negT"][s, h * 16:(h + 1) * 16, c0:c1], writes=[q])
                    for hh in range(2):
                        q = qa[qr + hh]
                        kk, vv = ka[kb + hh], v1[kb + hh]
                        A["acc"] = accs[hh]
                        attn_core(P, A, q[0:80, :], lambda kt: kk[0:80, kt * 128:(kt + 1) * 128], lambda kt: vv[:, kt, :],
                                  qb, 64, 0.125, [q, kk, vv], tri=tri)
                        for qt in range(4):
                            acc = A["acc"][qt // 2]
                            P.op("dve", lambda e: e.reciprocal(out=rc[:, qt:qt + 1], in_=acc[:, qt % 2, 64:65]), reads=[acc], writes=[rc])
                            P.op("dve", lambda e: e.tensor_scalar(out=ob[orr][:, qt, hh * 64:(hh + 1) * 64], in0=acc[:, qt % 2, 0:64],
                                                                  scalar1=rc[:, qt:qt + 1], scalar2=None, op0=ALU.mult),
                                 reads=[acc, rc], writes=[ob[orr]])
                    for qt in range(4):
                        P.op("pe", lambda e: e.transpose(out=tpx[:, qt * 128:(qt + 1) * 128], in_=ob[orr][:, qt, :], identity=P.ident[:, :]),
                             reads=[ob[orr], P.ident], writes=[tpx])
                    P.op("act", lambda e: e.copy(out=oT[orr][:, :], in_=tpx[:, :]), reads=[tpx], writes=[oT[orr]])
                    P.dma("sp", mixT[s, 1024 + hp * 128:1024 + (hp + 1) * 128, c0:c1], oT[orr][:, :], reads=[oT[orr]])
    P.barrier()


def v3(ap2d, a):
    return ap2d.rearrange("p (a b) -> p a b", a=a)


def phase_ssd(P, nc, T):
    xbcT, tm1, mixT = T["xbcT"], T["tm1"], T["mixT1"]
    dbg = T.get("dbg", {})
    n_s2, n_ch = dbg.get("n_s", SPC), dbg.get("n_ch", 32)
    upto = dbg.get("upto", 99)
    pre = dbg.get("pre", 99)
    pre3 = dbg.get("pre3", 99)
    with ExitStack() as ctx:
        tri = P.sb(ctx, "tri", [128, 128], BF16)
        P.dma("sp", tri[:, :], T["tri"], writes=[tri])
        ones = P.sb(ctx, "ones", [128, 128], BF16)
        dsp = [P.sb(ctx, f"dsp{i}", [128, 512], BF16) for i in range(3)]
        dres = P.sb(ctx, "dres", [128, 512], F32)
        P.op("dve", lambda e: e.memset(ones[:, :], 1.0), writes=[ones])
        gn = P.sb(ctx, "gn", [128, 1024], F32)
        P.dma("sp", gn[:, :], T["ssm_norm"].partition_broadcast(128), writes=[gn])
        dsk = P.sb(ctx, "dsk", [128, 1024], F32)
        P.dma("sp", dsk[:, :], T["dsk"].partition_broadcast(128), writes=[dsk])
        dtb = P.sb(ctx, "dtb", [128, 16], F32)
        P.dma("sp", dtb[:, :], T["dt_bias"].partition_broadcast(128), writes=[dtb])
        abc_ = P.sb(ctx, "a_bc", [128, 16], F32)
        P.dma("sp", abc_[:, :], T["a_log"].partition_broadcast(128), writes=[abc_])
        P.op("act", lambda e: e.activation(out=abc_[:, :], in_=abc_[:, :], func=AF.Exp), reads=[abc_], writes=[abc_])
        P.op("dve", lambda e: e.tensor_scalar(out=abc_[:, :], in0=abc_[:, :], scalar1=-1.0, scalar2=None, op0=ALU.mult), reads=[abc_], writes=[abc_])
        dt_all = P.sb(ctx, "dt_all", [128, 512], F32)
        da_all = P.sb(ctx, "da_all", [128, 512], F32)
        nac = P.sb(ctx, "nac", [128, 512], F32)
        eac = P.sb(ctx, "eac", [128, 512], F32)
        eal = P.sb(ctx, "eal", [128, 512], F32)
        state = P.sb(ctx, "state", [128, 1024], F32)
        stt = P.sb(ctx, "stt", [128, 1024], F32)
        state_bf = P.sb(ctx, "state_bf", [128, 1024], BF16)
        Rm = [P.sb(ctx, f"Rm{i}", [128, 2048], BF16) for i in range(2)]
        exa = P.sb(ctx, "exa", [128, 2048], F32)
        dec = P.sb(ctx, "dec", [128, 2048], F32)
        cbm = P.sb(ctx, "cbm", [128, 256], BF16)
        scT = P.sb(ctx, "scT", [128, 2048], BF16)
        xT = [P.sb(ctx, f"xT{r}", [128, 8, 128], BF16) for r in range(2)]
        bcT = [P.sb(ctx, f"bcT{r}", [128, 4, 128], BF16) for r in range(2)]
        zt = [P.sb(ctx, f"zt{r}", [128, 1024], BF16) for r in range(2)]
        xtm = P.sb(ctx, "xtm", [128, 1024], BF16)
        xdt = P.sb(ctx, "xdt", [128, 1024], BF16)
        xdtt = P.sb(ctx, "xdtt", [128, 1024], BF16)
        btm = P.sb(ctx, "btm", [128, 256], BF16)
        ytmp = P.sb(ctx, "ytmp", [128, 1024], F32)
        y = P.sb(ctx, "y", [128, 1024], F32)
        t2 = P.sb(ctx, "t2", [128, 1024], F32)
        sz = P.sb(ctx, "sz", [128, 1024], F32)
        yn = P.sb(ctx, "yn", [128, 1024], BF16)
        junk = P.sb(ctx, "junk", [128, 512], BF16)
        ss = P.sb(ctx, "ss", [128, 2], F32)
        lnv = P.sb(ctx, "lnv", [128, 2], F32)
        rstd = P.sb(ctx, "rstd", [128, 2], F32)
        yT = [P.sb(ctx, f"yT{r}", [128, 8, TB], BF16) for r in range(2)]
        abc = P.ps(ctx, "abc", [128, 1024])
        cbp = P.ps(ctx, "cbp", [128, 512])
        tpb = P.ps(ctx, "tpb", [128, 1024], BF16)
        yps = [P.ps(ctx, f"yps{g}", [128, 512]) for g in range(2)]
        yip = [P.ps(ctx, f"yip{g}", [128, 512]) for g in range(2)]
        ci = 0
        for s in range(n_s2):
            if pre >= 1:
                P.dma("sp", dt_all[:, :], T["dtraw"][s].rearrange("p c h -> p (c h)"), writes=[dt_all])
            if pre >= 1:
                P.op("dve", lambda e: e.tensor_tensor(out=v3(dt_all[:, :], 32), in0=v3(dt_all[:, :], 32),
                                                      in1=dtb[:, :].unsqueeze(1).to_broadcast([128, 32, 16]), op=ALU.add), reads=[dt_all, dtb], writes=[dt_all])
            if pre >= 2:
                P.op("act", lambda e: e.activation(out=dt_all[:, :], in_=dt_all[:, :], func=AF.Exp), reads=[dt_all], writes=[dt_all])
            if pre >= 2:
                P.op("act", lambda e: e.activation(out=dt_all[:, :], in_=dt_all[:, :], func=AF.Ln, bias=P.one_t[:, 0:1]), reads=[dt_all, P.one_t], writes=[dt_all])
            if pre >= 2:
                P.op("dve", lambda e: e.tensor_tensor(out=v3(da_all[:, :], 32), in0=v3(dt_all[:, :], 32),
                                                      in1=abc_[:, :].unsqueeze(1).to_broadcast([128, 32, 16]), op=ALU.mult), reads=[dt_all, abc_], writes=[da_all])
            if pre >= 3:
                if pre3 >= 1:
                    P.op("dve", lambda e: e.tensor_copy(out=dsp[0][:, :], in_=da_all[:, :]), reads=[da_all], writes=[dsp[0]])
                if pre3 >= 2:
                    P.op("dve", lambda e: e.tensor_tensor(out=dres[:, :], in0=da_all[:, :], in1=dsp[0][:, :], op=ALU.subtract), reads=[da_all, dsp[0]], writes=[dres])
                if pre3 >= 3:
                    P.op("dve", lambda e: e.tensor_copy(out=dsp[1][:, :], in_=dres[:, :]), reads=[dres], writes=[dsp[1]])
                if pre3 >= 4:
                    P.op("dve", lambda e: e.tensor_tensor(out=dres[:, :], in0=dres[:, :], in1=dsp[1][:, :], op=ALU.subtract), reads=[dres, dsp[1]], writes=[dres])
                if pre3 >= 5:
                    P.op("dve", lambda e: e.tensor_copy(out=dsp[2][:, :], in_=dres[:, :]), reads=[dres], writes=[dsp[2]])
                if pre3 >= 6:
                    for i in range(3):
                        P.op("pe", lambda e: e.matmul(yps[0][:, :], lhsT=tri[:, :], rhs=dsp[i][:, :], start=(i == 0), stop=(i == 2)), reads=[tri, dsp[i]], writes=[yps[0]])
                if pre3 >= 7:
                    P.op("dve", lambda e: e.tensor_scalar(out=nac[:, :], in0=yps[0][:, :], scalar1=-1.0, scalar2=None, op0=ALU.mult), reads=[yps[0]], writes=[nac])
                if pre3 >= 8:
                    P.op("act", lambda e: e.activation(out=eac[:, :], in_=nac[:, :], func=AF.Exp, scale=-1.0), reads=[nac], writes=[eac])
            if pre >= 4:
                for i in range(3):
                    P.op("pe", lambda e: e.matmul(yps[1][:, :], lhsT=ones[:, :], rhs=dsp[i][:, :], start=(i == 0), stop=(i == 2)), reads=[ones, dsp[i]], writes=[yps[1]])
                P.op("dve", lambda e: e.tensor_copy(out=eal[:, :], in_=yps[1][:, :]), reads=[yps[1]], writes=[eal])
                P.op("act", lambda e: e.activation(out=eal[:, :], in_=eal[:, :], func=AF.Exp), reads=[eal], writes=[eal])
            if pre >= 5:
                P.op("dve", lambda e: e.memset(state[:, :], 0.0), writes=[state])
            if pre >= 5:
                P.op("pool", lambda e: e.memset(state_bf[:, :], 0.0), writes=[state_bf])

            def loads(ch, r):
                t0 = ch * 128
                P.dma("sp", xT[r][:, :, :], xbcT[s, 0:8, :, t0:t0 + 128].rearrange("f p t -> p f t"), writes=[xT[r]])
                P.dma("sp", bcT[r][:, :, :], xbcT[s, 8:12, :, t0:t0 + 128].rearrange("f p t -> p f t"), writes=[bcT[r]])
                P.dma("sp", zt[r][:, :], tm1[s, t0:t0 + 128, 0:1024], writes=[zt[r]])

            if pre >= 6:
                loads(0, ci % 2)
            for ch in range(n_ch if pre >= 6 else 0):
                r = ci % 2
                ci += 1
                if ch + 1 < n_ch:
                    loads(ch + 1, ci % 2)
                c16 = slice(ch * 16, ch * 16 + 16)
                if upto < 1:
                    continue
                for g in range(2):
                    P.op("pe", lambda e: e.matmul(cbp[:, g * 128:(g + 1) * 128], lhsT=bcT[r][:, g, :], rhs=bcT[r][:, 2 + g, :], start=True, stop=True),
                         reads=[bcT[r]], writes=[cbp])
                P.op("dve", lambda e: e.tensor_tensor(out=v3(cbm[:, :], 2), in0=v3(cbp[:, 0:256], 2),
                                                      in1=tri[:, :].unsqueeze(1).to_broadcast([128, 2, 128]), op=ALU.mult), reads=[cbp, tri], writes=[cbm])
                if upto < 2:
                    continue
                for i in range(2):
                    P.op("dve", lambda e: e.tensor_tensor(out=v3(Rm[i][:, :], 16), in0=tri[:, :].unsqueeze(1).to_broadcast([128, 16, 128]),
                                                          in1=dsp[i][:, c16].unsqueeze(2).to_broadcast([128, 16, 128]), op=ALU.mult),
                         reads=[tri, dsp[i]], writes=[Rm[i]])
                for g in range(2):
                    for q in range(2):
                        for i in range(2):
                            P.op("pe", lambda e: e.matmul(abc[:, q * 512:(q + 1) * 512], lhsT=ones[:, :],
                                                          rhs=Rm[i][:, g * 1024 + q * 512:g * 1024 + (q + 1) * 512],
                                                          start=(i == 0), stop=(i == 1)), reads=[ones, Rm[i]], writes=[abc])
                    for hl in range(8):
                        h = 8 * g + hl
                        P.op("dve", lambda e: e.tensor_scalar(out=exa[:, h * 128:(h + 1) * 128], in0=abc[:, hl * 128:(hl + 1) * 128],
                                                              scalar1=nac[:, ch * 16 + h:ch * 16 + h + 1], scalar2=None, op0=ALU.add),
                             reads=[abc, nac], writes=[exa])
                if upto < 3:
                    continue
                P.op("dve", lambda e: e.tensor_scalar(out=exa[:, :], in0=exa[:, :], scalar1=0.0, scalar2=None, op0=ALU.min), reads=[exa], writes=[exa])
                P.op("act", lambda e: e.activation(out=dec[:, :], in_=exa[:, :], func=AF.Exp), reads=[exa], writes=[dec])
                if upto < 4:
                    continue
                for g in range(2):
                    P.op("dve", lambda e: e.tensor_tensor(out=v3(scT[:, g * 1024:(g + 1) * 1024], 8), in0=v3(dec[:, g * 1024:(g + 1) * 1024], 8),
                                                          in1=cbm[:, g * 128:(g + 1) * 128].unsqueeze(1).to_broadcast([128, 8, 128]), op=ALU.mult),
                         reads=[dec, cbm], writes=[scT])
                if upto < 5:
                    continue
                for f in range(8):
                    P.op("pe", lambda e: e.transpose(out=tpb[:, f * 128:(f + 1) * 128], in_=xT[r][:, f, :], identity=P.ident[:, :]),
                         reads=[xT[r], P.ident], writes=[tpb])
                P.op("act", lambda e: e.copy(out=xtm[:, :], in_=tpb[:, :]), reads=[tpb], writes=[xtm])
                P.op("dve", lambda e: e.tensor_tensor(out=v3(xdt[:, :], 16), in0=v3(xtm[:, :], 16),
                                                      in1=dt_all[:, c16].unsqueeze(2).to_broadcast([128, 16, 64]), op=ALU.mult),
                     reads=[xtm, dt_all], writes=[xdt])
                if upto < 6:
                    continue
                for h in range(16):
                    P.op("pe", lambda e: e.matmul(yps[h // 8][:, (h % 8) * 64:(h % 8 + 1) * 64], lhsT=scT[:, h * 128:(h + 1) * 128],
                                                  rhs=xdt[:, h * 64:(h + 1) * 64], start=True, stop=True), reads=[scT, xdt], writes=[yps[h // 8]])
                for g in range(2):
                    P.op("pe", lambda e: e.matmul(yip[g][:, :], lhsT=bcT[r][:, 2 + g, :], rhs=state_bf[:, g * 512:(g + 1) * 512], start=True, stop=True),
                         reads=[bcT[r], state_bf], writes=[yip[g]])
                if upto < 7:
                    continue
                for g in range(2):
                    hs = slice(g * 512, (g + 1) * 512)
                    P.op("dve", lambda e: e.tensor_tensor(out=v3(ytmp[:, hs], 8), in0=v3(yip[g][:, :], 8),
                                                          in1=eac[:, ch * 16 + 8 * g:ch * 16 + 8 * g + 8].unsqueeze(2).to_broadcast([128, 8, 64]), op=ALU.mult),
                         reads=[yip[g], eac], writes=[ytmp])
                    P.op("dve", lambda e: e.tensor_tensor(out=y[:, hs], in0=yps[g][:, :], in1=ytmp[:, hs], op=ALU.add), reads=[yps[g], ytmp], writes=[y])
                if upto < 8:
                    continue
                P.op("pool", lambda e: e.tensor_tensor(out=t2[:, :], in0=xtm[:, :], in1=dsk[:, :], op=ALU.mult), reads=[xtm, dsk], writes=[t2])
                P.op("pool", lambda e: e.tensor_tensor(out=y[:, :], in0=y[:, :], in1=t2[:, :], op=ALU.add), reads=[y, t2], writes=[y])
                P.op("act", lambda e: e.activation(out=sz[:, :], in_=zt[r][:, :], func=AF.Silu), reads=[zt[r]], writes=[sz])
                P.op("dve", lambda e: e.tensor_tensor(out=y[:, :], in0=y[:, :], in1=sz[:, :], op=ALU.mult), reads=[y, sz], writes=[y])
                if upto < 9:
                    continue
                for g in range(2):
                    P.op("act", lambda e: e.activation(out=junk[:, :], in_=y[:, g * 512:(g + 1) * 512], func=AF.Square, accum_out=ss[:, g:g + 1]),
                         reads=[y], writes=[junk, ss])
                rstd_from_ss(P, ss, lnv, rstd, 2, 512)
                for g in range(2):
                    hs = slice(g * 512, (g + 1) * 512)
                    P.op("dve", lambda e: e.scalar_tensor_tensor(out=yn[:, hs], in0=y[:, hs], scalar=rstd[:, g:g + 1], in1=gn[:, hs],
                                                                 op0=ALU.mult, op1=ALU.mult), reads=[y, rstd, gn], writes=[yn])
                if upto < 10:
                    continue
                P.op("dve", lambda e: e.tensor_tensor(out=v3(xdtt[:, :], 16), in0=v3(xdt[:, :], 16),
                                                      in1=v3(dec[:, :], 16)[:, :, 127:128].to_broadcast([128, 16, 64]), op=ALU.mult),
                     reads=[xdt, dec], writes=[xdtt])
                for g in range(2):
                    P.op("pe", lambda e: e.transpose(out=tpb[:, g * 128:(g + 1) * 128], in_=bcT[r][:, g, :], identity=P.ident[:, :]),
                         reads=[bcT[r], P.ident], writes=[tpb])
                P.op("act", lambda e: e.copy(out=btm[:, :], in_=tpb[:, 0:256]), reads=[tpb], writes=[btm])
                for g in range(2):
                    P.op("pe", lambda e: e.matmul(yip[g][:, :], lhsT=btm[:, g * 128:(g + 1) * 128], rhs=xdtt[:, g * 512:(g + 1) * 512], start=True, stop=True),
                         reads=[btm, xdtt], writes=[yip[g]])
                for g in range(2):
                    hs = slice(g * 512, (g + 1) * 512)
                    P.op("pool", lambda e: e.tensor_tensor(out=v3(stt[:, hs], 8), in0=v3(state[:, hs], 8),
                                                           in1=eal[:, ch * 16 + 8 * g:ch * 16 + 8 * g + 8].unsqueeze(2).to_broadcast([128, 8, 64]), op=ALU.mult),
                         reads=[state, eal], writes=[stt])
                    P.op("dve", lambda e: e.tensor_tensor(out=state[:, hs], in0=yip[g][:, :], in1=stt[:, hs], op=ALU.add), reads=[yip[g], stt], writes=[state])
                P.op("act", lambda e: e.copy(out=state_bf[:, :], in_=state[:, :]), reads=[state], writes=[state_bf])
                if upto < 11:
                    continue
                yr = (ci // 4) % 2 if False else ((s * 32 + ch) // 4) % 2
                for f in range(8):
                    P.op("pe", lambda e: e.transpose(out=tpb[:, f * 128:(f + 1) * 128], in_=yn[:, f * 128:(f + 1) * 128], identity=P.ident[:, :]),
                         reads=[yn, P.ident], writes=[tpb])
                P.op("act", lambda e: e.copy(out=yT[yr][:, :, (ch % 4) * 128:(ch % 4 + 1) * 128], in_=v3(tpb[:, :], 8)), reads=[tpb], writes=[yT[yr]])
                if ch % 4 == 3:
                    tb = ch // 4
                    P.dma("sp", mixT[s, 0:1024, tb * TB:(tb + 1) * TB].rearrange("(f p) t -> p f t", p=128), yT[yr][:, :, :], reads=[yT[yr]])
    P.barrier()
```

```python
from contextlib import ExitStack
import math
import numpy as np
import ml_dtypes
import concourse.bass as bass
import concourse.mybir as mybir
from concourse.bass_utils import run_bass_kernel_spmd

F32 = mybir.dt.float32
BF16 = mybir.dt.bfloat16
AF = mybir.ActivationFunctionType
ALU = mybir.AluOpType
AX = mybir.AxisListType

NCORES = 8
SPC = 2
S = 4096
D = 1024
DFF = 2816
NFT = DFF // 128
EPS = 1e-6
TB = 512
NTB = S // TB
NEG = -60000.0


class Buf:
    __slots__ = ("name", "w", "r")

    def __init__(self, name=""):
        self.name = name
        self.w = None
        self.r = {}


class Tile:
    __slots__ = ("t", "b")

    def __init__(self, t, name):
        self.t = t
        self.b = Buf(name)

    def __getitem__(self, idx):
        return self.t[idx]


class Prog:
    ENG = ("pe", "act", "dve", "pool", "sp")

    def __init__(self, nc, stack):
        self.nc = nc
        self.stack = stack
        self.eobj = {"pe": nc.tensor, "act": nc.scalar, "dve": nc.vector,
                     "pool": nc.gpsimd, "sp": nc.sync}
        self.esem = {}
        self.ecnt = {}
        for e in ("pe", "act", "dve", "pool"):
            self.esem[e] = stack.enter_context(nc.semaphore("s_" + e))
            self.ecnt[e] = 0
        self.waited = {e: {} for e in self.ENG}
        self.rings = {}
        for q, n in (("sp", 24), ("pool", 12), ("act", 8)):
            sems = [stack.enter_context(nc.semaphore(f"r_{q}{i}")) for i in range(n)]
            self.rings[q] = {"sems": sems, "n": 0}
        self.ninstr = 0

    def _need(self, eng, toks):
        best = {}
        for tk in toks:
            if tk is None:
                continue
            key, sem, val = tk
            if key == "pe" and eng == "pe":
                continue
            if self.waited[eng].get(key, 0) >= val:
                continue
            if key not in best or best[key][2] < val:
                best[key] = tk
        for key, (k, sem, val) in best.items():
            self.eobj[eng].wait_ge(sem, val)
            self.waited[eng][key] = val
            self.ninstr += 1

    def _deps(self, reads, writes):
        toks = []
        for b in reads:
            toks.append(b.w)
        for b in writes:
            toks.append(b.w)
            toks.extend(b.r.values())
        return toks

    def _record(self, tok, reads, writes):
        for b in reads:
            old = b.r.get(tok[0])
            if old is None or old[2] < tok[2]:
                b.r[tok[0]] = tok
        for b in writes:
            b.w = tok
            b.r = {}

    @staticmethod
    def _bufs(lst):
        return [x.b if isinstance(x, Tile) else x for x in lst]

    def op(self, eng, fn, reads=(), writes=()):
        reads = self._bufs(reads)
        writes = self._bufs(writes)
        self._need(eng, self._deps(reads, writes))
        ins = fn(self.eobj[eng])
        self.ecnt[eng] += 1
        ins.then_inc(self.esem[eng], 1)
        tok = (eng, self.esem[eng], self.ecnt[eng])
        self._record(tok, reads, writes)
        self.ninstr += 1
        return ins

    def dma(self, q, out, in_, reads=(), writes=(), **kw):
        reads = self._bufs(reads)
        writes = self._bufs(writes)
        ring = self.rings[q]
        n = ring["n"]
        R = len(ring["sems"])
        sem = ring["sems"][n % R]
        key = f"ring_{q}{n % R}"
        prev = 16 * (n // R)
        toks = self._deps(reads, writes)
        if prev > 0:
            toks.append((key, sem, prev))
        self._need(q, toks)
        ins = self.eobj[q].dma_start(out=out, in_=in_, **kw)
        ins.then_inc(sem, 16)
        ring["n"] = n + 1
        tok = (key, sem, prev + 16)
        self._record(tok, reads, writes)
        self.ninstr += 1
        return ins

    def barrier(self):
        toks = []
        for e in ("pe", "act", "dve", "pool"):
            if self.ecnt[e] > 0:
                toks.append((e, self.esem[e], self.ecnt[e]))
        for q, ring in self.rings.items():
            R = len(ring["sems"])
            for i in range(min(R, ring["n"])):
                cnt = (ring["n"] - 1 - i) // R + 1
                toks.append((f"ring_{q}{i}", ring["sems"][i], 16 * cnt))
        for e in self.ENG:
            self._need(e, toks)

    def sb(self, ctx, name, shape, dt):
        self.uid = getattr(self, "uid", 0) + 1
        name = f"sb{self.uid}_{name}"
        return Tile(ctx.enter_context(self.nc.sbuf_tensor(name, list(shape), dt)), name)

    def ps(self, ctx, name, shape, dt=F32):
        self.uid = getattr(self, "uid", 0) + 1
        name = f"ps{self.uid}_{name}"
        return Tile(ctx.enter_context(self.nc.psum_tensor(name, list(shape), dt)), name)


def bcast_rows(ap_1d, nparts):
    return ap_1d.partition_broadcast(nparts)


def _rope_np(dim):
    inv = (np.float32(10000.0) ** (-np.arange(0, dim, 2, dtype=np.float32) / np.float32(dim))).astype(np.float32)
    ang = (np.arange(S, dtype=np.float32)[:, None] * inv[None, :]).astype(np.float32)
    return np.cos(ang).astype(np.float32).T.copy(), np.sin(ang).astype(np.float32).T.copy()


def rope_tables_host():
    c64, s64 = _rope_np(64)
    c128, s128 = _rope_np(128)
    tab = np.zeros((3, 2, 128, S), np.float32)
    tab[0, 0] = np.tile(c64, (4, 1)); tab[0, 1] = np.tile(s64, (4, 1))
    tab[1, 0] = np.tile(c128, (2, 1)); tab[1, 1] = np.tile(s128, (2, 1))
    tab[2, 0, 0:64] = c128; tab[2, 1, 0:64] = s128
    tab[2, 0, 64:96] = c64; tab[2, 1, 64:96] = s64
    return tab


def pair_cols_h64(base, p):
    A = [base + (4 * p + m) * 64 + d for m in range(4) for d in range(32)]
    return A, [c + 32 for c in A]


def pair_cols_h128(base, p):
    A = [base + (2 * p + m) * 128 + d for m in range(2) for d in range(64)]
    return A, [c + 64 for c in A]


E_AQ, E_AK, E_AV, E_BQ, E_BK, E_BV, E_IQ, E_IK, E_IW = 0, 512, 1024, 1536, 2048, 2176, 2304, 2816, 2880


def l0_layout():
    fm_cols, types = [], []
    for base in (E_AQ, E_AK, E_IQ):
        for p in range(2):
            A, B = pair_cols_h64(base, p)
            fm_cols += A + B
            types.append(0)
    for p in range(2):
        A, B = pair_cols_h128(E_BQ, p)
        fm_cols += A + B
        types.append(1)
    A = [E_BK + d for d in range(64)] + [E_IK + d for d in range(32)] + [0] * 32
    B = [E_BK + 64 + d for d in range(64)] + [E_IK + 32 + d for d in range(32)] + [0] * 32
    fm_cols += A + B
    types.append(2)
    tm_cols = list(range(E_AV, E_AV + 512)) + list(range(E_BV, E_BV + 128)) + list(range(E_IW, E_IW + 8))
    return np.array(fm_cols), types, np.array(tm_cols)


def load_weight_bf16(P, ctx, name, w_ap, K, N, wt=None):
    kc = K // 128
    if wt is None:
        wt = P.sb(ctx, name, [128, kc, N], BF16)
    src = w_ap.rearrange("(c p) n -> p c n", p=128)
    step = max(1, 2048 // N) if N <= 2048 else 1
    for c0 in range(0, kc, step):
        c1 = min(kc, c0 + step)
        if N <= 2048:
            P.dma("pool", wt[:, c0:c1, :], src[:, c0:c1, :], writes=[wt])
        else:
            for n0 in range(0, N, 2048):
                n1 = min(N, n0 + 2048)
                P.dma("pool", wt[:, c0:c1, n0:n1], src[:, c0:c1, n0:n1], writes=[wt])
    return wt


def rms_block(P, xt, ss, junk, lnv, rstd, ntile):
    for j in range(ntile):
        P.op("act", lambda e, j=j: e.activation(out=junk[:, :], in_=xt[j][:, :], func=AF.Square,
                                                  accum_out=ss[:, j:j + 1]),
             reads=[xt[j]], writes=[junk, ss])
    P.op("act", lambda e: e.activation(out=lnv[:, 0:ntile], in_=ss[:, 0:ntile], func=AF.Ln,
                                        scale=1.0 / D, bias=P.eps_t[:, 0:1]),
         reads=[ss, P.eps_t], writes=[lnv])
    P.op("act", lambda e: e.activation(out=rstd[:, 0:ntile], in_=lnv[:, 0:ntile], func=AF.Exp, scale=-0.5),
         reads=[lnv], writes=[rstd])


def phase_inproj(P, nc, cfg):
    x_ap, g_ap = cfg["x"], cfg["g"]
    nfm, ntm = cfg["nfm"], cfg["ntm"]
    with ExitStack() as ctx:
        wfm = load_weight_bf16(P, ctx, "wfm", cfg["wfm"], D, nfm * 128)
        wtm = load_weight_bf16(P, ctx, "wtm", cfg["wtm"], D, ntm)
        gbc = P.sb(ctx, "gbc", [128, D], F32)
        P.dma("sp", gbc[:, :], g_ap.partition_broadcast(128), writes=[gbc])
        xt = [[P.sb(ctx, f"xt{r}_{j}", [128, D], F32) for j in range(4)] for r in range(2)]
        hn = [P.sb(ctx, f"hn{j}", [128, D], BF16) for j in range(4)]
        hnT = [P.sb(ctx, f"hnT{r}", [128, 8, TB], BF16) for r in range(2)]
        junk = P.sb(ctx, "junk", [128, D], BF16)
        ss = [P.sb(ctx, f"ss{r}", [128, 4], F32) for r in range(2)]
        lnv = P.sb(ctx, "lnv", [128, 4], F32)
        rstd = [P.sb(ctx, f"rstd{r}", [128, 4], F32) for r in range(2)]
        ntab = len(set(t for t in cfg["types"] if t is not None))
        tabs = {}
        for ty in sorted(set(t for t in cfg["types"] if t is not None)):
            tabs[ty] = [[P.sb(ctx, f"tab{ty}_{cs}_{r}", [128, TB], F32) for cs in range(2)] for r in range(2)]
        tmp = [[P.sb(ctx, f"rt{r}_{i}", [128, TB], F32) for i in range(4)] for r in range(2)]
        oA = [P.sb(ctx, f"oA{r}", [128, TB], BF16) for r in range(3)]
        oB = [P.sb(ctx, f"oB{r}", [128, TB], BF16) for r in range(3)]
        tp = [P.ps(ctx, f"tp{r}", [128, TB], BF16) for r in range(2)]
        psA = [P.ps(ctx, f"psA{r}", [128, TB]) for r in range(2)]
        psB = [P.ps(ctx, f"psB{r}", [128, TB]) for r in range(2)]
        pst = [P.ps(ctx, f"pst{r}", [128, TB]) for r in range(2)]
        extra = cfg["alloc"](P, ctx) if "alloc" in cfg else None

        blocks = [(s, tb) for s in range(SPC) for tb in range(NTB)]

        def issue_loads(bi):
            s, tb = blocks[bi]
            r = bi % 2
            for j in range(4):
                t0 = tb * TB + j * 128
                P.dma("sp", xt[r][j][:, :], x_ap[s, t0:t0 + 128, :], writes=[xt[r][j]])
            for ty, tt in tabs.items():
                for cs in range(2):
                    P.dma("sp", tt[r][cs][:, :], cfg["rope"][ty, cs, :, tb * TB:(tb + 1) * TB], writes=[tt[r][cs]])

        issue_loads(0)
        ocnt = 0
        for bi, (s, tb) in enumerate(blocks):
            r = bi % 2
            if bi + 1 < len(blocks):
                issue_loads(bi + 1)
            rms_block(P, xt[r], ss[r], junk, lnv, rstd[r], 4)
            for j in range(4):
                P.op("dve", lambda e, j=j: e.scalar_tensor_tensor(
                    out=hn[j][:, :], in0=xt[r][j][:, :], scalar=rstd[r][:, j:j + 1], in1=gbc[:, :],
                    op0=ALU.mult, op1=ALU.mult), reads=[xt[r][j], rstd[r], gbc], writes=[hn[j]])
            for c in range(8):
                tpc = tp[c % 2]
                for j in range(4):
                    P.op("pe", lambda e, j=j, c=c, tpc=tpc: e.transpose(
                        out=tpc[:, j * 128:(j + 1) * 128], in_=hn[j][:, c * 128:(c + 1) * 128],
                        identity=P.ident[:, :]), reads=[hn[j], P.ident], writes=[tpc])
                P.op("act", lambda e, c=c, tpc=tpc: e.copy(out=hnT[r][:, c, :], in_=tpc[:, :]),
                     reads=[tpc], writes=[hnT[r]])
            ft = 0
            pi = 0
            while ft < nfm:
                ty = cfg["types"][pi]
                if ty is None:
                    pr = pi % 2
                    for c in range(8):
                        P.op("pe", lambda e, c=c, ft=ft, pr=pr: e.matmul(
                            psA[pr][:, :], lhsT=wfm[:, c, ft * 128:(ft + 1) * 128], rhs=hnT[r][:, c, :],
                            start=(c == 0), stop=(c == 7)), reads=[wfm, hnT[r]], writes=[psA[pr]])
                    cfg["plain_handler"](P, extra, s, tb, ft, psA[pr])
                    ft += 1
                    pi += 1
                    continue
                pr = pi % 2
                for half, ps in ((0, psA[pr]), (1, psB[pr])):
                    for c in range(8):
                        P.op("pe", lambda e, c=c, ps=ps, f=ft + half: e.matmul(
                            ps[:, :], lhsT=wfm[:, c, f * 128:(f + 1) * 128], rhs=hnT[r][:, c, :],
                            start=(c == 0), stop=(c == 7)), reads=[wfm, hnT[r]], writes=[ps])
                cosT, sinT = tabs[ty][r]
                t1, t2, t3, t4 = tmp[pr]
                A, B = psA[pr], psB[pr]
                for (o, a, b) in ((t1, A, cosT), (t2, B, sinT), (t3, B, cosT), (t4, A, sinT)):
                    P.op("dve", lambda e, o=o, a=a, b=b: e.tensor_tensor(out=o[:, :], in0=a[:, :], in1=b[:, :], op=ALU.mult),
                         reads=[a, b], writes=[o])
                oa, ob = oA[ocnt % 3], oB[ocnt % 3]
                ocnt += 1
                if cfg.get("rope_f32") and cfg["rope_f32"](pi):
                    cfg["rope_f32_handler"](P, extra, s, tb, pi, ft, t1, t2, t3, t4, oa, ob)
                else:
                    P.op("pool", lambda e, oa=oa: e.tensor_tensor(out=oa[:, :], in0=t1[:, :], in1=t2[:, :], op=ALU.subtract),
                         reads=[t1, t2], writes=[oa])
                    P.op("pool", lambda e, ob=ob: e.tensor_tensor(out=ob[:, :], in0=t3[:, :], in1=t4[:, :], op=ALU.add),
                         reads=[t3, t4], writes=[ob])
                P.dma("sp", cfg["fmT"][s, ft, :, tb * TB:(tb + 1) * TB], oa[:, :], reads=[oa])
                P.dma("sp", cfg["fmT"][s, ft + 1, :, tb * TB:(tb + 1) * TB], ob[:, :], reads=[ob])
                ft += 2
                pi += 1
            cfg["tm_handler"](P, extra, s, tb, r, hnT[r], wtm, pst)
    P.barrier()


def l0_alloc(P, ctx):
    ex = {}
    ex["tmo"] = [P.sb(ctx, f"tmo{r}", [128, 640], BF16) for r in range(2)]
    ex["tmw"] = [P.sb(ctx, f"tmw{r}", [128, 8], F32) for r in range(2)]
    ex["cnt"] = 0
    return ex


def make_l0_tm_handler(tmv, tmw):
    def handler(P, ex, s, tb, r, hnT, wtm, pst):
        for j in range(4):
            t0 = tb * TB + j * 128
            for c in range(8):
                P.op("pe", lambda e: e.matmul(pst[0][:, :], lhsT=hnT[:, c, j * 128:(j + 1) * 128], rhs=wtm[:, c, 0:512],
                                              start=(c == 0), stop=(c == 7)), reads=[hnT, wtm], writes=[pst[0]])
            for c in range(8):
                P.op("pe", lambda e: e.matmul(pst[1][:, 0:136], lhsT=hnT[:, c, j * 128:(j + 1) * 128], rhs=wtm[:, c, 512:648],
                                              start=(c == 0), stop=(c == 7)), reads=[hnT, wtm], writes=[pst[1]])
            k = ex["cnt"] % 2
            ex["cnt"] += 1
            o, w = ex["tmo"][k], ex["tmw"][k]
            P.op("act", lambda e: e.copy(out=o[:, 0:512], in_=pst[0][:, :]), reads=[pst[0]], writes=[o])
            P.op("act", lambda e: e.copy(out=o[:, 512:640], in_=pst[1][:, 0:128]), reads=[pst[1]], writes=[o])
            P.op("act", lambda e: e.copy(out=w[:, :], in_=pst[1][:, 128:136]), reads=[pst[1]], writes=[w])
            P.dma("sp", tmv[s, t0:t0 + 128, :], o[:, :], reads=[o])
            P.dma("sp", tmw[s, :, t0 // 128, :], w[:, :], reads=[w])
    return handler


def build_program(phases, debug=False, h2_input=False):
    nc = bass.Bass("TRN2", target_bir_lowering=False)
    dbg_names = set(debug) if isinstance(debug, (list, tuple, set)) else None

    def din(name, shape, dt=F32):
        return nc.dram_tensor(name, list(shape), dt, kind="ExternalInput").ap()

    def dsc(name, shape, dt):
        ext = debug and (dbg_names is None or name in dbg_names)
        return nc.dram_tensor(name, list(shape), dt, kind="ExternalOutput" if ext else "Internal").ap()

    spec = {}

    spec["x"] = (din, ("x", [SPC, S, D],))
    spec["norms"] = (din, ("norms", [8, D]))
    spec["rope"] = (din, ("rope", [3, 2, 128, S],))
    spec["ident"] = (din, ("ident", [128, 128], BF16,))
    spec["wfm0"] = (din, ("wfm0", [D, 18 * 128],))
    spec["wtm0"] = (din, ("wtm0", [D, 648],))
    spec["fm0"] = (dsc, ("fm0", [SPC, 18, 128, S], BF16,))
    spec["tmv0"] = (dsc, ("tmv0", [SPC, S, 640], BF16,))
    spec["tmw0"] = (dsc, ("tmw0", [SPC, 128, 32, 8], F32,))
    spec["mixT0"] = (dsc, ("mixT0", [SPC, 1024, S], BF16,))
    spec["diff_lambda"] = (din, ("diff_lambda", [4, 64],))
    spec["diff_subln"] = (din, ("diff_subln", [128],))
    spec["tri"] = (din, ("tri", [128, 128], BF16,))
    spec["dmask"] = (din, ("dmask", [4, 128, TB],))
    spec["pow2"] = (din, ("pow2", [NIT],))
    spec["wout0"] = (din, ("wout0", [1024, D],))
    spec["wg"] = (din, ("wg", [2, D, DFF],))
    spec["wu"] = (din, ("wu", [2, D, DFF],))
    spec["wd"] = (din, ("wd", [2, DFF, D],))
    spec["h1"] = (dsc, ("h1", [SPC, S, D], F32,))
    spec["h2"] = (dsc, ("h2", [SPC, S, D], F32,))
    spec["wfm1"] = (din, ("wfm1", [D, 20 * 128],))
    spec["wtm1"] = (din, ("wtm1", [D, 1552],))
    spec["wout1"] = (din, ("wout1", [1536, D],))
    spec["convw"] = (din, ("convw", [1536, 4],))
    spec["convb"] = (din, ("convb", [128, 12],))
    spec["pm"] = (din, ("pm", [256],))
    spec["oh"] = (din, ("oh", [256],))
    spec["blk1h"] = (din, ("blk1h", [16, S], BF16,))
    spec["triU"] = (din, ("triU", [128, 128],))
    spec["sel16"] = (din, ("sel16", [16, 16 * 128],))
    spec["ssm_norm"] = (din, ("ssm_norm", [1024],))
    spec["dsk"] = (din, ("dsk", [1024],))
    spec["dt_bias"] = (din, ("dt_bias", [16],))
    spec["a_log"] = (din, ("a_log", [16],))
    spec["fm1"] = (dsc, ("fm1", [SPC, 20, 128, S], BF16,))
    spec["xbcT"] = (dsc, ("xbcT", [SPC, 12, 128, S], BF16,))
    spec["mqf"] = (dsc, ("mqf", [SPC, 4, 128, S], F32,))
    spec["kmean"] = (dsc, ("kmean", [SPC, 4, 128, 16], F32,))
    spec["tm1"] = (dsc, ("tm1", [SPC, S, 1536], BF16,))
    spec["dtraw"] = (dsc, ("dtraw", [SPC, 128, 32, 16], F32,))
    spec["negT"] = (dsc, ("negT", [SPC, 128, S], BF16,))
    spec["mixT1"] = (dsc, ("mixT1", [SPC, 1536, S], BF16,))
    spec["h3"] = (dsc, ("h3", [SPC, S, D], F32,))
    if h2_input:
        spec["h2"] = (din, ("h2", [SPC, S, D]))
    if not debug:
        spec["out"] = (lambda name, shape, dt: nc.dram_tensor(name, list(shape), dt, kind="ExternalOutput").ap(), ("out", [SPC, S, D], F32))

    class Lazy(dict):
        def __missing__(self, key):
            fn, args = spec[key]
            v = fn(*args)
            self[key] = v
            return v

    T = Lazy()
    T["dbg"] = DBG
    with ExitStack() as stack:
        P = Prog(nc, stack)
        P.ident = P.sb(stack, "ident_sb", [128, 128], BF16)
        P.eps_t = P.sb(stack, "eps_t", [128, 1], F32)
        P.one_t = P.sb(stack, "one_t", [128, 1], F32)
        P.dma("sp", P.ident[:, :], T["ident"], writes=[P.ident])
        P.op("dve", lambda e: e.memset(P.eps_t[:, :], EPS), writes=[P.eps_t])
        P.op("dve", lambda e: e.memset(P.one_t[:, :], 1.0), writes=[P.one_t])
        _, types0, _ = l0_layout()
        if "A0" in phases:
            phase_inproj(P, nc, dict(x=T["x"], g=T["norms"][0], wfm=T["wfm0"], wtm=T["wtm0"], nfm=18, ntm=648,
                                     types=types0, rope=T["rope"], fmT=T["fm0"], alloc=l0_alloc,
                                     tm_handler=make_l0_tm_handler(T["tmv0"], T["tmw0"])))
        if "B0" in phases:
            phase_diff(P, nc, T, 0.8 - 0.6 * math.exp(-0.3 * 0))
        if "B1" in phases:
            phase_dsa(P, nc, T)
        if "C0" in phases:
            phase_outproj(P, nc, T["mixT0"], T["wout0"], 1024, T["norms"][1], T["x"], T["h1"],
                          (T["wg"][0], T["wu"][0], T["wd"][0], T["norms"][2], T["norms"][3], T["h1"], T["h2"]))
        if "A1" in phases:
            phase_inproj(P, nc, make_l1_cfg(T))
        if "B2" in phases:
            phase_ssd(P, nc, T)
        if "B3" in phases:
            phase_moba_gate(P, nc, T)
            phase_moba_attn(P, nc, T)
        if "C1" in phases:
            phase_outproj(P, nc, T["mixT1"], T["wout1"], 1536, T["norms"][5], T["h2"], T["h3"],
                          (T["wg"][1], T["wu"][1], T["wd"][1], T["norms"][6], T["norms"][7], T["h3"], T["h3"] if debug else T["out"]))
        P.barrier()
        nc.used_inputs = set(k for k in T.keys() if k in spec and spec[k][0] is din)
        print("instructions:", P.ninstr, {e: P.ecnt[e] for e in P.ecnt})
    return nc


def host_inputs(inputs):
    x = np.ascontiguousarray(inputs["x"], dtype=np.float32)
    fm_cols, _, tm_cols = l0_layout()
    w_in0 = np.asarray(inputs["even_w_in"][0], np.float32)
    fm_cols1, _, tm_cols1 = l1_layout()
    w_in1 = np.asarray(inputs["odd_w_in"][0], np.float32)
    norms = np.stack([inputs["norm_mix_pre"][0], inputs["norm_mix_post"][0], inputs["norm_ffn_pre"][0], inputs["norm_ffn_post"][0],
                      inputs["norm_mix_pre"][1], inputs["norm_mix_post"][1], inputs["norm_ffn_pre"][1], inputs["norm_ffn_post"][1]]).astype(np.float32)
    common = {
        "norms": norms,
        "rope": rope_tables_host(),
        "ident": np.eye(128, dtype=np.float32).astype(ml_dtypes.bfloat16),
        "wfm0": np.ascontiguousarray(w_in0[:, fm_cols]),
        "wtm0": np.ascontiguousarray(w_in0[:, tm_cols]),
        "diff_lambda": np.asarray(inputs["diff_lambda"][0], np.float32),
        "diff_subln": np.asarray(inputs["diff_subln"][0], np.float32),
        "dmask": np.stack([np.where(np.arange(TB)[None, :] <= 128 * qt + np.arange(128)[:, None], 0.0, NEG) for qt in range(4)]).astype(np.float32),
        "pow2": (0.5 ** np.arange(1, NIT + 1)).astype(np.float32),
        "wout0": np.asarray(inputs["even_w_out"][0], np.float32),
        "wg": np.asarray(inputs["ffn_gate"], np.float32),
        "wu": np.asarray(inputs["ffn_up"], np.float32),
        "wd": np.asarray(inputs["ffn_down"], np.float32),
        "wfm1": np.ascontiguousarray(w_in1[:, fm_cols1]),
        "wtm1": np.ascontiguousarray(w_in1[:, tm_cols1]),
        "wout1": np.asarray(inputs["odd_w_out"][0], np.float32),
        "convw": np.ascontiguousarray(np.asarray(inputs["ssm_conv_w"][0], np.float32).T),
        "convb": np.ascontiguousarray(np.asarray(inputs["ssm_conv_b"][0], np.float32).reshape(12, 128).T),
        "pm": np.where(np.arange(16)[None, :] < np.arange(16)[:, None], 0.0, NEG).astype(np.float32).reshape(256),
        "oh": np.eye(16, dtype=np.float32).reshape(256),
        "blk1h": (np.arange(S)[None, :] // 256 == np.arange(16)[:, None]).astype(np.float32).astype(ml_dtypes.bfloat16),
        "triU": np.triu(np.ones((128, 128), np.float32)),
        "sel16": np.repeat(np.eye(16, dtype=np.float32)[:, :, None], 128, axis=2).reshape(16, 16 * 128),
        "ssm_norm": np.asarray(inputs["ssm_norm"][0], np.float32),
        "dsk": np.repeat(np.asarray(inputs["ssm_d"][0], np.float32), 64),
        "dt_bias": np.asarray(inputs["ssm_dt_bias"][0], np.float32),
        "a_log": np.asarray(inputs["ssm_a_log"][0], np.float32),
        "tri": np.triu(np.ones((128, 128), np.float32)).astype(ml_dtypes.bfloat16),
    }
    maps = []
    for c in range(NCORES):
        m = dict(common)
        m["x"] = x[c * SPC:(c + 1) * SPC]
        maps.append(m)
    return maps


def filter_maps(nc, maps):
    used = nc.used_inputs
    return [{k: v for k, v in m.items() if k in used} for m in maps]


DBG = {}
ALL_PHASES = ["A0", "B0", "B1", "C0", "A1", "B2", "B3", "C1"]


def kernel(**inputs):
    nc = build_program(ALL_PHASES)
    maps = filter_maps(nc, host_inputs(inputs))
    res = run_bass_kernel_spmd(nc, maps, core_ids=list(range(NCORES)))
    return np.concatenate([r["out"] for r in res.results], axis=0)


def attn_core(P, A, qT, kT_of, v1_of, qb, E, scale, rd, mask_of=None, tri=None, diag_only_tri=True):
    nkt = 4 * (qb + 1)
    started = [False, False]

    def stage1(kt):
        r = kt - 4 * qb
        st = A["st"][A["i"] % 2]
        pt = A["pt"][A["i"] % len(A["pt"])]
        A["i"] += 1
        P.op("pe", lambda e: e.matmul(st[:, :], lhsT=kT_of(kt), rhs=qT, start=True, stop=True),
             reads=rd, writes=[st])
        P.op("act", lambda e: e.activation(out=pt[:, :], in_=st[:, :], func=AF.Exp, scale=scale),
             reads=[st], writes=[pt])
        if mask_of is not None:
            m_ap, m_t = mask_of(kt)
            P.op("pool", lambda e: e.tensor_tensor(out=pt[:, :], in0=pt[:, :], in1=m_ap, op=ALU.mult),
                 reads=[pt, m_t], writes=[pt])
        elif r >= 0:
            P.op("pool", lambda e: e.tensor_tensor(out=pt[:, r * 128:(r + 1) * 128], in0=pt[:, r * 128:(r + 1) * 128],
                                                   in1=tri[:, :], op=ALU.mult), reads=[pt, tri], writes=[pt])
        return pt

    def stage2(kt, pt):
        r = kt - 4 * qb
        for qt in range(4):
            if r > qt:
                continue
            acc = A["acc"][qt // 2]
            first = not started[qt // 2]
            started[qt // 2] = True
            last = (kt == 4 * qb + qt)
            P.op("pe", lambda e: e.matmul(acc[:, qt % 2, 0:E + 1], lhsT=pt[:, qt * 128:(qt + 1) * 128], rhs=v1_of(kt),
                                          start=first, stop=last, skip_group_check=True), reads=[pt] + rd, writes=[acc])

    prev = None
    for kt in range(nkt):
        cur = stage1(kt)
        if prev is not None:
            stage2(*prev)
        prev = (kt, cur)
    stage2(*prev)


def q_rows_h64(fm, s, base_tile, m):
    p, ml = m // 4, m % 4
    return fm[s, base_tile + 2 * p, 32 * ml:32 * ml + 32, :], fm[s, base_tile + 2 * p + 1, 32 * ml:32 * ml + 32, :]


def compute_lambda(P, ctx, dl_ap, lam_init):
    lf = P.sb(ctx, "lf", [128, 4, 64], F32)
    P.dma("sp", lf[:, :, :], dl_ap.rearrange("a d -> (a d)").partition_broadcast(128).rearrange("p (a d) -> p a d", a=4), writes=[lf])
    pr = P.sb(ctx, "lpr", [128, 2, 64], F32)
    sm = P.sb(ctx, "lsm", [128, 2], F32)
    ex = P.sb(ctx, "lex", [128, 2], F32)
    nl = P.sb(ctx, "nlam", [128, 1], F32)
    P.op("dve", lambda e: e.tensor_tensor(out=pr[:, 0, :], in0=lf[:, 0, :], in1=lf[:, 1, :], op=ALU.mult), reads=[lf], writes=[pr])
    P.op("dve", lambda e: e.tensor_tensor(out=pr[:, 1, :], in0=lf[:, 2, :], in1=lf[:, 3, :], op=ALU.mult), reads=[lf, pr], writes=[pr])
    P.op("dve", lambda e: e.tensor_reduce(out=sm[:, :], in_=pr[:, :, :], axis=AX.X, op=ALU.add), reads=[pr], writes=[sm])
    P.op("act", lambda e: e.activation(out=ex[:, :], in_=sm[:, :], func=AF.Exp), reads=[sm], writes=[ex])
    P.op("dve", lambda e: e.tensor_tensor(out=nl[:, :], in0=ex[:, 1:2], in1=ex[:, 0:1], op=ALU.subtract), reads=[ex], writes=[nl])
    P.op("dve", lambda e: e.tensor_scalar(out=nl[:, :], in0=nl[:, :], scalar1=-lam_init, scalar2=None, op0=ALU.add), reads=[nl], writes=[nl])
    return nl


def phase_diff(P, nc, T, lam_init):
    fm, tmv, mixT = T["fm0"], T["tmv0"], T["mixT0"]
    with ExitStack() as ctx:
        nlam = compute_lambda(P, ctx, T["diff_lambda"], lam_init)
        g2 = P.sb(ctx, "g2", [128, 128], F32)
        P.dma("sp", g2[:, :], T["diff_subln"].partition_broadcast(128), writes=[g2])
        P.op("dve", lambda e: e.tensor_scalar(out=g2[:, :], in0=g2[:, :], scalar1=1.0 - lam_init, scalar2=None, op0=ALU.mult),
             reads=[g2], writes=[g2])
        tri = P.sb(ctx, "tri", [128, 128], BF16)
        P.dma("sp", tri[:, :], T["tri"], writes=[tri])
        kT = [P.sb(ctx, f"kT{r}", [128, S], BF16) for r in range(2)]
        v1 = [P.sb(ctx, f"v1{r}", [128, 32, 129], BF16) for r in range(2)]
        for r in range(2):
            P.op("pool", lambda e: e.memset(v1[r][:, :, 128:129], 1.0), writes=[v1[r]])
        qT = [P.sb(ctx, f"qT{r}", [128, TB], BF16) for r in range(2)]
        A = {"st": [P.ps(ctx, f"st{r}", [128, TB]) for r in range(2)],
             "pt": [P.sb(ctx, f"pt{r}", [128, TB], BF16) for r in range(3)], "i": 0}
        accs = [[P.ps(ctx, f"acc{m}_{r}", [128, 2, 256]) for r in range(2)] for m in range(2)]
        tp = P.ps(ctx, "tpo", [128, TB], BF16)
        o1 = [P.sb(ctx, f"o1_{r}", [128, 4, 128], F32) for r in range(2)]
        rc = [P.sb(ctx, f"rc{r}", [128, 8], F32) for r in range(2)]
        ss = P.sb(ctx, "dss", [128, 4], F32)
        lnv = P.sb(ctx, "dlnv", [128, 4], F32)
        rstd = P.sb(ctx, "drstd", [128, 4], F32)
        junk = P.sb(ctx, "djunk", [128, 128], BF16)
        ob = [P.sb(ctx, f"ob{r}", [128, 4, 128], BF16) for r in range(2)]
        oT = [P.sb(ctx, f"oT{r}", [128, TB], BF16) for r in range(2)]
        it = 0
        hi = 0
        for s in range(SPC):
            for h in range(4):
                kr = hi % 2
                hi += 1
                hh = h % 2
                p = h // 2
                srcs = [(4 + 2 * p, 64 * hh), (5 + 2 * p, 64 * hh), (4 + 2 * p, 64 * hh + 32), (5 + 2 * p, 64 * hh + 32)]
                for i, (tile, row) in enumerate(srcs):
                    P.dma("sp", kT[kr][32 * i:32 * i + 32, :], fm[s, tile, row:row + 32, :], writes=[kT[kr]])
                for half in range(2):
                    P.dma("sp", v1[kr][:, 16 * half:16 * half + 16, 0:128],
                          tmv[s, 2048 * half:2048 * (half + 1), h * 128:(h + 1) * 128].rearrange("(kt p) e -> p kt e", p=128),
                          writes=[v1[kr]])
                for qb in range(NTB):
                    qr = it % 2
                    it += 1
                    qsrcs = [(2 * p, 64 * hh), (2 * p + 1, 64 * hh), (2 * p, 64 * hh + 32), (2 * p + 1, 64 * hh + 32)]
                    for i, (tile, row) in enumerate(qsrcs):
                        P.dma("sp", qT[qr][32 * i:32 * i + 32, :], fm[s, tile, row:row + 32, qb * TB:(qb + 1) * TB], writes=[qT[qr]])
                    for m in range(2):
                        A["acc"] = accs[m]
                        attn_core(P, A, qT[qr][64 * m:64 * m + 64, :],
                                  lambda kt: kT[kr][64 * m:64 * m + 64, kt * 128:(kt + 1) * 128],
                                  lambda kt: v1[kr][:, kt, :], qb, 128, 0.125, [qT[qr], kT[kr], v1[kr]], tri=tri)
                        for qt in range(4):
                            acc = accs[m][qt // 2]
                            P.op("dve", lambda e: e.reciprocal(out=rc[qr][:, 4 * m + qt:4 * m + qt + 1], in_=acc[:, qt % 2, 128:129]),
                                 reads=[acc], writes=[rc[qr]])
                        if m == 1:
                            P.op("dve", lambda e: e.tensor_scalar(out=rc[qr][:, 4:8], in0=rc[qr][:, 4:8], scalar1=nlam[:, 0:1], scalar2=None,
                                                                  op0=ALU.mult), reads=[rc[qr], nlam], writes=[rc[qr]])
                        for qt in range(4):
                            acc = accs[m][qt // 2]
                            if m == 0:
                                P.op("dve", lambda e: e.tensor_scalar(out=o1[qr][:, qt, :], in0=acc[:, qt % 2, 0:128],
                                                                      scalar1=rc[qr][:, qt:qt + 1], scalar2=None, op0=ALU.mult),
                                     reads=[acc, rc[qr]], writes=[o1[qr]])
                            else:
                                P.op("dve", lambda e: e.scalar_tensor_tensor(out=o1[qr][:, qt, :], in0=acc[:, qt % 2, 0:128],
                                                                             scalar=rc[qr][:, 4 + qt:5 + qt], in1=o1[qr][:, qt, :],
                                                                             op0=ALU.mult, op1=ALU.add),
                                     reads=[acc, rc[qr], o1[qr]], writes=[o1[qr]])
                    for qt in range(4):
                        P.op("act", lambda e: e.activation(out=junk[:, :], in_=o1[qr][:, qt, :], func=AF.Square, accum_out=ss[:, qt:qt + 1]),
                             reads=[o1[qr]], writes=[junk, ss])
                    P.op("act", lambda e: e.activation(out=lnv[:, :], in_=ss[:, :], func=AF.Ln, scale=1.0 / 128, bias=P.eps_t[:, 0:1]),
                         reads=[ss, P.eps_t], writes=[lnv])
                    P.op("act", lambda e: e.activation(out=rstd[:, :], in_=lnv[:, :], func=AF.Exp, scale=-0.5), reads=[lnv], writes=[rstd])
                    for qt in range(4):
                        P.op("dve", lambda e: e.scalar_tensor_tensor(out=ob[qr][:, qt, :], in0=o1[qr][:, qt, :], scalar=rstd[:, qt:qt + 1],
                                                                     in1=g2[:, :], op0=ALU.mult, op1=ALU.mult),
                             reads=[o1[qr], rstd, g2], writes=[ob[qr]])
                        P.op("pe", lambda e: e.transpose(out=tp[:, qt * 128:(qt + 1) * 128], in_=ob[qr][:, qt, :], identity=P.ident[:, :]),
                             reads=[ob[qr], P.ident], writes=[tp])
                    P.op("act", lambda e: e.copy(out=oT[qr][:, :], in_=tp[:, :]), reads=[tp], writes=[oT[qr]])
                    P.dma("sp", mixT[s, h * 128:(h + 1) * 128, qb * TB:(qb + 1) * TB], oT[qr][:, :], reads=[oT[qr]])
    P.barrier()


def rstd_from_ss(P, ss, lnv, rstd, n, dim):
    P.op("act", lambda e: e.activation(out=lnv[:, 0:n], in_=ss[:, 0:n], func=AF.Ln, scale=1.0 / dim, bias=P.eps_t[:, 0:1]),
         reads=[ss, P.eps_t], writes=[lnv])
    P.op("act", lambda e: e.activation(out=rstd[:, 0:n], in_=lnv[:, 0:n], func=AF.Exp, scale=-0.5), reads=[lnv], writes=[rstd])


def phase_outproj(P, nc, mixT, wout_ap, kmix, g_ap, h_in, h_out, ffn_args):
    kc = kmix // 128
    with ExitStack() as octx:
      wg_t = P.sb(octx, "wg", [128, 8, DFF], BF16)
      wu_t = P.sb(octx, "wu", [128, 8, DFF], BF16)
      with ExitStack() as ctx:
        wout = load_weight_bf16(P, ctx, "wout", wout_ap, kmix, D)
        pre = {"wg": load_weight_bf16(P, ctx, "wg", ffn_args[0], D, DFF, wt=wg_t),
               "wu": load_weight_bf16(P, ctx, "wu", ffn_args[1], D, DFF, wt=wu_t)}
        gbc = P.sb(ctx, "gbc", [128, D], F32)
        P.dma("sp", gbc[:, :], g_ap.partition_broadcast(128), writes=[gbc])
        mt = [P.sb(ctx, f"mt{r}", [128, kc, TB], BF16) for r in range(2)]
        ht = [[P.sb(ctx, f"ht{r}_{j}", [128, D], F32) for j in range(4)] for r in range(2)]
        mo = [P.sb(ctx, f"mo{r}", [128, D], F32) for r in range(2)]
        junk = P.sb(ctx, "junk", [128, D], BF16)
        ss = [P.sb(ctx, f"ss{r}", [128, 1], F32) for r in range(2)]
        lnv = P.sb(ctx, "lnv", [128, 1], F32)
        rstd = [P.sb(ctx, f"rstd{r}", [128, 1], F32) for r in range(2)]
        ps = [P.ps(ctx, f"pso{r}", [128, TB]) for r in range(4)]
        blocks = [(s, tb) for s in range(SPC) for tb in range(NTB)]

        def loads(bi):
            s, tb = blocks[bi]
            r = bi % 2
            P.dma("sp", mt[r][:, :, :], mixT[s, :, tb * TB:(tb + 1) * TB].rearrange("(c p) t -> p c t", p=128), writes=[mt[r]])
            for j in range(4):
                t0 = tb * TB + j * 128
                P.dma("sp", ht[r][j][:, :], h_in[s, t0:t0 + 128, :], writes=[ht[r][j]])

        loads(0)
        k = 0
        for bi, (s, tb) in enumerate(blocks):
            r = bi % 2
            if bi + 1 < len(blocks):
                loads(bi + 1)
            for j in range(4):
                t0 = tb * TB + j * 128
                kk = k % 2
                k += 1
                for half in range(2):
                    pp = ps[2 * kk + half]
                    for c in range(kc):
                        P.op("pe", lambda e: e.matmul(pp[:, :], lhsT=mt[r][:, c, j * 128:(j + 1) * 128],
                                                      rhs=wout[:, c, half * 512:(half + 1) * 512], start=(c == 0), stop=(c == kc - 1)),
                             reads=[mt[r], wout], writes=[pp])
                    P.op("act", lambda e: e.copy(out=mo[kk][:, half * 512:(half + 1) * 512], in_=pp[:, :]), reads=[pp], writes=[mo[kk]])
                P.op("act", lambda e: e.activation(out=junk[:, :], in_=mo[kk][:, :], func=AF.Square, accum_out=ss[kk][:, 0:1]),
                     reads=[mo[kk]], writes=[junk, ss[kk]])
                rstd_from_ss(P, ss[kk], lnv, rstd[kk], 1, D)
                P.op("dve", lambda e: e.scalar_tensor_tensor(out=mo[kk][:, :], in0=mo[kk][:, :], scalar=rstd[kk][:, 0:1], in1=gbc[:, :],
                                                             op0=ALU.mult, op1=ALU.mult), reads=[mo[kk], rstd[kk], gbc], writes=[mo[kk]])
                P.op("pool", lambda e: e.tensor_tensor(out=ht[r][j][:, :], in0=ht[r][j][:, :], in1=mo[kk][:, :], op=ALU.add),
                     reads=[ht[r][j], mo[kk]], writes=[ht[r][j]])
                P.dma("sp", h_out[s, t0:t0 + 128, :], ht[r][j][:, :], reads=[ht[r][j]])
      P.barrier()
      phase_ffn(P, nc, *ffn_args, pre=pre)


FB = 256


def phase_ffn(P, nc, wg_ap, wu_ap, wd_ap, gpre_ap, gpost_ap, h_in, h_out, pre=None):
    with ExitStack() as ctx:
        wg, wu = pre["wg"], pre["wu"]
        wd = load_weight_bf16(P, ctx, "wd", wd_ap, DFF, D)
        gpre = P.sb(ctx, "gpre", [128, D], F32)
        gpost = P.sb(ctx, "gpost", [128, D], F32)
        P.dma("sp", gpre[:, :], gpre_ap.partition_broadcast(128), writes=[gpre])
        P.dma("sp", gpost[:, :], gpost_ap.partition_broadcast(128), writes=[gpost])
        ht = [[P.sb(ctx, f"ht{r}_{j}", [128, D], F32) for j in range(2)] for r in range(2)]
        hn = [P.sb(ctx, f"hn{j}", [128, D], BF16) for j in range(2)]
        hnT = P.sb(ctx, "hnT", [128, 8, FB], BF16)
        actT = P.sb(ctx, "actT", [128, NFT, FB], BF16)
        sg = [P.sb(ctx, f"sg{r}", [128, FB], F32) for r in range(2)]
        mo = [P.sb(ctx, f"mo{r}", [128, D], F32) for r in range(2)]
        junk = P.sb(ctx, "junk", [128, D], BF16)
        ss = [P.sb(ctx, f"ss{r}", [128, 2], F32) for r in range(2)]
        lnv = P.sb(ctx, "lnv", [128, 2], F32)
        rstd = [P.sb(ctx, f"rstd{r}", [128, 2], F32) for r in range(2)]
        ss2 = [P.sb(ctx, f"ss2{r}", [128, 1], F32) for r in range(2)]
        rstd2 = [P.sb(ctx, f"rstd2{r}", [128, 1], F32) for r in range(2)]
        tp = [P.ps(ctx, f"tp{r}", [128, FB], BF16) for r in range(2)]
        psg = [P.ps(ctx, f"psg{r}", [128, FB]) for r in range(2)]
        psu = [P.ps(ctx, f"psu{r}", [128, FB]) for r in range(2)]
        psd = [P.ps(ctx, f"psd{r}", [128, TB]) for r in range(2)]
        nb = S // FB
        blocks = [(s, tb) for s in range(SPC) for tb in range(nb)]

        def loads(bi):
            s, tb = blocks[bi]
            r = bi % 2
            for j in range(2):
                t0 = tb * FB + j * 128
                P.dma("sp", ht[r][j][:, :], h_in[s, t0:t0 + 128, :], writes=[ht[r][j]])

        loads(0)
        k = 0
        for bi, (s, tb) in enumerate(blocks):
            r = bi % 2
            if bi + 1 < len(blocks):
                loads(bi + 1)
            for j in range(2):
                P.op("act", lambda e: e.activation(out=junk[:, :], in_=ht[r][j][:, :], func=AF.Square, accum_out=ss[r][:, j:j + 1]),
                     reads=[ht[r][j]], writes=[junk, ss[r]])
            rstd_from_ss(P, ss[r], lnv, rstd[r], 2, D)
            for j in range(2):
                P.op("dve", lambda e: e.scalar_tensor_tensor(out=hn[j][:, :], in0=ht[r][j][:, :], scalar=rstd[r][:, j:j + 1], in1=gpre[:, :],
                                                             op0=ALU.mult, op1=ALU.mult), reads=[ht[r][j], rstd[r], gpre], writes=[hn[j]])
            for c in range(8):
                tpc = tp[c % 2]
                for j in range(2):
                    P.op("pe", lambda e: e.transpose(out=tpc[:, j * 128:(j + 1) * 128], in_=hn[j][:, c * 128:(c + 1) * 128],
                                                     identity=P.ident[:, :]), reads=[hn[j], P.ident], writes=[tpc])
                P.op("dve", lambda e: e.tensor_copy(out=hnT[:, c, :], in_=tpc[:, :]), reads=[tpc], writes=[hnT])
            for f in range(NFT):
                fr = f % 2
                for c in range(8):
                    P.op("pe", lambda e: e.matmul(psg[fr][:, :], lhsT=wg[:, c, f * 128:(f + 1) * 128], rhs=hnT[:, c, :],
                                                  start=(c == 0), stop=(c == 7)), reads=[wg, hnT], writes=[psg[fr]])
                for c in range(8):
                    P.op("pe", lambda e: e.matmul(psu[fr][:, :], lhsT=wu[:, c, f * 128:(f + 1) * 128], rhs=hnT[:, c, :],
                                                  start=(c == 0), stop=(c == 7)), reads=[wu, hnT], writes=[psu[fr]])
                P.op("act", lambda e: e.activation(out=sg[fr][:, :], in_=psg[fr][:, :], func=AF.Silu), reads=[psg[fr]], writes=[sg[fr]])
                P.op("dve", lambda e: e.tensor_tensor(out=actT[:, f, :], in0=psu[fr][:, :], in1=sg[fr][:, :], op=ALU.mult),
                     reads=[psu[fr], sg[fr]], writes=[actT])
            for j in range(2):
                t0 = tb * FB + j * 128
                kk = k % 2
                k += 1
                for half in range(2):
                    pp = psd[half]
                    for f in range(NFT):
                        P.op("pe", lambda e: e.matmul(pp[:, :], lhsT=actT[:, f, j * 128:(j + 1) * 128],
                                                      rhs=wd[:, f, half * 512:(half + 1) * 512], start=(f == 0), stop=(f == NFT - 1)),
                             reads=[actT, wd], writes=[pp])
                    P.op("act", lambda e: e.copy(out=mo[kk][:, half * 512:(half + 1) * 512], in_=pp[:, :]), reads=[pp], writes=[mo[kk]])
                P.op("act", lambda e: e.activation(out=junk[:, :], in_=mo[kk][:, :], func=AF.Square, accum_out=ss2[kk][:, 0:1]),
                     reads=[mo[kk]], writes=[junk, ss2[kk]])
                rstd_from_ss(P, ss2[kk], lnv, rstd2[kk], 1, D)
                P.op("dve", lambda e: e.scalar_tensor_tensor(out=mo[kk][:, :], in0=mo[kk][:, :], scalar=rstd2[kk][:, 0:1], in1=gpost[:, :],
                                                             op0=ALU.mult, op1=ALU.mult), reads=[mo[kk], rstd2[kk], gpost], writes=[mo[kk]])
                P.op("pool", lambda e: e.tensor_tensor(out=ht[r][j][:, :], in0=ht[r][j][:, :], in1=mo[kk][:, :], op=ALU.add),
                     reads=[ht[r][j], mo[kk]], writes=[ht[r][j]])
                P.dma("sp", h_out[s, t0:t0 + 128, :], ht[r][j][:, :], reads=[ht[r][j]])
    P.barrier()


NIT = 12
TOPK = 256


def phase_dsa(P, nc, T):
    fm, tmv, tmw, mixT = T["fm0"], T["tmv0"], T["tmw0"], T["mixT0"]
    dbg = T.get("dbg", {})
    n_s, n_qb, stages = dbg.get("n_s", SPC), dbg.get("n_qb", NTB), dbg.get("stages", "idx,bis,tr,att")
    with ExitStack() as ctx:
        dmask = P.sb(ctx, "dmask", [128, 4, TB], F32)
        P.dma("sp", dmask[:, :, :], T["dmask"].rearrange("a p k -> p a k"), writes=[dmask])
        pow2 = P.sb(ctx, "pow2", [128, NIT], F32)
        P.dma("sp", pow2[:, :], T["pow2"].partition_broadcast(128), writes=[pow2])
        bkT = P.sb(ctx, "bkT", [128, S], BF16)
        ikT = P.sb(ctx, "ikT", [128, S], BF16)
        bv1 = P.sb(ctx, "bv1", [128, 32, 129], BF16)
        P.op("pool", lambda e: e.memset(bv1[:, :, 128:129], 1.0), writes=[bv1])
        iw = P.sb(ctx, "iw", [128, 32, 8], F32)
        iqT = [[P.sb(ctx, f"iqT{r}_{i}", [128, TB], BF16) for i in range(4)] for r in range(2)]
        bqT = [[P.sb(ctx, f"bqT{r}_{i}", [128, TB], BF16) for i in range(4)] for r in range(2)]
        score = [P.sb(ctx, f"score{r}", [128, S], F32) for r in range(2)]
        mask = [P.sb(ctx, f"mask{i}", [128, S], BF16) for i in range(4)]
        maskT = [P.sb(ctx, f"maskT{r}", [128, 32, TB], BF16) for r in range(2)]
        junk = P.sb(ctx, "junk", [128, S], BF16)
        rl = [P.sb(ctx, f"rl{r}", [128, TB], F32) for r in range(4)]
        sm = {n: P.sb(ctx, "sm_" + n, [128, 1], F32) for n in ("hi", "lo", "rng", "th", "cand", "cnt", "m")}
        steps = P.sb(ctx, "steps", [128, NIT], F32)
        A = {"st": [P.ps(ctx, f"st{r}", [128, TB]) for r in range(2)],
             "pt": [P.sb(ctx, f"pt{r}", [128, TB], BF16) for r in range(3)], "i": 0}
        accs = [[P.ps(ctx, f"acc{m}_{r}", [128, 2, 256]) for r in range(2)] for m in range(2)]
        tpx = P.ps(ctx, "tpx", [128, TB], BF16)
        ist = A["st"] + [P.ps(ctx, "st_x", [128, TB])]
        ii = 0
        rc = P.sb(ctx, "rc", [128, 4], F32)
        ob = [P.sb(ctx, f"ob{r}", [128, 4, 128], BF16) for r in range(2)]
        oT = [P.sb(ctx, f"oT{r}", [128, TB], BF16) for r in range(2)]
        st8 = {"hcnt": 0, "sc_i": 0, "ii": 0}
        lnl = P.sb(ctx, "lnl", [128, 4], F32)

        def load_idx_side(s):
            for rep in range(2):
                P.dma("sp", ikT[64 * rep:64 * rep + 32, :], fm[s, 16, 64:96, :], writes=[ikT])
                P.dma("sp", ikT[64 * rep + 32:64 * rep + 64, :], fm[s, 17, 64:96, :], writes=[ikT])
            P.dma("sp", iw[:, :, :], tmw[s, :, :, :], writes=[iw])

        def load_att_side(s):
            P.dma("sp", bkT[0:64, :], fm[s, 16, 0:64, :], writes=[bkT])
            P.dma("sp", bkT[64:128, :], fm[s, 17, 0:64, :], writes=[bkT])
            for half in range(2):
                P.dma("sp", bv1[:, 16 * half:16 * half + 16, 0:128],
                      tmv[s, 2048 * half:2048 * (half + 1), 512:640].rearrange("(kt p) e -> p kt e", p=128), writes=[bv1])

        def front_a(s, qb, r):
            c0, c1 = qb * TB, (qb + 1) * TB
            for i in range(4):
                p, hl0 = (2 * i) // 4, (2 * i) % 4
                for k2 in range(2):
                    hl = hl0 + k2
                    P.dma("sp", iqT[r][i][64 * k2:64 * k2 + 32, :], fm[s, 8 + 2 * p, 32 * hl:32 * hl + 32, c0:c1], writes=[iqT[r][i]])
                    P.dma("sp", iqT[r][i][64 * k2 + 32:64 * k2 + 64, :], fm[s, 9 + 2 * p, 32 * hl:32 * hl + 32, c0:c1], writes=[iqT[r][i]])
            for h in range(4):
                p, hl = h // 2, h % 2
                P.dma("sp", bqT[r][h][0:64, :], fm[s, 12 + 2 * p, 64 * hl:64 * hl + 64, c0:c1], writes=[bqT[r][h]])
                P.dma("sp", bqT[r][h][64:128, :], fm[s, 13 + 2 * p, 64 * hl:64 * hl + 64, c0:c1], writes=[bqT[r][h]])
            for qt in range(4):
                gq = 4 * qb + qt
                n = 128 * (gq + 1)
                sc = score[st8["sc_i"] % 2]
                st8["sc_i"] += 1
                for kc in range(qb + 1):
                    for h in range(8):
                        st = ist[st8["ii"] % 3]
                        rlb = rl[st8["ii"] % 4]
                        st8["ii"] += 1
                        ro = 64 * (h % 2)
                        P.op("pe", lambda e: e.matmul(st[:, :], lhsT=iqT[r][h // 2][ro:ro + 64, qt * 128:(qt + 1) * 128],
                                                      rhs=ikT[ro:ro + 64, kc * TB:(kc + 1) * TB], start=True, stop=True),
                             reads=[iqT[r][h // 2], ikT], writes=[st])
                        P.op("act", lambda e: e.activation(out=rlb[:, :], in_=st[:, :], func=AF.Relu), reads=[st], writes=[rlb])
                        if h == 0:
                            P.op("dve", lambda e: e.tensor_scalar(out=sc[:, kc * TB:(kc + 1) * TB], in0=rlb[:, :], scalar1=iw[:, gq, 0:1],
                                                                  scalar2=None, op0=ALU.mult), reads=[rlb, iw], writes=[sc])
                        else:
                            P.op("dve", lambda e: e.scalar_tensor_tensor(out=sc[:, kc * TB:(kc + 1) * TB], in0=rlb[:, :],
                                                                         scalar=iw[:, gq, h:h + 1], in1=sc[:, kc * TB:(kc + 1) * TB],
                                                                         op0=ALU.mult, op1=ALU.add), reads=[rlb, iw, sc], writes=[sc])
                P.op("pool", lambda e: e.tensor_tensor(out=sc[:, c0:c1], in0=sc[:, c0:c1], in1=dmask[:, qt, :], op=ALU.add),
                     reads=[sc, dmask], writes=[sc])
                th = sm["th"]
                if gq < 2:
                    P.op("dve", lambda e: e.memset(th[:, :], NEG / 2), writes=[th])
                else:
                    P.op("dve", lambda e: e.tensor_reduce(out=sm["hi"][:, :], in_=sc[:, 0:n], axis=AX.X, op=ALU.max), reads=[sc], writes=[sm["hi"]])
                    P.op("dve", lambda e: e.tensor_reduce(out=th[:, :], in_=sc[:, 0:128 * gq], axis=AX.X, op=ALU.min), reads=[sc], writes=[th])
                    P.op("dve", lambda e: e.tensor_tensor(out=sm["rng"][:, :], in0=sm["hi"][:, :], in1=th[:, :], op=ALU.subtract),
                         reads=[sm["hi"], th], writes=[sm["rng"]])
                    P.op("dve", lambda e: e.tensor_scalar(out=steps[:, :], in0=pow2[:, :], scalar1=sm["rng"][:, 0:1], scalar2=None, op0=ALU.mult),
                         reads=[pow2, sm["rng"]], writes=[steps])
                    P.op("dve", lambda e: e.tensor_tensor(out=sm["cand"][:, :], in0=th[:, :], in1=steps[:, 0:1], op=ALU.add),
                         reads=[th, steps], writes=[sm["cand"]])
                    for j in range(NIT):
                        P.op("dve", lambda e: e.tensor_scalar(out=junk[:, 0:n], in0=sc[:, 0:n], scalar1=sm["cand"][:, 0:1], scalar2=None,
                                                              op0=ALU.is_ge, op1=ALU.add, accum_out=sm["cnt"][:, 0:1]),
                             reads=[sc, sm["cand"]], writes=[junk, sm["cnt"]])
                        P.op("dve", lambda e: e.tensor_scalar(out=sm["m"][:, :], in0=sm["cnt"][:, :], scalar1=float(TOPK), scalar2=-0.5,
                                                              op0=ALU.is_ge, op1=ALU.add), reads=[sm["cnt"]], writes=[sm["m"]])
                        P.op("dve", lambda e: e.scalar_tensor_tensor(out=sm["cand"][:, :], in0=sm["m"][:, :], scalar=steps[:, j:j + 1],
                                                                     in1=sm["cand"][:, :], op0=ALU.mult, op1=ALU.add),
                             reads=[sm["m"], steps, sm["cand"]], writes=[sm["cand"]])
                    P.op("dve", lambda e: e.scalar_tensor_tensor(out=th[:, :], in0=sm["rng"][:, :], scalar=-(0.5 ** (NIT + 1)),
                                                                 in1=sm["cand"][:, :], op0=ALU.mult, op1=ALU.add),
                         reads=[sm["rng"], sm["cand"]], writes=[th])
                P.op("dve", lambda e: e.tensor_scalar(out=mask[qt][:, 0:n], in0=sc[:, 0:n], scalar1=th[:, 0:1], scalar2=None, op0=ALU.is_ge),
                     reads=[sc, th], writes=[mask[qt]])

        def front_b(s, qb, r):
            mT = maskT[r]
            for kt in range(4 * qb + 4):
                for qt in range(4):
                    if kt <= 4 * qb + qt:
                        P.op("pe", lambda e: e.transpose(out=tpx[:, qt * 128:(qt + 1) * 128], in_=mask[qt][:, kt * 128:(kt + 1) * 128],
                                                         identity=P.ident[:, :]), reads=[mask[qt], P.ident], writes=[tpx])
                P.op("act", lambda e: e.copy(out=mT[:, kt, :], in_=tpx[:, :]), reads=[tpx], writes=[mT])

        def back(s, qb, r):
            c0, c1 = qb * TB, (qb + 1) * TB
            mT = maskT[r]
            for h in range(4):
                A["acc"] = accs[st8["hcnt"] % 2]
                orr = st8["hcnt"] % 2
                st8["hcnt"] += 1
                attn_core(P, A, bqT[r][h][:, :], lambda kt: bkT[:, kt * 128:(kt + 1) * 128], lambda kt: bv1[:, kt, :],
                          qb, 128, 128 ** -0.5, [bqT[r][h], bkT, bv1], mask_of=lambda kt: (mT[:, kt, :], mT))
                for qt in range(4):
                    acc = A["acc"][qt // 2]
                    P.op("act", lambda e: e.activation(out=lnl[:, qt:qt + 1], in_=acc[:, qt % 2, 128:129], func=AF.Ln), reads=[acc], writes=[lnl])
                P.op("act", lambda e: e.activation(out=rc[:, :], in_=lnl[:, :], func=AF.Exp, scale=-1.0), reads=[lnl], writes=[rc])
                for qt in range(4):
                    acc = A["acc"][qt // 2]
                    P.op("act", lambda e: e.activation(out=ob[orr][:, qt, :], in_=acc[:, qt % 2, 0:128], func=AF.Identity, scale=rc[:, qt:qt + 1]),
                         reads=[acc, rc], writes=[ob[orr]])
                    P.op("pe", lambda e: e.transpose(out=tpx[:, qt * 128:(qt + 1) * 128], in_=ob[orr][:, qt, :], identity=P.ident[:, :]),
                         reads=[ob[orr], P.ident], writes=[tpx])
                P.op("act", lambda e: e.copy(out=oT[orr][:, :], in_=tpx[:, :]), reads=[tpx], writes=[oT[orr]])
                P.dma("sp", mixT[s, 512 + h * 128:512 + (h + 1) * 128, c0:c1], oT[orr][:, :], reads=[oT[orr]])

        blocks = [(s, qb) for s in range(n_s) for qb in range(n_qb)]
        load_idx_side(blocks[0][0])
        front_a(*blocks[0], 0)
        front_b(*blocks[0], 0)
        for i, (s, qb) in enumerate(blocks):
            if qb == 0:
                load_att_side(s)
            if i + 1 < len(blocks):
                s2, qb2 = blocks[i + 1]
                if qb2 == 0:
                    load_idx_side(s2)
                front_a(s2, qb2, (i + 1) % 2)
            back(s, qb, i % 2)
            if i + 1 < len(blocks):
                front_b(*blocks[i + 1], (i + 1) % 2)
    P.barrier()


O_Z, O_XBC, O_DT, O_MQ, O_MK, O_MV = 0, 1024, 2560, 2576, 3088, 3600


def l1_layout():
    fm_cols = list(range(O_XBC, O_XBC + 1536))
    types = [None] * 12
    for base in (O_MQ, O_MK):
        for p in range(2):
            A, B = pair_cols_h64(base, p)
            fm_cols += A + B
            types.append(0)
    tm_cols = list(range(O_Z, O_Z + 1024)) + list(range(O_MV, O_MV + 512)) + list(range(O_DT, O_DT + 16))
    return np.array(fm_cols), types, np.array(tm_cols)


def make_l1_cfg(T):
    def alloc(P, ctx):
        ex = {}
        ex["xb"] = [P.sb(ctx, f"xb{f}", [128, 3 + TB], F32) for f in range(12)]
        ex["cacc"] = [P.sb(ctx, f"cacc{r}", [128, TB], F32) for r in range(2)]
        ex["co"] = [P.sb(ctx, f"co{r}", [128, TB], BF16) for r in range(2)]
        ex["convw"] = P.sb(ctx, "convw", [128, 12, 4], F32)
        ex["convb"] = P.sb(ctx, "convb", [128, 12], F32)
        P.dma("sp", ex["convw"][:, :, :], T["convw"].rearrange("(f p) j -> p f j", p=128), writes=[ex["convw"]])
        P.dma("sp", ex["convb"][:, :], T["convb"], writes=[ex["convb"]])
        ex["fa"] = [P.sb(ctx, f"fa{r}", [128, TB], F32) for r in range(2)]
        ex["fb"] = [P.sb(ctx, f"fb{r}", [128, TB], F32) for r in range(2)]
        ex["km"] = [P.sb(ctx, f"km{r}", [128, 2, 2], F32) for r in range(2)]
        ex["tmo"] = [P.sb(ctx, f"tmo{r}", [128, 1536], BF16) for r in range(2)]
        ex["tmw"] = [P.sb(ctx, f"tmw{r}", [128, 16], F32) for r in range(2)]
        ex["cnt"] = 0
        ex["c2"] = 0
        ex["c3"] = 0
        return ex

    def plain(P, ex, s, tb, ft, ps):
        xb = ex["xb"][ft]
        k = ex["c2"] % 2
        ex["c2"] += 1
        acc, co = ex["cacc"][k], ex["co"][k]
        cw = ex["convw"]
        if tb == 0:
            P.op("pool", lambda e: e.memset(xb[:, 0:3], 0.0), writes=[xb])
        P.op("act", lambda e: e.copy(out=xb[:, 3:3 + TB], in_=ps[:, :]), reads=[ps], writes=[xb])
        P.op("dve", lambda e: e.tensor_scalar(out=acc[:, :], in0=xb[:, 0:TB], scalar1=cw[:, ft, 0:1], scalar2=None, op0=ALU.mult),
             reads=[xb, cw], writes=[acc])
        for j in range(1, 4):
            P.op("dve", lambda e: e.scalar_tensor_tensor(out=acc[:, :], in0=xb[:, j:j + TB], scalar=cw[:, ft, j:j + 1], in1=acc[:, :],
                                                         op0=ALU.mult, op1=ALU.add), reads=[xb, cw, acc], writes=[acc])
        P.op("act", lambda e: e.activation(out=co[:, :], in_=acc[:, :], func=AF.Silu, bias=ex["convb"][:, ft:ft + 1]),
             reads=[acc, ex["convb"]], writes=[co])
        P.op("pool", lambda e: e.tensor_copy(out=xb[:, 0:3], in_=xb[:, TB:TB + 3]), reads=[xb], writes=[xb])
        P.dma("sp", T["xbcT"][s, ft, :, tb * TB:(tb + 1) * TB], co[:, :], reads=[co])

    def rope_f32(pi):
        return True

    def rope_handler(P, ex, s, tb, pi, ft, t1, t2, t3, t4, oa, ob):
        k = ex["c3"] % 2
        ex["c3"] += 1
        fa, fb = ex["fa"][k], ex["fb"][k]
        P.op("pool", lambda e: e.tensor_tensor(out=fa[:, :], in0=t1[:, :], in1=t2[:, :], op=ALU.subtract), reads=[t1, t2], writes=[fa])
        P.op("pool", lambda e: e.tensor_tensor(out=fb[:, :], in0=t3[:, :], in1=t4[:, :], op=ALU.add), reads=[t3, t4], writes=[fb])
        P.op("act", lambda e: e.copy(out=oa[:, :], in_=fa[:, :]), reads=[fa], writes=[oa])
        P.op("act", lambda e: e.copy(out=ob[:, :], in_=fb[:, :]), reads=[fb], writes=[ob])
        pr = pi - 12
        if pr < 2:
            P.dma("sp", T["mqf"][s, 2 * pr, :, tb * TB:(tb + 1) * TB], fa[:, :], reads=[fa])
            P.dma("sp", T["mqf"][s, 2 * pr + 1, :, tb * TB:(tb + 1) * TB], fb[:, :], reads=[fb])
        else:
            km = ex["km"][k]
            P.op("dve", lambda e: e.tensor_reduce(out=km[:, 0, :], in_=fa[:, :].rearrange("p (b t) -> p b t", b=2), axis=AX.X, op=ALU.add),
                 reads=[fa], writes=[km])
            P.op("dve", lambda e: e.tensor_reduce(out=km[:, 1, :], in_=fb[:, :].rearrange("p (b t) -> p b t", b=2), axis=AX.X, op=ALU.add),
                 reads=[fb, km], writes=[km])
            P.op("dve", lambda e: e.tensor_scalar(out=km[:, :, :], in0=km[:, :, :], scalar1=1.0 / 256, scalar2=None, op0=ALU.mult),
                 reads=[km], writes=[km])
            q = pr - 2
            P.dma("sp", T["kmean"][s, 2 * q, :, 2 * tb:2 * tb + 2], km[:, 0, :], reads=[km])
            P.dma("sp", T["kmean"][s, 2 * q + 1, :, 2 * tb:2 * tb + 2], km[:, 1, :], reads=[km])

    def tm_handler(P, ex, s, tb, r, hnT, wtm, pst):
        for j in range(4):
            t0 = tb * TB + j * 128
            k = ex["cnt"] % 2
            ex["cnt"] += 1
            o, w = ex["tmo"][k], ex["tmw"][k]
            for grp in range(4):
                n0 = grp * 512
                nw = 512 if grp < 3 else 16
                pp = pst[grp % 2]
                for c in range(8):
                    P.op("pe", lambda e: e.matmul(pp[:, 0:nw], lhsT=hnT[:, c, j * 128:(j + 1) * 128], rhs=wtm[:, c, n0:n0 + nw],
                                                  start=(c == 0), stop=(c == 7)), reads=[hnT, wtm], writes=[pp])
                if grp < 3:
                    P.op("act", lambda e: e.copy(out=o[:, n0:n0 + 512], in_=pp[:, :]), reads=[pp], writes=[o])
                else:
                    P.op("act", lambda e: e.copy(out=w[:, :], in_=pp[:, 0:16]), reads=[pp], writes=[w])
            P.dma("sp", T["tm1"][s, t0:t0 + 128, :], o[:, :], reads=[o])
            P.dma("sp", T["dtraw"][s, :, t0 // 128, :], w[:, :], reads=[w])

    _, types1, _ = l1_layout()
    return dict(x=T["h2"], g=T["norms"][4], wfm=T["wfm1"], wtm=T["wtm1"], nfm=20, ntm=1552, types=types1, rope=T["rope"],
                fmT=T["fm1"], alloc=alloc, tm_handler=tm_handler, plain_handler=plain, rope_f32=rope_f32, rope_f32_handler=rope_handler)


BIGQ = 240000.0


def phase_moba_gate(P, nc, T):
    with ExitStack() as ctx:
        pm = P.sb(ctx, "pm", [128, 16, 16], F32)
        oh = P.sb(ctx, "oh", [128, 16, 16], F32)
        P.dma("sp", pm[:, :, :], T["pm"].partition_broadcast(128).rearrange("p (a b) -> p a b", a=16), writes=[pm])
        P.dma("sp", oh[:, :, :], T["oh"].partition_broadcast(128).rearrange("p (a b) -> p a b", a=16), writes=[oh])
        kmbd = P.sb(ctx, "kmbd", [128, 4, 64], F32)
        qf = [P.sb(ctx, f"qf{r}", [128, 4, TB], F32) for r in range(2)]
        g2 = P.sb(ctx, "g2", [128, 8, 16], F32)
        mx = P.sb(ctx, "mx", [128, 8, 8], F32)
        sel = P.sb(ctx, "sel", [128, 8, 16], F32)
        negm = [P.sb(ctx, f"negm{r}", [128, 128], BF16) for r in range(2)]
        nT = [P.sb(ctx, f"nT{r}", [128, TB], BF16) for r in range(2)]
        gps = [P.ps(ctx, f"gps{r}", [128, 128]) for r in range(2)]
        tpx = P.ps(ctx, "tpx", [128, TB], BF16)
        bi = 0
        k = 0
        for s in range(SPC):
            P.op("dve", lambda e: e.memset(kmbd[:, :, :], 0.0), writes=[kmbd])
            for f in range(4):
                for hl in range(4):
                    P.dma("sp", kmbd[32 * hl:32 * hl + 32, f, 16 * hl:16 * hl + 16], T["kmean"][s, f, 32 * hl:32 * hl + 32, :], writes=[kmbd])
            for tb in range(NTB):
                r = bi % 2
                bi += 1
                P.dma("sp", qf[r][:, :, :], T["mqf"][s, :, :, tb * TB:(tb + 1) * TB].rearrange("f p t -> p f t"), writes=[qf[r]])
                for j in range(4):
                    own = (tb * 4 + j) // 2
                    kk = k % 2
                    k += 1
                    gp = gps[kk]
                    for p in range(2):
                        for ab in range(2):
                            P.op("pe", lambda e: e.matmul(gp[:, p * 64:(p + 1) * 64], lhsT=qf[r][:, 2 * p + ab, j * 128:(j + 1) * 128],
                                                          rhs=kmbd[:, 2 * p + ab, :], start=(ab == 0), stop=(ab == 1)),
                                 reads=[qf[r], kmbd], writes=[gp])
                    P.op("dve", lambda e: e.tensor_tensor(out=g2[:, :, :], in0=gp[:, :].rearrange("p (h n) -> p h n", h=8),
                                                          in1=pm[:, own, :].unsqueeze(1).to_broadcast([128, 8, 16]), op=ALU.add),
                         reads=[gp, pm], writes=[g2])
                    for h in range(8):
                        P.op("dve", lambda e: e.max(out=mx[:, h, :], in_=g2[:, h, :]), reads=[g2], writes=[mx])
                    P.op("dve", lambda e: e.tensor_tensor(out=sel[:, :, :], in0=g2[:, :, :], in1=mx[:, :, 2:3].to_broadcast([128, 8, 16]),
                                                          op=ALU.is_ge), reads=[g2, mx], writes=[sel])
                    P.op("dve", lambda e: e.tensor_tensor(out=sel[:, :, :], in0=sel[:, :, :],
                                                          in1=oh[:, own, :].unsqueeze(1).to_broadcast([128, 8, 16]), op=ALU.max),
                         reads=[sel, oh], writes=[sel])
                    P.op("dve", lambda e: e.tensor_scalar(out=negm[kk][:, :].rearrange("p (h n) -> p h n", h=8), in0=sel[:, :, :],
                                                          scalar1=-1.0, scalar2=BIGQ, op0=ALU.add, op1=ALU.mult), reads=[sel], writes=[negm[kk]])
                    P.op("pe", lambda e: e.transpose(out=tpx[:, j * 128:(j + 1) * 128], in_=negm[kk][:, :], identity=P.ident[:, :]),
                         reads=[negm[kk], P.ident], writes=[tpx])
                P.op("act", lambda e: e.copy(out=nT[r][:, :], in_=tpx[:, :]), reads=[tpx], writes=[nT[r]])
                P.dma("sp", T["negT"][s, :, tb * TB:(tb + 1) * TB], nT[r][:, :], reads=[nT[r]])
    P.barrier()


def phase_moba_attn(P, nc, T):
    fm, tm1, mixT = T["fm1"], T["tm1"], T["mixT1"]
    with ExitStack() as ctx:
        tri = P.sb(ctx, "tri", [128, 128], BF16)
        P.dma("sp", tri[:, :], T["tri"], writes=[tri])
        ka = [P.sb(ctx, f"ka{r}", [80, S], BF16) for r in range(4)]
        v1 = [P.sb(ctx, f"v1{r}", [128, 32, 65], BF16) for r in range(4)]
        for r in range(4):
            P.op("pool", lambda e: e.memset(v1[r][:, :, 64:65], 1.0), writes=[v1[r]])
            P.dma("sp", ka[r][64:80, :], T["blk1h"], writes=[ka[r]])
        qa = [P.sb(ctx, f"qa{r}", [80, TB], BF16) for r in range(4)]
        A = {"st": [P.ps(ctx, f"st{r}", [128, TB]) for r in range(2)],
             "pt": [P.sb(ctx, f"pt{r}", [128, TB], BF16) for r in range(3)], "i": 0}
        accs = [[P.ps(ctx, f"acc{m}_{r}", [128, 2, 256]) for r in range(2)] for m in range(2)]
        tpx = P.ps(ctx, "tpx", [128, TB], BF16)
        rc = P.sb(ctx, "rc", [128, 4], F32)
        ob = [P.sb(ctx, f"ob{r}", [128, 4, 128], BF16) for r in range(2)]
        oT = [P.sb(ctx, f"oT{r}", [128, TB], BF16) for r in range(2)]
        hpi = 0
        qi = 0
        for s in range(SPC):
            for hp in range(4):
                kb = 2 * (hpi % 2)
                hpi += 1
                for hh in range(2):
                    h = 2 * hp + hh
                    p, hl = h // 4, h % 4
                    P.dma("sp", ka[kb + hh][0:32, :], fm[s, 16 + 2 * p, 32 * hl:32 * hl + 32, :], writes=[ka[kb + hh]])
                    P.dma("sp", ka[kb + hh][32:64, :], fm[s, 17 + 2 * p, 32 * hl:32 * hl + 32, :], writes=[ka[kb + hh]])
                    for half in range(2):
                        P.dma("sp", v1[kb + hh][:, 16 * half:16 * half + 16, 0:64],
                              tm1[s, 2048 * half:2048 * (half + 1), 1024 + h * 64:1024 + (h + 1) * 64].rearrange("(kt p) e -> p kt e", p=128),
                              writes=[v1[kb + hh]])
                for qb in range(NTB):
                    qr = 2 * (qi % 2)
                    orr = qi % 2
                    qi += 1
                    c0, c1 = qb * TB, (qb + 1) * TB
                    for hh in range(2):
                        h = 2 * hp + hh
                        p, hl = h // 4, h % 4
                        q = qa[qr + hh]
                        P.dma("sp", q[0:32, :], fm[s, 12 + 2 * p, 32 * hl:32 * hl + 32, c0:c1], writes=[q])
                        P.dma("sp", q[32:64, :], fm[s, 13 + 2 * p, 32 * hl:32 * hl + 32, c0:c1], writes=[q])
                        P.dma("sp", q[64:80, :], T["negT"][s, h * 16:(h + 1) * 16, c0:c1], writes=[q])
                    for hh in range(2):
                        q = qa[qr + hh]
                        kk, vv = ka[kb + hh], v1[kb + hh]
                        A["acc"] = accs[hh]
                        attn_core(P, A, q[0:80, :], lambda kt: kk[0:80, kt * 128:(kt + 1) * 128], lambda kt: vv[:, kt, :],
                                  qb, 64, 0.125, [q, kk, vv], tri=tri)
                        for qt in range(4):
                            acc = A["acc"][qt // 2]
                            P.op("dve", lambda e: e.reciprocal(out=rc[:, qt:qt + 1], in_=acc[:, qt % 2, 64:65]), reads=[acc], writes=[rc])
                            P.op("dve", lambda e: e.tensor_scalar(out=ob[orr][:, qt, hh * 64:(hh + 1) * 64], in0=acc[:, qt % 2, 0:64],
                                                                  scalar1=rc[:, qt:qt + 1], scalar2=None, op0=ALU.mult),
                                 reads=[acc, rc], writes=[ob[orr]])
                    for qt in range(4):
                        P.op("pe", lambda e: e.transpose(out=tpx[:, qt * 128:(qt + 1) * 128], in_=ob[orr][:, qt, :], identity=P.ident[:, :]),
                             reads=[ob[orr], P.ident], writes=[tpx])
                    P.op("act", lambda e: e.copy(out=oT[orr][:, :], in_=tpx[:, :]), reads=[tpx], writes=[oT[orr]])
                    P.dma("sp", mixT[s, 1024 + hp * 128:1024 + (hp + 1) * 128, c0:c1], oT[orr][:, :], reads=[oT[orr]])
    P.barrier()


def v3(ap2d, a):
    return ap2d.rearrange("p (a b) -> p a b", a=a)


def phase_ssd(P, nc, T):
    xbcT, tm1, mixT = T["xbcT"], T["tm1"], T["mixT1"]
    dbg = T.get("dbg", {})
    n_s2, n_ch = dbg.get("n_s", SPC), dbg.get("n_ch", 32)
    upto = dbg.get("upto", 99)
    pre = dbg.get("pre", 99)
    pre3 = dbg.get("pre3", 99)
    with ExitStack() as ctx:
        tri = P.sb(ctx, "tri", [128, 128], BF16)
        P.dma("sp", tri[:, :], T["tri"], writes=[tri])
        ones = P.sb(ctx, "ones", [128, 128], BF16)
        dsp = [P.sb(ctx, f"dsp{i}", [128, 512], BF16) for i in range(3)]
        dres = P.sb(ctx, "dres", [128, 512], F32)
        P.op("dve", lambda e: e.memset(ones[:, :], 1.0), writes=[ones])
        gn = P.sb(ctx, "gn", [128, 1024], F32)
        P.dma("sp", gn[:, :], T["ssm_norm"].partition_broadcast(128), writes=[gn])
        dsk = P.sb(ctx, "dsk", [128, 1024], F32)
        P.dma("sp", dsk[:, :], T["dsk"].partition_broadcast(128), writes=[dsk])
        dtb = P.sb(ctx, "dtb", [128, 16], F32)
        P.dma("sp", dtb[:, :], T["dt_bias"].partition_broadcast(128), writes=[dtb])
        abc_ = P.sb(ctx, "a_bc", [128, 16], F32)
        P.dma("sp", abc_[:, :], T["a_log"].partition_broadcast(128), writes=[abc_])
        P.op("act", lambda e: e.activation(out=abc_[:, :], in_=abc_[:, :], func=AF.Exp), reads=[abc_], writes=[abc_])
        P.op("dve", lambda e: e.tensor_scalar(out=abc_[:, :], in0=abc_[:, :], scalar1=-1.0, scalar2=None, op0=ALU.mult), reads=[abc_], writes=[abc_])
        dt_all = P.sb(ctx, "dt_all", [128, 512], F32)
        da_all = P.sb(ctx, "da_all", [128, 512], F32)
        nac = P.sb(ctx, "nac", [128, 512], F32)
        eac = P.sb(ctx, "eac", [128, 512], F32)
        eal = P.sb(ctx, "eal", [128, 512], F32)
        state = P.sb(ctx, "state", [128, 1024], F32)
        stt = P.sb(ctx, "stt", [128, 1024], F32)
        state_bf = P.sb(ctx, "state_bf", [128, 1024], BF16)
        Rm = [P.sb(ctx, f"Rm{i}", [128, 2048], BF16) for i in range(2)]
        exa = P.sb(ctx, "exa", [128, 2048], F32)
        dec = P.sb(ctx, "dec", [128, 2048], F32)
        cbm = P.sb(ctx, "cbm", [128, 256], BF16)
        scT = P.sb(ctx, "scT", [128, 2048], BF16)
        xT = [P.sb(ctx, f"xT{r}", [128, 8, 128], BF16) for r in range(2)]
        bcT = [P.sb(ctx, f"bcT{r}", [128, 4, 128], BF16) for r in range(2)]
        zt = [P.sb(ctx, f"zt{r}", [128, 1024], BF16) for r in range(2)]
        xtm = P.sb(ctx, "xtm", [128, 1024], BF16)
        xdt = P.sb(ctx, "xdt", [128, 1024], BF16)
        xdtt = P.sb(ctx, "xdtt", [128, 1024], BF16)
        btm = P.sb(ctx, "btm", [128, 256], BF16)
        ytmp = P.sb(ctx, "ytmp", [128, 1024], F32)
        y = P.sb(ctx, "y", [128, 1024], F32)
        t2 = P.sb(ctx, "t2", [128, 1024], F32)
        sz = P.sb(ctx, "sz", [128, 1024], F32)
        yn = P.sb(ctx, "yn", [128, 1024], BF16)
        junk = P.sb(ctx, "junk", [128, 512], BF16)
        ss = P.sb(ctx, "ss", [128, 2], F32)
        lnv = P.sb(ctx, "lnv", [128, 2], F32)
        rstd = P.sb(ctx, "rstd", [128, 2], F32)
        yT = [P.sb(ctx, f"yT{r}", [128, 8, TB], BF16) for r in range(2)]
        abc = P.ps(ctx, "abc", [128, 1024])
        cbp = P.ps(ctx, "cbp", [128, 512])
        tpb = P.ps(ctx, "tpb", [128, 1024], BF16)
        yps = [P.ps(ctx, f"yps{g}", [128, 512]) for g in range(2)]
        yip = [P.ps(ctx, f"yip{g}", [128, 512]) for g in range(2)]
        ci = 0
        for s in range(n_s2):
            if pre >= 1:
                P.dma("sp", dt_all[:, :], T["dtraw"][s].rearrange("p c h -> p (c h)"), writes=[dt_all])
            if pre >= 1:
                P.op("dve", lambda e: e.tensor_tensor(out=v3(dt_all[:, :], 32), in0=v3(dt_all[:, :], 32),
                                                      in1=dtb[:, :].unsqueeze(1).to_broadcast([128, 32, 16]), op=ALU.add), reads=[dt_all, dtb], writes=[dt_all])
            if pre >= 2:
                P.op("act", lambda e: e.activation(out=dt_all[:, :], in_=dt_all[:, :], func=AF.Exp), reads=[dt_all], writes=[dt_all])
            if pre >= 2:
                P.op("act", lambda e: e.activation(out=dt_all[:, :], in_=dt_all[:, :], func=AF.Ln, bias=P.one_t[:, 0:1]), reads=[dt_all, P.one_t], writes=[dt_all])
            if pre >= 2:
                P.op("dve", lambda e: e.tensor_tensor(out=v3(da_all[:, :], 32), in0=v3(dt_all[:, :], 32),
                                                      in1=abc_[:, :].unsqueeze(1).to_broadcast([128, 32, 16]), op=ALU.mult), reads=[dt_all, abc_], writes=[da_all])
            if pre >= 3:
                if pre3 >= 1:
                    P.op("dve", lambda e: e.tensor_copy(out=dsp[0][:, :], in_=da_all[:, :]), reads=[da_all], writes=[dsp[0]])
                if pre3 >= 2:
                    P.op("dve", lambda e: e.tensor_tensor(out=dres[:, :], in0=da_all[:, :], in1=dsp[0][:, :], op=ALU.subtract), reads=[da_all, dsp[0]], writes=[dres])
                if pre3 >= 3:
                    P.op("dve", lambda e: e.tensor_copy(out=dsp[1][:, :], in_=dres[:, :]), reads=[dres], writes=[dsp[1]])
                if pre3 >= 4:
                    P.op("dve", lambda e: e.tensor_tensor(out=dres[:, :], in0=dres[:, :], in1=dsp[1][:, :], op=ALU.subtract), reads=[dres, dsp[1]], writes=[dres])
                if pre3 >= 5:
                    P.op("dve", lambda e: e.tensor_copy(out=dsp[2][:, :], in_=dres[:, :]), reads=[dres], writes=[dsp[2]])
                if pre3 >= 6:
                    for i in range(3):
                        P.op("pe", lambda e: e.matmul(yps[0][:, :], lhsT=tri[:, :], rhs=dsp[i][:, :], start=(i == 0), stop=(i == 2)), reads=[tri, dsp[i]], writes=[yps[0]])
                if pre3 >= 7:
                    P.op("dve", lambda e: e.tensor_scalar(out=nac[:, :], in0=yps[0][:, :], scalar1=-1.0, scalar2=None, op0=ALU.mult), reads=[yps[0]], writes=[nac])
                if pre3 >= 8:
                    P.op("act", lambda e: e.activation(out=eac[:, :], in_=nac[:, :], func=AF.Exp, scale=-1.0), reads=[nac], writes=[eac])
            if pre >= 4:
                for i in range(3):
                    P.op("pe", lambda e: e.matmul(yps[1][:, :], lhsT=ones[:, :], rhs=dsp[i][:, :], start=(i == 0), stop=(i == 2)), reads=[ones, dsp[i]], writes=[yps[1]])
                P.op("dve", lambda e: e.tensor_copy(out=eal[:, :], in_=yps[1][:, :]), reads=[yps[1]], writes=[eal])
                P.op("act", lambda e: e.activation(out=eal[:, :], in_=eal[:, :], func=AF.Exp), reads=[eal], writes=[eal])
            if pre >= 5:
                P.op("dve", lambda e: e.memset(state[:, :], 0.0), writes=[state])
            if pre >= 5:
                P.op("pool", lambda e: e.memset(state_bf[:, :], 0.0), writes=[state_bf])

            def loads(ch, r):
                t0 = ch * 128
                P.dma("sp", xT[r][:, :, :], xbcT[s, 0:8, :, t0:t0 + 128].rearrange("f p t -> p f t"), writes=[xT[r]])
                P.dma("sp", bcT[r][:, :, :], xbcT[s, 8:12, :, t0:t0 + 128].rearrange("f p t -> p f t"), writes=[bcT[r]])
                P.dma("sp", zt[r][:, :], tm1[s, t0:t0 + 128, 0:1024], writes=[zt[r]])

            if pre >= 6:
                loads(0, ci % 2)
            for ch in range(n_ch if pre >= 6 else 0):
                r = ci % 2
                ci += 1
                if ch + 1 < n_ch:
                    loads(ch + 1, ci % 2)
                c16 = slice(ch * 16, ch * 16 + 16)
                if upto < 1:
                    continue
                for g in range(2):
                    P.op("pe", lambda e: e.matmul(cbp[:, g * 128:(g + 1) * 128], lhsT=bcT[r][:, g, :], rhs=bcT[r][:, 2 + g, :], start=True, stop=True),
                         reads=[bcT[r]], writes=[cbp])
                P.op("dve", lambda e: e.tensor_tensor(out=v3(cbm[:, :], 2), in0=v3(cbp[:, 0:256], 2),
                                                      in1=tri[:, :].unsqueeze(1).to_broadcast([128, 2, 128]), op=ALU.mult), reads=[cbp, tri], writes=[cbm])
                if upto < 2:
                    continue
                for i in range(2):
                    P.op("dve", lambda e: e.tensor_tensor(out=v3(Rm[i][:, :], 16), in0=tri[:, :].unsqueeze(1).to_broadcast([128, 16, 128]),
                                                          in1=dsp[i][:, c16].unsqueeze(2).to_broadcast([128, 16, 128]), op=ALU.mult),
                         reads=[tri, dsp[i]], writes=[Rm[i]])
                for g in range(2):
                    for q in range(2):
                        for i in range(2):
                            P.op("pe", lambda e: e.matmul(abc[:, q * 512:(q + 1) * 512], lhsT=ones[:, :],
                                                          rhs=Rm[i][:, g * 1024 + q * 512:g * 1024 + (q + 1) * 512],
                                                          start=(i == 0), stop=(i == 1)), reads=[ones, Rm[i]], writes=[abc])
                    for hl in range(8):
                        h = 8 * g + hl
                        P.op("dve", lambda e: e.tensor_scalar(out=exa[:, h * 128:(h + 1) * 128], in0=abc[:, hl * 128:(hl + 1) * 128],
                                                              scalar1=nac[:, ch * 16 + h:ch * 16 + h + 1], scalar2=None, op0=ALU.add),
                             reads=[abc, nac], writes=[exa])
                if upto < 3:
                    continue
                P.op("dve", lambda e: e.tensor_scalar(out=exa[:, :], in0=exa[:, :], scalar1=0.0, scalar2=None, op0=ALU.min), reads=[exa], writes=[exa])
                P.op("act", lambda e: e.activation(out=dec[:, :], in_=exa[:, :], func=AF.Exp), reads=[exa], writes=[dec])
                if upto < 4:
                    continue
                for g in range(2):
                    P.op("dve", lambda e: e.tensor_tensor(out=v3(scT[:, g * 1024:(g + 1) * 1024], 8), in0=v3(dec[:, g * 1024:(g + 1) * 1024], 8),
                                                          in1=cbm[:, g * 128:(g + 1) * 128].unsqueeze(1).to_broadcast([128, 8, 128]), op=ALU.mult),
                         reads=[dec, cbm], writes=[scT])
                if upto < 5:
                    continue
                for f in range(8):
                    P.op("pe", lambda e: e.transpose(out=tpb[:, f * 128:(f + 1) * 128], in_=xT[r][:, f, :], identity=P.ident[:, :]),
                         reads=[xT[r], P.ident], writes=[tpb])
                P.op("act", lambda e: e.copy(out=xtm[:, :], in_=tpb[:, :]), reads=[tpb], writes=[xtm])
                P.op("dve", lambda e: e.tensor_tensor(out=v3(xdt[:, :], 16), in0=v3(xtm[:, :], 16),
                                                      in1=dt_all[:, c16].unsqueeze(2).to_broadcast([128, 16, 64]), op=ALU.mult),
                     reads=[xtm, dt_all], writes=[xdt])
                if upto < 6:
                    continue
                for h in range(16):
                    P.op("pe", lambda e: e.matmul(yps[h // 8][:, (h % 8) * 64:(h % 8 + 1) * 64], lhsT=scT[:, h * 128:(h + 1) * 128],
                                                  rhs=xdt[:, h * 64:(h + 1) * 64], start=True, stop=True), reads=[scT, xdt], writes=[yps[h // 8]])
                for g in range(2):
                    P.op("pe", lambda e: e.matmul(yip[g][:, :], lhsT=bcT[r][:, 2 + g, :], rhs=state_bf[:, g * 512:(g + 1) * 512], start=True, stop=True),
                         reads=[bcT[r], state_bf], writes=[yip[g]])
                if upto < 7:
                    continue
                for g in range(2):
                    hs = slice(g * 512, (g + 1) * 512)
                    P.op("dve", lambda e: e.tensor_tensor(out=v3(ytmp[:, hs], 8), in0=v3(yip[g][:, :], 8),
                                                          in1=eac[:, ch * 16 + 8 * g:ch * 16 + 8 * g + 8].unsqueeze(2).to_broadcast([128, 8, 64]), op=ALU.mult),
                         reads=[yip[g], eac], writes=[ytmp])
                    P.op("dve", lambda e: e.tensor_tensor(out=y[:, hs], in0=yps[g][:, :], in1=ytmp[:, hs], op=ALU.add), reads=[yps[g], ytmp], writes=[y])
                if upto < 8:
                    continue
                P.op("pool", lambda e: e.tensor_tensor(out=t2[:, :], in0=xtm[:, :], in1=dsk[:, :], op=ALU.mult), reads=[xtm, dsk], writes=[t2])
                P.op("pool", lambda e: e.tensor_tensor(out=y[:, :], in0=y[:, :], in1=t2[:, :], op=ALU.add), reads=[y, t2], writes=[y])
                P.op("act", lambda e: e.activation(out=sz[:, :], in_=zt[r][:, :], func=AF.Silu), reads=[zt[r]], writes=[sz])
                P.op("dve", lambda e: e.tensor_tensor(out=y[:, :], in0=y[:, :], in1=sz[:, :], op=ALU.mult), reads=[y, sz], writes=[y])
                if upto < 9:
                    continue
                for g in range(2):
                    P.op("act", lambda e: e.activation(out=junk[:, :], in_=y[:, g * 512:(g + 1) * 512], func=AF.Square, accum_out=ss[:, g:g + 1]),
                         reads=[y], writes=[junk, ss])
                rstd_from_ss(P, ss, lnv, rstd, 2, 512)
                for g in range(2):
                    hs = slice(g * 512, (g + 1) * 512)
                    P.op("dve", lambda e: e.scalar_tensor_tensor(out=yn[:, hs], in0=y[:, hs], scalar=rstd[:, g:g + 1], in1=gn[:, hs],
                                                                 op0=ALU.mult, op1=ALU.mult), reads=[y, rstd, gn], writes=[yn])
                if upto < 10:
                    continue
                P.op("dve", lambda e: e.tensor_tensor(out=v3(xdtt[:, :], 16), in0=v3(xdt[:, :], 16),
                                                      in1=v3(dec[:, :], 16)[:, :, 127:128].to_broadcast([128, 16, 64]), op=ALU.mult),
                     reads=[xdt, dec], writes=[xdtt])
                for g in range(2):
                    P.op("pe", lambda e: e.transpose(out=tpb[:, g * 128:(g + 1) * 128], in_=bcT[r][:, g, :], identity=P.ident[:, :]),
                         reads=[bcT[r], P.ident], writes=[tpb])
                P.op("act", lambda e: e.copy(out=btm[:, :], in_=tpb[:, 0:256]), reads=[tpb], writes=[btm])
                for g in range(2):
                    P.op("pe", lambda e: e.matmul(yip[g][:, :], lhsT=btm[:, g * 128:(g + 1) * 128], rhs=xdtt[:, g * 512:(g + 1) * 512], start=True, stop=True),
                         reads=[btm, xdtt], writes=[yip[g]])
                for g in range(2):
                    hs = slice(g * 512, (g + 1) * 512)
                    P.op("pool", lambda e: e.tensor_tensor(out=v3(stt[:, hs], 8), in0=v3(state[:, hs], 8),
                                                           in1=eal[:, ch * 16 + 8 * g:ch * 16 + 8 * g + 8].unsqueeze(2).to_broadcast([128, 8, 64]), op=ALU.mult),
                         reads=[state, eal], writes=[stt])
                    P.op("dve", lambda e: e.tensor_tensor(out=state[:, hs], in0=yip[g][:, :], in1=stt[:, hs], op=ALU.add), reads=[yip[g], stt], writes=[state])
                P.op("act", lambda e: e.copy(out=state_bf[:, :], in_=state[:, :]), reads=[state], writes=[state_bf])
                if upto < 11:
                    continue
                yr = (ci // 4) % 2 if False else ((s * 32 + ch) // 4) % 2
                for f in range(8):
                    P.op("pe", lambda e: e.transpose(out=tpb[:, f * 128:(f + 1) * 128], in_=yn[:, f * 128:(f + 1) * 128], identity=P.ident[:, :]),
                         reads=[yn, P.ident], writes=[tpb])
                P.op("act", lambda e: e.copy(out=yT[yr][:, :, (ch % 4) * 128:(ch % 4 + 1) * 128], in_=v3(tpb[:, :], 8)), reads=[tpb], writes=[yT[yr]])
                if ch % 4 == 3:
                    tb = ch // 4
                    P.dma("sp", mixT[s, 0:1024, tb * TB:(tb + 1) * TB].rearrange("(f p) t -> p f t", p=128), yT[yr][:, :, :], reads=[yT[yr]])
    P.barrier()
```
